# Optimizing a Trainium2 kernel written in Bass

```python
import jax, jax.numpy as jnp
from jax import lax
import numpy as np


D_MODEL = 1024
BATCH = 16
SEQ = 4096
DEPTH = 1

GRID_W = 64
CTX_LEN = 256
N_HEADS = 8
N_KV_HEADS = 2
HEAD_DIM = 64
WINDOW = 128
BLOCK = 128
ROPE_BASE = 10000.0
M_HEADS = 4
M_DIM = 128
CHUNK = 128
CONV_W = 3
ATT_WIDTH = N_HEADS * HEAD_DIM
KV_WIDTH = N_KV_HEADS * HEAD_DIM
M_WIDTH = M_HEADS * M_DIM
MIX_WIDTH = ATT_WIDTH + M_WIDTH
IN_SPLITS = (ATT_WIDTH, KV_WIDTH, KV_WIDTH, M_WIDTH, M_WIDTH, M_WIDTH, M_WIDTH, 4 * M_HEADS)
IN_COLS = sum(IN_SPLITS)
N_EXPERTS = 16
EXPERT_FF = 512
CAPACITY = 2
EPS = 1e-6
NEG = -1e30

kernel_name = 'hybrid_swa_mlstm_ecmoe_dit_block'

f32 = jnp.float32


def rmsnorm(x, w):
    xf = x.astype(f32)
    y = xf * lax.rsqrt(jnp.mean(xf * xf, -1, keepdims=True) + EPS)
    return (y * w.astype(f32)).astype(x.dtype)


def modulate(h, shift, scale):
    return h * (1 + scale) + shift


def axial_rope_tables(n):
    rows = n // GRID_W
    row, col = jnp.meshgrid(jnp.arange(rows), jnp.arange(GRID_W), indexing='ij')
    n_freq = HEAD_DIM // 4
    freqs = ROPE_BASE ** (-jnp.arange(n_freq, dtype=f32) / n_freq)
    ang = jnp.concatenate([row.reshape(-1, 1).astype(f32) * freqs,
                           col.reshape(-1, 1).astype(f32) * freqs], -1)
    return jnp.cos(ang), jnp.sin(ang)


def apply_rope(x, cos, sin):
    shape = (1, cos.shape[0]) + (1,) * (x.ndim - 3) + (cos.shape[1],)
    cos = cos.reshape(shape)
    sin = sin.reshape(shape)
    x1, x2 = jnp.split(x.astype(f32), 2, -1)
    return jnp.concatenate([x1 * cos - x2 * sin, x2 * cos + x1 * sin], -1).astype(x.dtype)


def short_conv(u, w):
    n = u.shape[1]
    r = CONV_W // 2
    up = jnp.pad(u, ((0, 0), (r, r), (0, 0)))
    out = up[:, 0:n] * w[0]
    for j in range(1, CONV_W):
        out = out + up[:, j:j + n] * w[j]
    return out


def mixer_inputs(h, w_in, b_gates, conv_qk, q_norm_w, k_norm_w):
    B, N, _ = h.shape
    offs = np.cumsum(IN_SPLITS)[:-1].tolist()
    aq, ak, av, mq, mk, mv, mo, gates = jnp.split(h @ w_in, offs, -1)
    aq = rmsnorm(aq.reshape(B, N, N_KV_HEADS, N_HEADS // N_KV_HEADS, HEAD_DIM), q_norm_w)
    ak = rmsnorm(ak.reshape(B, N, N_KV_HEADS, HEAD_DIM), k_norm_w)
    av = av.reshape(B, N, N_KV_HEADS, HEAD_DIM)
    mqk = jax.nn.silu(short_conv(jnp.concatenate([mq, mk], -1), conv_qk))
    mq, mk = jnp.split(mqk, 2, -1)

    def heads(a):
        return a.reshape(B, N, M_HEADS, M_DIM).transpose(0, 2, 1, 3).astype(f32)

    g = (gates + b_gates).astype(f32).reshape(B, N, 4, M_HEADS).transpose(2, 0, 3, 1)
    li_f, f_f, li_b, f_b = g[0], g[1], g[2], g[3]
    return (aq, ak, av, heads(mq) * (M_DIM ** -0.5), heads(mk), heads(mv), mo,
            li_f, jax.nn.log_sigmoid(f_f), li_b, jax.nn.log_sigmoid(f_b))


def latent_attention(q, k, v, k_ctx, v_ctx, sink):
    B, N = q.shape[:2]
    L = k_ctx.shape[1]
    nb = N // BLOCK
    span = BLOCK + 2 * WINDOW
    pad = ((0, 0), (WINDOW, WINDOW), (0, 0), (0, 0))
    idx = jnp.arange(nb)[:, None] * BLOCK + jnp.arange(span)[None]
    k_blk = jnp.moveaxis(jnp.pad(k, pad)[:, idx], 1, 0)
    v_blk = jnp.moveaxis(jnp.pad(v, pad)[:, idx], 1, 0)
    q_blk = jnp.moveaxis(q.reshape((B, nb, BLOCK) + q.shape[2:]), 1, 0)
    scale = HEAD_DIM ** -0.5
    sink_f = sink.astype(f32)

    def one_block(args):
        qb, kb, vb, n = args
        qpos = n * BLOCK + jnp.arange(BLOCK)
        kpos = n * BLOCK - WINDOW + jnp.arange(span)
        ok = (kpos[None] >= 0) & (kpos[None] < N) & (jnp.abs(qpos[:, None] - kpos[None]) <= WINDOW)
        s_loc = jnp.einsum('bqkgd,bskd->bkgqs', qb, kb, preferred_element_type=f32) * scale
        s_loc = jnp.where(ok, s_loc, NEG)
        s_ctx = jnp.einsum('bqkgd,bckd->bkgqc', qb, k_ctx, preferred_element_type=f32) * scale
        s_sink = jnp.broadcast_to(sink_f[None, :, :, None, None], s_loc.shape[:-1] + (1,))
        p = jax.nn.softmax(jnp.concatenate([s_loc, s_ctx, s_sink], -1), -1)
        p_loc = p[..., :span].astype(vb.dtype)
        p_ctx = p[..., span:span + L].astype(vb.dtype)
        return (jnp.einsum('bkgqs,bskd->bqkgd', p_loc, vb)
                + jnp.einsum('bkgqc,bckd->bqkgd', p_ctx, v_ctx))

    o = lax.map(one_block, (q_blk, k_blk, v_blk, jnp.arange(nb)))
    return jnp.moveaxis(o, 0, 1).reshape(B, N, ATT_WIDTH)


def context_attention(q, k, v, sink):
    B, L = q.shape[:2]
    s = jnp.einsum('bqkgd,bckd->bkgqc', q, k, preferred_element_type=f32) * (HEAD_DIM ** -0.5)
    s_sink = jnp.broadcast_to(sink.astype(f32)[None, :, :, None, None], s.shape[:-1] + (1,))
    p = jax.nn.softmax(jnp.concatenate([s, s_sink], -1), -1)[..., :L].astype(v.dtype)
    return jnp.einsum('bkgqc,bckd->bqkgd', p, v).reshape(B, L, ATT_WIDTH)


def zero_state(batch):
    return (jnp.zeros((batch, M_HEADS, M_DIM, M_DIM), f32),
            jnp.zeros((batch, M_HEADS, M_DIM), f32),
            jnp.zeros((batch, M_HEADS), f32))


def mlstm_state_update(state, k, v, li, lf):
    C, n, m = state
    b = jnp.cumsum(lf, -1)
    b_last = b[..., -1]
    w = b_last[..., None] - b + li
    m_new = jnp.maximum(b_last + m, w.max(-1))
    a = jnp.exp(b_last + m - m_new)
    ws = jnp.exp(w - m_new[..., None])
    C_new = a[..., None, None] * C + jnp.einsum('bhs,bhsd,bhsv->bhdv', ws, k, v)
    n_new = a[..., None] * n + jnp.einsum('bhs,bhsd->bhd', ws, k)
    return (C_new, n_new, m_new)


def mlstm_chunk(state, xs):
    q, k, v, li, lf = xs
    C, n, m = state
    b = jnp.cumsum(lf, -1)
    within = jnp.tril(jnp.ones((CHUNK, CHUNK), bool))
    d = jnp.where(within, b[..., :, None] - b[..., None, :] + li[..., None, :], -jnp.inf)
    inter = b + m[..., None]
    m_t = jnp.maximum(inter, d.max(-1))
    wts = jnp.exp(d - m_t[..., None]) * jnp.einsum('bhtd,bhsd->bhts', q, k)
    decay = jnp.exp(inter - m_t)
    num = jnp.einsum('bhts,bhsv->bhtv', wts, v) + decay[..., None] * jnp.einsum('bhtd,bhdv->bhtv', q, C)
    den = wts.sum(-1) + decay * jnp.einsum('bhtd,bhd->bht', q, n)
    h = num / jnp.maximum(jnp.abs(den), jnp.exp(-m_t))[..., None]
    return mlstm_state_update(state, k, v, li, lf), h


def mlstm_scan(q, k, v, li, lf, state):
    B, H, N, _ = q.shape
    nc = N // CHUNK

    def chunks(a):
        return jnp.moveaxis(a.reshape((B, H, nc, CHUNK) + a.shape[3:]), 2, 0)

    state, h = lax.scan(mlstm_chunk, state, (chunks(q), chunks(k), chunks(v), chunks(li), chunks(lf)))
    return jnp.moveaxis(h, 0, 2).reshape(B, H, N, M_DIM), state


def flip(a):
    return jnp.flip(a, 2)


def bidir_mlstm(mq, mk, mv, li_f, lf_f, li_b, lf_b, st_f, st_b):
    h_f, end_f = mlstm_scan(mq, mk, mv, li_f, lf_f, st_f)
    h_b, end_b = mlstm_scan(flip(mq), flip(mk), flip(mv), flip(li_b), flip(lf_b), st_b)
    return h_f + flip(h_b), end_f, end_b


def mlstm_out(h_sum, mo, norm_w):
    B, H, N, _ = h_sum.shape
    h = jnp.moveaxis(h_sum, 1, 2)
    h = h * lax.rsqrt(jnp.mean(h * h, -1, keepdims=True) + EPS) * norm_w.astype(f32).reshape(M_HEADS, M_DIM)
    return (h.reshape(B, N, M_WIDTH) * jax.nn.sigmoid(mo.astype(f32))).astype(mo.dtype)


def expert_choice_ffn(h, w_router, w_gate, w_up, w_down):
    B, N, _ = h.shape
    cap = CAPACITY * N // N_EXPERTS
    aff = jax.nn.softmax((h @ w_router).astype(f32), -1)
    gate, idx = lax.top_k(jnp.swapaxes(aff, 1, 2), cap)
    bidx = jnp.arange(B)[:, None, None]
    xg = h[bidx, idx]
    g = jnp.einsum('becd,edf->becf', xg, w_gate)
    u = jnp.einsum('becd,edf->becf', xg, w_up)
    y = jnp.einsum('becf,efd->becd', jax.nn.silu(g) * u, w_down) * gate[..., None].astype(h.dtype)
    return jnp.zeros_like(h).at[bidx, idx].add(y)


def layer(x, ctx, mod_x, mod_c, norm1_w, norm2_w, w_in, b_gates, conv_qk, q_norm_w, k_norm_w,
          sink, mlstm_norm_w, w_out, w_router, w_gate, w_up, w_down, update_ctx):
    B = x.shape[0]
    sh1, sc1, g1, sh2, sc2, g2 = jnp.split(mod_x[:, None, :], 6, -1)
    csh1, csc1, cg1, csh2, csc2, cg2 = jnp.split(mod_c, 6, -1)
    sink_r = sink.reshape(N_KV_HEADS, N_HEADS // N_KV_HEADS)

    hx = modulate(rmsnorm(x, norm1_w), sh1, sc1)
    hc = modulate(rmsnorm(ctx, norm1_w), csh1, csc1)
    aq, ak, av, mq, mk, mv, mo, li_f, lf_f, li_b, lf_b = mixer_inputs(hx, w_in, b_gates, conv_qk, q_norm_w, k_norm_w)
    caq, cak, cav, cmq, cmk, cmv, cmo, cli_f, clf_f, cli_b, clf_b = mixer_inputs(hc, w_in, b_gates, conv_qk, q_norm_w, k_norm_w)

    cos, sin = axial_rope_tables(x.shape[1])
    att = latent_attention(apply_rope(aq, cos, sin), apply_rope(ak, cos, sin), av, cak, cav, sink_r)

    if update_ctx:
        hc_sum, st_f, st_b = bidir_mlstm(cmq, cmk, cmv, cli_f, clf_f, cli_b, clf_b, zero_state(B), zero_state(B))
        c_mix = jnp.concatenate([context_attention(caq, cak, cav, sink_r), mlstm_out(hc_sum, cmo, mlstm_norm_w)], -1)
        ctx_new = ctx + cg1 * (c_mix @ w_out)
        hc2 = modulate(rmsnorm(ctx_new, norm2_w), csh2, csc2)
        ctx_new = ctx_new + cg2 * expert_choice_ffn(hc2, w_router, w_gate, w_up, w_down)
    else:
        st_f = mlstm_state_update(zero_state(B), cmk, cmv, cli_f, clf_f)
        st_b = mlstm_state_update(zero_state(B), flip(cmk), flip(cmv), flip(cli_b), flip(clf_b))
        ctx_new = ctx

    h_sum, _, _ = bidir_mlstm(mq, mk, mv, li_f, lf_f, li_b, lf_b, st_f, st_b)
    mix = jnp.concatenate([att, mlstm_out(h_sum, mo, mlstm_norm_w)], -1)
    x = x + g1 * (mix @ w_out)

    hx2 = modulate(rmsnorm(x, norm2_w), sh2, sc2)
    x = x + g2 * expert_choice_ffn(hx2, w_router, w_gate, w_up, w_down)
    return x, ctx_new


def setup_inputs(seed: int = 0) -> dict:
    key = jax.random.key(seed)
    ks = jax.random.split(key, 24)
    D = D_MODEL

    def nrm(k, shape, scale):
        return jax.random.normal(k, shape, f32) * scale

    f_bias = 3.0 + 0.5 * jax.random.normal(ks[20], (DEPTH, 2, M_HEADS), f32)
    i_bias = 0.1 * jax.random.normal(ks[21], (DEPTH, 2, M_HEADS), f32)
    b_gates = jnp.stack([i_bias[:, 0], f_bias[:, 0], i_bias[:, 1], f_bias[:, 1]], 1).reshape(DEPTH, 4 * M_HEADS)
    return {
        'x': nrm(ks[0], (BATCH, SEQ, D), 1.0),
        'c': nrm(ks[1], (BATCH, D), 1.0),
        'ctx': nrm(ks[2], (BATCH, CTX_LEN, D), 1.0),
        'c_ctx': nrm(ks[3], (D,), 1.0),
        'w_mod': nrm(ks[4], (DEPTH, D, 6 * D), 0.5 * D ** -0.5),
        'b_mod': nrm(ks[5], (DEPTH, 6 * D), 0.02),
        'norm1_w': 1.0 + nrm(ks[6], (DEPTH, D), 0.02),
        'norm2_w': 1.0 + nrm(ks[7], (DEPTH, D), 0.02),
        'w_in': nrm(ks[8], (DEPTH, D, IN_COLS), D ** -0.5),
        'b_gates': b_gates,
        'conv_qk': nrm(ks[9], (DEPTH, CONV_W, 2 * M_WIDTH), CONV_W ** -0.5),
        'q_norm_w': 1.0 + nrm(ks[10], (DEPTH, HEAD_DIM), 0.02),
        'k_norm_w': 1.0 + nrm(ks[11], (DEPTH, HEAD_DIM), 0.02),
        'sink': nrm(ks[12], (DEPTH, N_HEADS), 0.5),
        'mlstm_norm_w': 1.0 + nrm(ks[13], (DEPTH, M_WIDTH), 0.02),
        'w_out': nrm(ks[14], (DEPTH, MIX_WIDTH, D), MIX_WIDTH ** -0.5),
        'w_router': nrm(ks[15], (DEPTH, D, N_EXPERTS), D ** -0.5),
        'w_gate': nrm(ks[16], (DEPTH, N_EXPERTS, D, EXPERT_FF), D ** -0.5),
        'w_up': nrm(ks[17], (DEPTH, N_EXPERTS, D, EXPERT_FF), D ** -0.5),
        'w_down': nrm(ks[18], (DEPTH, N_EXPERTS, EXPERT_FF, D), EXPERT_FF ** -0.5),
    }


def reference(x, c, ctx, c_ctx, w_mod, b_mod, norm1_w, norm2_w, w_in, b_gates, conv_qk, q_norm_w,
              k_norm_w, sink, mlstm_norm_w, w_out, w_router, w_gate, w_up, w_down):
    for l in range(DEPTH):
        mod_x = jax.nn.silu(c) @ w_mod[l] + b_mod[l]
        mod_c = jax.nn.silu(c_ctx) @ w_mod[l] + b_mod[l]
        x, ctx = layer(x, ctx, mod_x, mod_c, norm1_w[l], norm2_w[l], w_in[l], b_gates[l], conv_qk[l],
                       q_norm_w[l], k_norm_w[l], sink[l], mlstm_norm_w[l], w_out[l], w_router[l],
                       w_gate[l], w_up[l], w_down[l], l < DEPTH - 1)
    return x
```

```python
import numpy as np
from contextlib import ExitStack
import concourse.bass as bass
import concourse.mybir as mybir
from concourse.bass_utils import run_bass_kernel_spmd

F32 = mybir.dt.float32
BF16 = mybir.dt.bfloat16
I32 = mybir.dt.int32
U32 = mybir.dt.uint32
AF = mybir.ActivationFunctionType
ALU = mybir.AluOpType
AX = mybir.AxisListType

D = 1024
SEQ = 4096
NT = SEQ // 128
NG = SEQ // 512
CTX = 256
NB = 2
EPS = 1e-6
NEXP = 16
CAP = 512
QS = 128 ** -0.5
C_AQ, C_MO, C_AK, C_AV, C_MV, C_GT, C_MQ, C_MK = 0, 512, 1024, 1152, 1280, 1792, 1808, 2320
SEM_CHUNK = 16000
CFG = {"stop": None, "nb": NB, "ng1": None, "ng2": None}


class Sched:
    ENGS = ("tensor", "vector", "scalar", "gpsimd", "sync")

    def __init__(self, nc, tag):
        self.nc = nc
        self.tag = tag
        self.ops = []
        self.last_writer = {}
        self.readers = {}
        self.eng_count = {e: 0 for e in self.ENGS}
        self.dma_count = {}
        self.sem_names = set()
        self.group_keys = set()

    def _event_compute(self, eng):
        c = self.eng_count[eng]
        self.eng_count[eng] = c + 1
        name = "%sp_%s_%d" % (self.tag, eng, c // SEM_CHUNK)
        self.sem_names.add(name)
        return (name, (c % SEM_CHUNK) + 1, 1)

    def _event_dma(self, key):
        c = self.dma_count.get(key, 0)
        self.dma_count[key] = c + 1
        per = SEM_CHUNK // 16
        name = "%sd_%s_%d" % (self.tag, key, c // per)
        self.sem_names.add(name)
        return (name, ((c % per) + 1) * 16, 16)

    def op(self, eng, fn, reads=(), writes=(), dma=None):
        def expand(keys):
            out = []
            for k in keys:
                if isinstance(k, tuple) and len(k) > 0 and k[0] == "MULTI":
                    out.extend(k[1:])
                else:
                    out.append(k)
            return out
        reads = expand(reads)
        writes = expand(writes)
        waits = {}

        def need(ev):
            if ev is None:
                return
            n, v, _ = ev
            if waits.get(n, 0) < v:
                waits[n] = v

        for r in reads:
            need(self.last_writer.get(r))
        for w in writes:
            need(self.last_writer.get(w))
            for ev in self.readers.get(w, ()):
                need(ev)
        ev = self._event_dma(dma) if dma is not None else self._event_compute(eng)
        for r in reads:
            self.readers.setdefault(r, []).append(ev)
        for w in writes:
            self.last_writer[w] = ev
            self.readers[w] = []
        self.ops.append((eng, fn, waits, ev))

    def emit(self):
        nc = self.nc
        with ExitStack() as es:
            sems = {}
            for n in sorted(self.sem_names):
                sems[n] = es.enter_context(nc.semaphore(n))
            block = es.enter_context(nc.Block())
            final_waits = {}
            for (eng, fn, waits, ev) in self.ops:
                n, v, _ = ev
                if final_waits.get(n, 0) < v:
                    final_waits[n] = v
            gnames = set("%sd_%s_0" % (self.tag, k) for k in self.group_keys) if CFG.get("gk", 1) else set()

            def make(engname):
                def body(e):
                    seen = {}
                    for (eng, fn, waits, ev) in self.ops:
                        if eng != engname:
                            continue
                        for n, v in waits.items():
                            if engname == "tensor" and "p_tensor_" in n:
                                continue
                            if n in gnames:
                                v = final_waits[n]
                            if seen.get(n, 0) >= v:
                                continue
                            e.wait_ge(sems[n], v)
                            seen[n] = v
                        ins = fn(e)
                        ins.then_inc(sems[ev[0]], ev[2])
                    if engname == "sync":
                        for n, v in final_waits.items():
                            if seen.get(n, 0) >= v:
                                continue
                            e.wait_ge(sems[n], v)
                return body

            block.tensor(make("tensor"))
            block.vector(make("vector"))
            block.scalar(make("scalar"))
            block.gpsimd(make("gpsimd"))
            block.sync(make("sync"))


class Buf:
    def __init__(self, t, k):
        self.t = t
        self.k = k

    def __getitem__(self, idx):
        return self.t[idx]


def build_program():
    nc = bass.Bass("TRN2", target_bir_lowering=False)

    def din(name, shape, dt=F32):
        return nc.dram_tensor(name, list(shape), dt, kind="ExternalInput").ap()

    x_d = din("x", [NB * SEQ, D])
    ctx_d = din("ctx", [NB * CTX, D])
    cvec_d = din("cvec", [3, D])
    wmod_d = din("w_mod", [D, 6 * D])
    bmod_d = din("b_mod", [1, 6 * D])
    n1_d = din("norm1_w", [1, D])
    n2_d = din("norm2_w", [1, D])
    win_d = din("w_in", [D, 2832])
    bg_d = din("b_gates", [1, 16])
    conv_d = din("conv_qk", [3, D])
    qn_d = din("q_norm_w", [1, 64])
    kn_d = din("k_norm_w", [1, 64])
    sink_d = din("sink", [1, 8])
    mn_d = din("mlstm_norm_w", [1, 512])
    wout_d = din("w_out", [D, D])
    wr_d = din("w_router", [D, NEXP])
    if CFG["stop"] is None:
        wg_d = din("w_gate", [NEXP, D, 512])
        wu_d = din("w_up", [NEXP, D, 512])
        wd_d = din("w_down", [NEXP, 512, D])
    cos_d = din("rope_cos", [SEQ, 32])
    sin_d = din("rope_sin", [SEQ, 32])
    idb_d = din("ident_bf", [128, 128], BF16)
    idf_d = din("ident_f", [128, 128])
    trif_d = din("tri_f", [128, 128])
    trib_d = din("tri_b", [128, 128])
    nones_d = din("neg_ones", [128, 128])
    mskf_d = din("mask_f", [128, 128], BF16)
    mskb_d = din("mask_b", [128, 128], BF16)
    amlo_d = din("amask_lo", [128, 128], BF16)
    amhi_d = din("amask_hi", [128, 128], BF16)
    boff_d = din("boff", [64, 1])
    anti_d = din("anti_ident", [128, 128])
    out_d = nc.dram_tensor("out", [NB * SEQ, D], F32, kind="ExternalOutput").ap()
    hx2s_d = nc.dram_tensor("hx2s", [NB * SEQ, D], BF16).ap()
    modsc_d = nc.dram_tensor("modsc", [3, 6 * D], F32).ap()
    cdb_d = nc.dram_tensor("cdbs", [NT, 128, 4, 130], BF16).ap()

    with ExitStack() as es0:
        def sb0(name, shape, dt=F32):
            return Buf(es0.enter_context(nc.sbuf_tensor(name, list(shape), dt)), name)

        aff_all = sb0("aff_all", [128, NT // 2, 2, NB, NEXP])
        identb = sb0("identb", [128, 128], BF16)
        identf = sb0("identf", [128, 128])

        with ExitStack() as es:
            S = Sched(nc, "m")

            def sb(name, shape, dt=F32):
                return Buf(es.enter_context(nc.sbuf_tensor(name, list(shape), dt)), name)

            def ps(name, shape, dt=F32):
                return Buf(es.enter_context(nc.psum_tensor(name, list(shape), dt)), name)

            PB = [ps("mb%d" % i, [128, 512]) for i in range(4)]
            pbi = [0]

            def nextps():
                p = PB[pbi[0] % 4]
                pbi[0] += 1
                return p

            def dma(eng, out, in_, r, w, key, **kw):
                S.op(eng, lambda e: e.dma_start(out=out, in_=in_, **kw), reads=r, writes=w, dma=key)

            def mm(out, lhsT, rhs, start, stop, r, w):
                S.op("tensor", lambda e: e.matmul(out, lhsT, rhs, start=start, stop=stop), reads=r, writes=w)

            def tr(out, in_, ident, r, w):
                S.op("tensor", lambda e: e.transpose(out, in_, ident), reads=r, writes=w)

            def tt(eng, out, in0, in1, op, r, w):
                S.op(eng, lambda e: e.tensor_tensor(out=out, in0=in0, in1=in1, op=op), reads=r, writes=w)

            def cp(eng, out, in_, r, w):
                S.op(eng, lambda e: e.tensor_copy(out=out, in_=in_), reads=r, writes=w)

            PK = "prm0"
            S.group_keys.add(PK)

            def load_const(buf, src, eng="sync", **kw):
                dma(eng, buf[:], src, [], [buf.k], PK, **kw)

            load_const(identb, idb_d)
            load_const(identf, idf_d)
            c3 = sb("c3", [3, D]); load_const(c3, cvec_d)
            S.op("scalar", lambda e: e.activation(out=c3[:], in_=c3[:], func=AF.Silu), reads=[c3.k], writes=[c3.k])
            sT = sb("sT", [128, 8, 3])
            p = nextps()
            for kc in range(8):
                tr(p[:, kc * 4:kc * 4 + 3], c3[:, kc * 128:(kc + 1) * 128], identf[0:3, 0:3], [c3.k, identf.k], [p.k])
            cp("vector", sT[:], p[:, 0:32].rearrange("p (a b) -> p a b", a=8)[:, :, 0:3], [p.k], [sT.k])
            bm3 = sb("bm3", [3, 6 * D]); load_const(bm3, bmod_d[0].partition_broadcast(3))
            modrows = sb("modrows", [3, 6 * D])
            wmc = [sb("wmc%d" % i, [128, 8, 512]) for i in range(4)]
            for ci in range(12):
                wb = wmc[ci % 4]
                dma("sync", wb[:], wmod_d.rearrange("(c p) n -> p c n", p=128)[:, :, ci * 512:(ci + 1) * 512], [], [wb.k], wb.k)
                p = nextps()
                for kc in range(8):
                    mm(p[0:3, :], sT[:, kc, :], wb[:, kc, :], kc == 0, kc == 7, [sT.k, wb.k], [p.k])
                tt("vector", modrows[:, ci * 512:(ci + 1) * 512], p[0:3, :], bm3[:, ci * 512:(ci + 1) * 512], ALU.add,
                   [p.k, bm3.k], [modrows.k])
            dma("sync", modsc_d, modrows[:], [modrows.k], ["modsc"], modrows.k)

            S.emit()

        if CFG["stop"] == "mod0":
            return nc
        with ExitStack() as es:
            S = Sched(nc, "a")

            def sb(name, shape, dt=F32):
                return Buf(es.enter_context(nc.sbuf_tensor(name, list(shape), dt)), name)

            def ps(name, shape, dt=F32):
                return Buf(es.enter_context(nc.psum_tensor(name, list(shape), dt)), name)

            PB = [ps("pb%d" % i, [128, 512]) for i in range(7)]
            PT = ps("ptb", [128, 1024], BF16)
            class RR:
                def __init__(self, banks):
                    self.b = banks
                    self.i = 0

                def next(self):
                    p = self.b[self.i % len(self.b)]
                    self.i += 1
                    return p

            al_all = RR(PB)
            cur_al = [al_all]

            def nextps():
                return cur_al[0].next()

            def run(pairs, bg=None):
                live = list(pairs)
                while live:
                    for item in list(live):
                        cur_al[0] = item[1]
                        try:
                            for _ in range(item[2] if len(item) > 2 else 1):
                                next(item[0])
                        except StopIteration:
                            live.remove(item)
                    if bg:
                        cur_al[0] = al_all
                        try:
                            next(bg[0])
                        except StopIteration:
                            bg.pop(0)
                cur_al[0] = al_all

            def drain(bg):
                cur_al[0] = al_all
                while bg:
                    try:
                        next(bg[0])
                    except StopIteration:
                        bg.pop(0)

            def dma(eng, out, in_, r, w, key, **kw):
                S.op(eng, lambda e: e.dma_start(out=out, in_=in_, **kw), reads=r, writes=w, dma=key)

            def mm(out, lhsT, rhs, start, stop, r, w):
                S.op("tensor", lambda e: e.matmul(out, lhsT, rhs, start=start, stop=stop), reads=r, writes=w)

            def tr(out, in_, ident, r, w):
                S.op("tensor", lambda e: e.transpose(out, in_, ident), reads=r, writes=w)

            def act(out, in_, func, r, w, bias=None, scale=None, accum=None):
                kw = {}
                if bias is not None:
                    kw["bias"] = bias
                if scale is not None:
                    kw["scale"] = scale
                if accum is not None:
                    kw["accum_out"] = accum
                S.op("scalar", lambda e: e.activation(out=out, in_=in_, func=func, **kw), reads=r, writes=w)

            def tt(eng, out, in0, in1, op, r, w):
                S.op(eng, lambda e: e.tensor_tensor(out=out, in0=in0, in1=in1, op=op), reads=r, writes=w)

            def ts(eng, out, in0, s1, s2, op0, op1, r, w):
                if op1 is None:
                    S.op(eng, lambda e: e.tensor_scalar(out=out, in0=in0, scalar1=s1, scalar2=None, op0=op0), reads=r, writes=w)
                else:
                    S.op(eng, lambda e: e.tensor_scalar(out=out, in0=in0, scalar1=s1, scalar2=s2, op0=op0, op1=op1), reads=r, writes=w)

            def stt(out, in0, scalar, in1, op0, op1, r, w):
                S.op("vector", lambda e: e.scalar_tensor_tensor(out=out, in0=in0, scalar=scalar, in1=in1, op0=op0, op1=op1), reads=r, writes=w)

            def cp(eng, out, in_, r, w):
                if eng == "scalar":
                    S.op(eng, lambda e: e.copy(out=out, in_=in_), reads=r, writes=w)
                else:
                    S.op(eng, lambda e: e.tensor_copy(out=out, in_=in_), reads=r, writes=w)

            def red(out, in_, r, w, op=ALU.add):
                S.op("vector", lambda e: e.tensor_reduce(out=out, in_=in_, axis=AX.X, op=op), reads=r, writes=w)

            def recip(out, in_, r, w):
                S.op("vector", lambda e: e.reciprocal(out=out, in_=in_), reads=r, writes=w)

            def mset(eng, out, val, w):
                S.op(eng, lambda e: e.memset(out, val), writes=w)

            DBG = {}

            def dump(name, buf, ap, shape, dt=F32):
                if not CFG.get("dbg") or name in DBG:
                    return
                d = nc.dram_tensor("dbg_" + name, list(shape), dt, kind="ExternalOutput").ap()
                DBG[name] = d
                dma("sync", d, ap, [buf.k], ["dbg_" + name], "dbgk")

            PK = "prm"
            S.group_keys.update([PK])
            def load_const(buf, src, eng="sync", **kw):
                dma(eng, buf[:], src, [], [buf.k], PK, **kw)

            trif = sb("trif", [128, 128]); load_const(trif, trif_d)
            trib = sb("trib", [128, 128]); load_const(trib, trib_d)
            nones = sb("nones", [128, 128]); load_const(nones, nones_d)
            mskf = sb("mskf", [128, 128], BF16); load_const(mskf, mskf_d)
            mskb = sb("mskb", [128, 128], BF16); load_const(mskb, mskb_d)
            amlo = sb("amlo", [128, 128], BF16); load_const(amlo, amlo_d)
            amhi = sb("amhi", [128, 128], BF16); load_const(amhi, amhi_d)
            cosT = sb("cosT", [128, NT, 32]); load_const(cosT, cos_d.rearrange("(n p) f -> p n f", p=128))
            sinT = sb("sinT", [128, NT, 32]); load_const(sinT, sin_d.rearrange("(n p) f -> p n f", p=128))
            bgB = sb("bgB", [128, 16]); load_const(bgB, bg_d[0].partition_broadcast(128))
            qnB = sb("qnB", [128, 64]); load_const(qnB, qn_d[0].partition_broadcast(128))
            knB = sb("knB", [128, 64]); load_const(knB, kn_d[0].partition_broadcast(128))
            mnB = sb("mnB", [128, 512]); load_const(mnB, mn_d[0].partition_broadcast(128))
            esink = sb("esink", [128, 8]); load_const(esink, sink_d[0].partition_broadcast(128))
            act(esink[:], esink[:], AF.Exp, [esink.k], [esink.k])
            cwT = sb("cwT", [128, 8, 3])
            for jj in range(3):
                S.op("sync", lambda e, jj=jj: e.dma_start(out=cwT[:, :, jj], in_=conv_d[jj].rearrange("(c p) -> p c", p=128),
                                                         allow_slow_non_contiguous=True), writes=[cwT.k], dma="cwk")
            if CFG["stop"] == "s1":
                S.emit()
                return nc
            wr = sb("wr", [128, 8, NEXP]); load_const(wr, wr_d.rearrange("(c p) n -> p c n", p=128))
            win = sb("win", [128, 8, 2832], BF16)
            hTB = sb("hTB", [128, 8, 512], BF16)
            hTB.k = ("MULTI",) + tuple(("hTB", i_, j_) for i_ in range(4) for j_ in range(4))
            hTBf = hTB[:].rearrange("p a b -> p (a b)").bitcast(F32)
            stg_ap = [hTBf[:, i_ * 512:(i_ + 1) * 512] for i_ in range(4)]
            stg_k = [("MULTI",) + tuple(("hTB", i_, j_) for j_ in range(4)) for i_ in range(4)]
            stc1 = [0]
            for kc in range(8):
                for c0 in range(0, 2832, 512):
                    w_ = min(512, 2832 - c0)
                    si = stc1[0] % 4
                    eng = ("scalar", "gpsimd", "vector")[stc1[0] % 3]
                    stc1[0] += 1
                    dma("sync", stg_ap[si][:, 0:w_], win_d[kc * 128:(kc + 1) * 128, c0:c0 + w_], [], [stg_k[si]], "stg%d" % si)
                    cp(eng, win[:, kc, c0:c0 + w_], stg_ap[si][:, 0:w_], [stg_k[si]], [win.k])
            wout = sb("wout", [128, 8, D], BF16)

            def load_wout_scaled():
                for kc in range(8):
                    for c0 in range(0, D, 512):
                        si = stc1[0] % 4
                        stc1[0] += 1
                        dma("sync", stg_ap[si], wout_d[kc * 128:(kc + 1) * 128, c0:c0 + 512], [], [stg_k[si]], "stg%d" % si)
                        tt("vector", wout[:, kc, c0:c0 + 512], stg_ap[si], t1[:, c0:c0 + 512], ALU.mult, [stg_k[si], t1.k], [wout.k])

            if CFG["stop"] == "s2":
                S.emit()
                return nc
            A1 = sb("A1", [128, D]); B1 = sb("B1", [128, D])
            A2 = sb("A2", [128, D]); B2 = sb("B2", [128, D])
            t1 = sb("t1", [128, D])
            t1.k = ("MULTI", "t1a", "t1b")

            def load_mod(row, want):
                def bc(i):
                    return modsc_d[row, i * D:(i + 1) * D].partition_broadcast(128)
                if "A1" in want:
                    dma("sync", t1[:], bc(1), [], [t1.k], "t1d")
                    dma("sync", A1[:], n1_d[0].partition_broadcast(128), [], [A1.k], A1.k)
                    stt(A1[:], t1[:], 1.0, A1[:], ALU.add, ALU.mult, [t1.k, A1.k], [A1.k])
                if "B1" in want:
                    dma("sync", B1[:], bc(0), [], [B1.k], B1.k)
                if "G1" in want:
                    dma("sync", t1[:], bc(2), [], [t1.k], "t1d")
                if "A2" in want:
                    dma("sync", t1[:], bc(4), [], [t1.k], "t1d")
                    dma("sync", A2[:], n2_d[0].partition_broadcast(128), [], [A2.k], A2.k)
                    stt(A2[:], t1[:], 1.0, A2[:], ALU.add, ALU.mult, [t1.k, A2.k], [A2.k])
                if "B2" in want:
                    dma("sync", B2[:], bc(3), [], [B2.k], B2.k)

            xt = [sb("xt%d" % i, [128, D]) for i in range(2)]
            hx = [sb("hx%d" % i, [128, D], BF16) for i in range(2)]
            hT = sb("hT", [128, 8, 512], BF16)
            hT.k = ("MULTI",) + tuple(("hT", j_) for j_ in range(4))
            HB = [hT, hTB]

            def tile_keys(hb_, j_):
                if hb_ is hT:
                    return [("hT", j_)]
                return [("hTB", i_, j_) for i_ in range(4)]
            st4 = [sb("st4_%d" % i, [128, 4]) for i in range(2)]
            HS = sb("HS", [128, 8, 2 * NG])
            KT = sb("KT", [128, SEQ + CTX], BF16)
            V = sb("V", [128, NT + 2, 2, 65], BF16)
            KT.k = ("MULTI",) + tuple(("KT", i_) for i_ in range(NT + 2))
            V.k = ("MULTI",) + tuple(("V", i_) for i_ in range(NT + 2))
            mset("vector", V[:], 1.0, [V.k])
            CdS = [sb("CdS%d" % i, [128, 4, 130], BF16) for i in range(2)]
            CdL = [sb("CdL%d" % i, [128, 4, 130], BF16) for i in range(2)]
            Sst = {"f": sb("Sf", [128, 4, 130]), "b": sb("Sb", [128, 4, 130])}
            Sdec = sb("Sdec", [128, 4, 130])
            CdF = sb("CdF", [128, 4, 130], BF16)
            mqT = sb("mqT", [128, 4, 512], BF16)
            mkT = sb("mkT", [128, 4, 512], BF16)
            mqT.k = ("MULTI",) + tuple(("mqT", c_) for c_ in range(4))
            mkT.k = ("MULTI",) + tuple(("mkT", c_) for c_ in range(4))
            Kt = sb("Kt", [128, 4, 512], BF16)
            qk_sq = sb("qk_sq", [128, 512])
            qk_t = sb("qk_t", [128, 512])
            qk_a = sb("qk_a", [128, 256])
            qk_b = sb("qk_b", [128, 256])
            qk_o = sb("qk_o", [128, 512], BF16)
            qk_ss = sb("qk_ss", [128, 10])
            QT = sb("QT", [128, 4, 128], BF16)
            PTs = sb("PTs", [128, 5, 512], BF16)
            arec = sb("arec", [128, 4])
            hrs = sb("hrs", [128, 4])
            mixr = [sb("mix%d" % i, [128, D], BF16) for i in range(2)]
            cv = Buf(t1.t, t1.k)
            mixT = sb("mixT", [128, 8, 128], BF16)
            mvs = sb("mvs", [128, 512])
            sig = sb("sig", [128, 512])
            gt = sb("gt", [128, 16])
            l1 = sb("l1", [128, 8])
            g8 = sb("g8", [128, 8])
            ws8 = sb("ws8", [128, 8])
            E8 = sb("E8", [128, 8])
            dec4 = sb("dec4", [128, 4])
            Vw = {"f": sb("Vwf", [128, 4, 130], BF16), "b": sb("Vwb", [128, 4, 130], BF16)}
            Sm = {"f": sb("Smf", [128, 4, 128], BF16), "b": sb("Smb", [128, 4, 128], BF16)}
            sc8 = sb("sc8", [128, 8])
            sc8n = sb("sc8n", [128, 8])
            hs = sb("hs", [128, 512])
            xn = [sb("xn%d" % i, [128, D]) for i in range(1)]
            h2 = t1
            h2b = [sb("h2b%d" % i, [128, D], BF16) for i in range(1)]
            h2T = sb("h2T", [128, 8, 128])
            rt = sb("rt", [128, 16])
            rs1 = sb("rs1", [128, 1])
            rs2 = sb("rs2", [128, 1])
            if CFG["stop"] == "s3":
                S.emit()
                return nc
            ring = {"xt": 0, "hx": 0, "xn": 0, "h2b": 0, "st": 0, "cds": 0, "cdl": 0}

            def rr(name, lst):
                b = lst[ring[name] % len(lst)]
                ring[name] += 1
                return b

            def norm_mod(src_ap_rows, npart, A, Bm, out_buf, xbufs, xname):
                xb = rr(xname, xbufs)
                dma("sync", xb[0:npart, :], src_ap_rows, [], [xb.k], xb.k)
                s4 = rr("st", st4)
                act(out_buf[0:npart, :], xb[0:npart, :], AF.Square, [xb.k], [out_buf.k, s4.k], accum=s4[0:npart, 0:1])
                act(s4[0:npart, 1:2], s4[0:npart, 0:1], AF.Ln, [s4.k], [s4.k], bias=EPS, scale=1.0 / D)
                act(s4[0:npart, 2:3], s4[0:npart, 1:2], AF.Exp, [s4.k], [s4.k], scale=-0.5)
                stt(out_buf[0:npart, :], xb[0:npart, :], s4[0:npart, 2:3], A[0:npart, :], ALU.mult, ALU.mult,
                    [xb.k, s4.k, A.k], [out_buf.k])
                tt("vector", out_buf[0:npart, :], out_buf[0:npart, :], Bm[0:npart, :], ALU.add, [out_buf.k, Bm.k], [out_buf.k])
                return xb

            def gen_stage_tile(src_rows_fn, j, A, Bm, hb_=None):
                hb_ = hT if hb_ is None else hb_
                hb = rr("hx", hx)
                xb = rr("xt", xt)
                dma("sync", xb[:], src_rows_fn(j), [], [xb.k], xb.k)
                s4 = rr("st", st4)
                yield
                act(hb[:], xb[:], AF.Square, [xb.k], [hb.k, s4.k], accum=s4[:, 0:1])
                act(s4[:, 1:2], s4[:, 0:1], AF.Ln, [s4.k], [s4.k], bias=EPS, scale=1.0 / D)
                act(s4[:, 2:3], s4[:, 1:2], AF.Exp, [s4.k], [s4.k], scale=-0.5)
                yield
                stt(hb[:], xb[:], s4[:, 2:3], A[:], ALU.mult, ALU.mult, [xb.k, s4.k, A.k], [hb.k])
                yield
                tt("vector", hb[:], hb[:], Bm[:], ALU.add, [hb.k, Bm.k], [hb.k])
                yield
                for kc in range(8):
                    tr(PT[:, kc * 128:(kc + 1) * 128], hb[:, kc * 128:(kc + 1) * 128], identb[:], [hb.k, identb.k], [PT.k])
                cp("scalar", hb_[:, :, j * 128:(j + 1) * 128], PT[:].rearrange("p (a b) -> p a b", a=8), [PT.k], tile_keys(hb_, j))
                yield

            def stage_a(src_rows_fn, ntile, A, Bm, hb_=None):
                for j0 in range(0, ntile, 2):
                    run([(gen_stage_tile(src_rows_fn, j, A, Bm, hb_), al_all) for j in range(j0, min(j0 + 2, ntile))])

            def gen_feat_chunk(col0, cc, N, dst, cw0, hprev, hnext, hb_=None):
                hb_ = hT if hb_ is None else hb_
                HTK = [hb_.k]
                p = nextps()
                base = (cc % 2) * 512
                ck = "t1a" if cc % 2 == 0 else "t1b"
                cvv = lambda a_, b_: t1[:, base + a_:base + b_]
                for kc in range(8):
                    mm(p[:, 0:N], win[:, kc, col0 + cc * 128: col0 + (cc + 1) * 128], hb_[:, kc, 0:N], kc == 0, kc == 7,
                       [win.k] + HTK, [p.k])
                yield
                act(cvv(0, N), p[:, 0:N], AF.Identity, [p.k, cwT.k], [ck], scale=cwT[:, cw0 + cc, 1:2])
                yield
                stt(cvv(1, N), p[:, 0:N - 1], cwT[:, cw0 + cc, 0:1], cvv(1, N), ALU.mult, ALU.add, [p.k, cwT.k, ck], [ck])
                stt(cvv(0, N - 1), p[:, 1:N], cwT[:, cw0 + cc, 2:3], cvv(0, N - 1), ALU.mult, ALU.add, [p.k, cwT.k, ck], [ck])
                if hprev is not None:
                    stt(cvv(0, 1), HS[:, cw0 + cc, hprev:hprev + 1], cwT[:, cw0 + cc, 0:1], cvv(0, 1), ALU.mult, ALU.add,
                        [HS.k, cwT.k, ck], [ck])
                if hnext is not None:
                    stt(cvv(N - 1, N), HS[:, cw0 + cc, hnext:hnext + 1], cwT[:, cw0 + cc, 2:3], cvv(N - 1, N), ALU.mult, ALU.add,
                        [HS.k, cwT.k, ck], [ck])
                yield
                act(dst[:, cc, 0:N], cvv(0, N), AF.Silu, [ck], [dst.k[1 + cc]])
                yield

            def feat_proj(col0, nchunks, N, dst, cw0, hprev, hnext, hb_=None):
                for c0 in range(0, nchunks, 2):
                    run([(gen_feat_chunk(col0, cc, N, dst, cw0, hprev, hnext, hb_), al_all) for cc in range(c0, c0 + 2)])

            def k_tokmajor(ntile):
                for j in range(ntile):
                    for h in range(4):
                        tr(PT[:, h * 128:(h + 1) * 128], mkT[:, h, j * 128:(j + 1) * 128], identb[:], [mkT.k, identb.k], [PT.k])
                    cp("vector", Kt[:, j, :], PT[:, 0:512], [PT.k], [Kt.k])

            def qk_norm_rope(src, srck, H, wB, tile_n, out_ap):
                W = H * 64
                s3 = lambda a: a.rearrange("p (h d) -> p h d", h=H)
                act(qk_sq[:, 0:W], src, AF.Square, [srck], [qk_sq.k])
                red(qk_ss[:, 0:H], s3(qk_sq[:, 0:W]), [qk_sq.k], [qk_ss.k])
                act(qk_ss[:, 0:H], qk_ss[:, 0:H], AF.Ln, [qk_ss.k], [qk_ss.k], bias=EPS, scale=1.0 / 64)
                act(qk_ss[:, 0:H], qk_ss[:, 0:H], AF.Exp, [qk_ss.k], [qk_ss.k], scale=-0.5)
                tt("vector", s3(qk_t[:, 0:W]), s3(src), qk_ss[:, 0:H].unsqueeze(2).to_broadcast([128, H, 64]), ALU.mult,
                   [srck, qk_ss.k], [qk_t.k])
                wbc = wB[:, :].unsqueeze(1).to_broadcast([128, H, 64])
                if tile_n is None:
                    tt("gpsimd", s3(out_ap), s3(qk_t[:, 0:W]), wbc, ALU.mult, [qk_t.k, wB.k], [out_ap.tensor.name if False else "qk_o"])
                    return
                tt("gpsimd", s3(qk_t[:, 0:W]), s3(qk_t[:, 0:W]), wbc, ALU.mult, [qk_t.k, wB.k], [qk_t.k])
                x1 = s3(qk_t[:, 0:W])[:, :, 0:32]
                x2 = s3(qk_t[:, 0:W])[:, :, 32:64]
                cb = cosT[:, tile_n, :].unsqueeze(1).to_broadcast([128, H, 32])
                sbn = sinT[:, tile_n, :].unsqueeze(1).to_broadcast([128, H, 32])
                h3 = lambda a: a.rearrange("p (h d) -> p h d", h=H)
                a_ = h3(qk_a[:, 0:H * 32]); b_ = h3(qk_b[:, 0:H * 32])
                o3 = s3(out_ap)
                tt("vector", a_, x1, cb, ALU.mult, [qk_t.k, cosT.k], [qk_a.k])
                tt("gpsimd", b_, x2, sbn, ALU.mult, [qk_t.k, sinT.k], [qk_b.k])
                tt("vector", o3[:, :, 0:32], a_, b_, ALU.subtract, [qk_a.k, qk_b.k], ["qk_o"])
                tt("vector", a_, x2, cb, ALU.mult, [qk_t.k, cosT.k, "qk_o"], [qk_a.k])
                tt("gpsimd", b_, x1, sbn, ALU.mult, [qk_t.k, sinT.k, "qk_o"], [qk_b.k])
                tt("vector", o3[:, :, 32:64], a_, b_, ALU.add, [qk_a.k, qk_b.k], ["qk_o"])

            def gates_prep(psg_ap, psgk):
                tt("vector", gt[:], psg_ap, bgB[:], ALU.add, [psgk, bgB.k], [gt.k])
                g3 = gt[:].rearrange("p (a b) -> p a b", a=2)
                act(l1[:].rearrange("p (a b) -> p a b", a=2), g3[:, :, 4:8], AF.Exp, [gt.k], [l1.k], scale=-1.0)
                act(l1[:], l1[:], AF.Ln, [l1.k], [l1.k], bias=1.0)
                cp("gpsimd", g8[:].rearrange("p (a b) -> p a b", a=2), g3[:, :, 0:4], [gt.k], [g8.k])

            def chunk_weights(dirs, need_tot):
                p = nextps()
                if "f" in dirs:
                    mm(p[:, 0:4], trif[:], l1[:, 0:4], True, True, [trif.k, l1.k], [p.k])
                if "b" in dirs:
                    mm(p[:, 4:8], trib[:], l1[:, 4:8], True, True, [trib.k, l1.k], [p.k])
                c0 = 0 if need_tot == "f" else 4
                mm(p[:, 8:12], nones[:], l1[:, c0:c0 + 4], True, True, [nones.k, l1.k], [p.k])
                lo, hi = (0, 8) if len(dirs) == 2 else ((0, 4) if "f" in dirs else (4, 8))
                tt("vector", ws8[:, lo:hi], g8[:, lo:hi], p[:, lo:hi], ALU.subtract, [g8.k, p.k], [ws8.k])
                act(ws8[:, lo:hi], ws8[:, lo:hi], AF.Exp, [ws8.k], [ws8.k])
                act(E8[:, lo:hi], p[:, lo:hi], AF.Exp, [p.k], [E8.k])
                act(dec4[:], p[:, 8:12], AF.Exp, [p.k], [dec4.k])

            def make_vw(d):
                c0 = 0 if d == "f" else 4
                tt("vector", Vw[d][:, :, 0:128], mvs[:].rearrange("p (h d) -> p h d", h=4),
                   ws8[:, c0:c0 + 4].unsqueeze(2).to_broadcast([128, 4, 128]), ALU.mult, [mvs.k, ws8.k], [Vw[d].k])
                cp("gpsimd", Vw[d][:, :, 128:129], ws8[:, c0:c0 + 4].unsqueeze(2), [ws8.k], [Vw[d].k])

            def state_update(d, j, cd_out_ap, cd_key):
                St = Sst[d]
                decb = dec4[:].unsqueeze(2).to_broadcast([128, 4, 130])
                tt("vector", Sdec[:], St[:], decb, ALU.mult, [St.k, dec4.k], [Sdec.k])
                ts("vector", cd_out_ap, Sdec[:], QS, None, ALU.mult, None, [Sdec.k], [cd_key])
                pa = nextps(); pb = nextps()
                for h in range(4):
                    pp = pa if h < 2 else pb
                    o = pp[:, 0:260].rearrange("p (a b) -> p a b", a=2)[:, h % 2, 0:129]
                    mm(o, Kt[:, j, h * 128:(h + 1) * 128], Vw[d][:, h, 0:129], True, True, [Kt.k, Vw[d].k], [pp.k])
                tt("vector", St[:, 0:2, 0:129], Sdec[:, 0:2, 0:129], pa[:, 0:260].rearrange("p (a b) -> p a b", a=2)[:, :, 0:129],
                   ALU.add, [Sdec.k, pa.k], [St.k])
                tt("vector", St[:, 2:4, 0:129], Sdec[:, 2:4, 0:129], pb[:, 0:260].rearrange("p (a b) -> p a b", a=2)[:, :, 0:129],
                   ALU.add, [Sdec.k, pb.k], [St.k])

            def p1_tile(j, kv_col, v_idx, rope_n, dirs_states, cdb_idx):
                pX = nextps(); pY = nextps()
                for kc in range(8):
                    mm(pX[:], hT[:, kc, j * 128:(j + 1) * 128], win[:, kc, C_AK:C_AK + 512], kc == 0, kc == 7, [hT.k, win.k], [pX.k])
                for kc in range(8):
                    mm(pY[:, 0:272], hT[:, kc, j * 128:(j + 1) * 128], win[:, kc, C_AK + 512:C_AK + 784], kc == 0, kc == 7,
                       [hT.k, win.k], [pY.k])
                qk_norm_rope(pX[:, 0:128], pX.k, 2, knB, rope_n, qk_o[:, 0:128])
                tr(PT[:, 0:128], qk_o[:, 0:128], identb[:], ["qk_o", identb.k], [PT.k])
                cp("scalar", KT[:, kv_col:kv_col + 128], PT[:, 0:128], [PT.k], [KT.k])
                cp("scalar", V[:, v_idx, :, 0:64], pX[:, 128:256].rearrange("p (a b) -> p a b", a=2), [pX.k], [V.k])
                cp("scalar", mvs[:, 0:256], pX[:, 256:512], [pX.k], [mvs.k])
                cp("scalar", mvs[:, 256:512], pY[:, 0:256], [pY.k], [mvs.k])
                gates_prep(pY[:, 256:272], pY.k)
                for d in dirs_states:
                    chunk_weights([d], d)
                    make_vw(d)
                    if d == "b" and cdb_idx is not None:
                        cs = rr("cds", CdS)
                        state_update(d, j, cs[:], cs.k)
                        dma("sync", cdb_d[cdb_idx], cs[:], [cs.k], [("cdb", cdb_idx)], cs.k)
                    else:
                        state_update(d, j, CdF[:], CdF.k)

            def p1_group_ctx(b):
                stage_a(lambda j: ctx_d[b * CTX + j * 128: b * CTX + (j + 1) * 128, :], 2, A1, B1)
                feat_proj(C_MK, 4, 256, mkT, 4, None, None)
                k_tokmajor(2)
                for j in (0, 1):
                    p1_tile(j, SEQ + j * 128, NT + j, None, ["f"], None)
                for j in (1, 0):
                    p1_tile(j, SEQ + j * 128, NT + j, None, ["b"], None)

            def gen_p1_proj(j, pX, pY, hb_):
                for kc in range(8):
                    mm(pX[:], hb_[:, kc, j * 128:(j + 1) * 128], win[:, kc, C_AK:C_AK + 512], kc == 0, kc == 7, [hb_.k, win.k], [pX.k])
                yield
                for kc in range(8):
                    mm(pY[:, 0:272], hb_[:, kc, j * 128:(j + 1) * 128], win[:, kc, C_AK + 512:C_AK + 784], kc == 0, kc == 7,
                       [hb_.k, win.k], [pY.k])
                yield

            def gen_p1_k(j, pX, kv_col, v_idx, rope_n):
                cp("scalar", V[:, v_idx, :, 0:64], pX[:, 128:256].rearrange("p (a b) -> p a b", a=2), [pX.k], [V.k[1 + v_idx]])
                yield
                qk_norm_rope(pX[:, 0:128], pX.k, 2, knB, rope_n, qk_o[:, 0:128])
                yield
                tr(PT[:, 0:128], qk_o[:, 0:128], identb[:], ["qk_o", identb.k], [PT.k])
                cp("scalar", KT[:, kv_col:kv_col + 128], PT[:, 0:128], [PT.k], [KT.k[1 + v_idx]])
                yield

            def gen_p1_s(j, pX, pY, d, cdb_idx):
                cp("scalar", mvs[:, 0:256], pX[:, 256:512], [pX.k], [mvs.k])
                cp("scalar", mvs[:, 256:512], pY[:, 0:256], [pY.k], [mvs.k])
                gates_prep(pY[:, 256:272], pY.k)
                yield
                chunk_weights([d], d)
                yield
                make_vw(d)
                yield
                cs = rr("cds", CdS)
                state_update(d, j, cs[:], cs.k)
                dma("sync", cdb_d[cdb_idx], cs[:], [cs.k], [("cdb", cdb_idx)], cs.k)
                yield

            def xrows(b, g):
                return lambda j: x_d[b * SEQ + g * 512 + j * 128: b * SEQ + g * 512 + (j + 1) * 128, :]

            def p1_group(b, g, gnext):
                hb_ = HB[g % 2]
                feat_proj(C_MK, 4, 512, mkT, 4, (NG + g - 1) if g > 0 else None, (g + 1) if g < NG - 1 else None, hb_)
                k_tokmajor(4)
                al_S = RR(PB[4:7])
                pairs = [(PB[0], PB[1]), (PB[2], PB[3])]
                js = (3, 2, 1, 0)
                bg = []
                if gnext is not None:
                    bg = [gen_stage_tile(xrows(b, gnext), i_, A1, B1, HB[gnext % 2]) for i_ in range(4)]
                run([(gen_p1_proj(js[0], pairs[0][0], pairs[0][1], hb_), al_all)])
                for i, j in enumerate(js):
                    n = g * 4 + j
                    pX, pY = pairs[i % 2]
                    gens = [(gen_p1_k(j, pX, n * 128, n, n), al_S), (gen_p1_s(j, pX, pY, "b", n), al_S)]
                    if i + 1 < len(js):
                        gens.append((gen_p1_proj(js[i + 1], pairs[(i + 1) % 2][0], pairs[(i + 1) % 2][1], hb_), al_all))
                    run(gens, bg)
                drain(bg)

            def p0(b):
                hb = rr("hx", hx)
                xb = rr("xt", xt)
                xv = x_d[b * SEQ:(b + 1) * SEQ, :].rearrange("(g t) d -> t g d", t=512)
                dma("sync", xb[0:NG, :], xv[0], [], [xb.k], xb.k)
                dma("sync", xb[NG:2 * NG, :], xv[511], [], [xb.k], xb.k)
                s4 = rr("st", st4)
                act(hb[0:2 * NG, :], xb[0:2 * NG, :], AF.Square, [xb.k], [hb.k, s4.k], accum=s4[0:2 * NG, 0:1])
                act(s4[0:2 * NG, 1:2], s4[0:2 * NG, 0:1], AF.Ln, [s4.k], [s4.k], bias=EPS, scale=1.0 / D)
                act(s4[0:2 * NG, 2:3], s4[0:2 * NG, 1:2], AF.Exp, [s4.k], [s4.k], scale=-0.5)
                stt(t1[0:2 * NG, :], xb[0:2 * NG, :], s4[0:2 * NG, 2:3], A1[0:2 * NG, :], ALU.mult, ALU.mult, [xb.k, s4.k, A1.k], [t1.k])
                tt("gpsimd", hb[0:2 * NG, :], t1[0:2 * NG, :], B1[0:2 * NG, :], ALU.add, [t1.k, B1.k], [hb.k])
                for kc in range(8):
                    tr(PT[:, kc * 128:kc * 128 + 2 * NG], hb[0:2 * NG, kc * 128:(kc + 1) * 128], identb[0:2 * NG, 0:2 * NG], [hb.k, identb.k], [PT.k])
                cp("scalar", hT[:, :, 0:2 * NG], PT[:].rearrange("p (a b) -> p a b", a=8)[:, :, 0:2 * NG], [PT.k], [hT.k])
                p = nextps()
                for cc in range(8):
                    for kc in range(8):
                        mm(p[:, cc * 2 * NG:(cc + 1) * 2 * NG], win[:, kc, C_MQ + cc * 128:C_MQ + (cc + 1) * 128], hT[:, kc, 0:2 * NG],
                           kc == 0, kc == 7, [win.k, hT.k], [p.k])
                cp("vector", HS[:], p[:, 0:16 * NG].rearrange("p (a b) -> p a b", a=8), [p.k], [HS.k])

            def gen_attention(n):
                blocks = []
                if n > 0:
                    blocks.append((n - 1) * 128)
                blocks.append(n * 128)
                if n < NT - 1:
                    blocks.append((n + 1) * 128)
                blocks += [SEQ, SEQ + 128]
                for kv in range(2):
                    pr = slice(64 * kv, 64 * kv + 64)
                    for bi, col in enumerate(blocks):
                        p = nextps()
                        mm(p[:], KT[pr, col:col + 128], QT[pr, :, :], True, True, [KT.k, QT.k], [p.k])
                        act(PTs[:, bi, :], p[:], AF.Exp, [p.k], [PTs.k], scale=0.125)
                        if col == (n - 1) * 128:
                            m = amlo
                        elif col == (n + 1) * 128 and col < SEQ:
                            m = amhi
                        else:
                            m = None
                        if m is not None:
                            tt("gpsimd", PTs[:, bi, :].rearrange("p (g q) -> p g q", g=4),
                               PTs[:, bi, :].rearrange("p (g q) -> p g q", g=4),
                               m[:, :].unsqueeze(1).to_broadcast([128, 4, 128]), ALU.mult, [PTs.k, m.k], [PTs.k])
                        yield
                    po = nextps()
                    for gi in range(4):
                        for bi, col in enumerate(blocks):
                            vi = col // 128
                            mm(po[:, gi * 65:(gi + 1) * 65], PTs[:, bi, gi * 128:(gi + 1) * 128], V[:, vi, kv, :],
                               bi == 0, bi == len(blocks) - 1, [PTs.k, V.k], [po.k])
                        yield
                    po3 = po[:, 0:260].rearrange("p (g c) -> p g c", g=4)
                    tt("vector", arec[:], po3[:, :, 64], esink[:, kv * 4:kv * 4 + 4], ALU.add, [po.k, esink.k], [arec.k])
                    recip(arec[:], arec[:], [arec.k], [arec.k])
                    tt("vector", mixr[n % 2][:, kv * 256:(kv + 1) * 256].rearrange("p (g c) -> p g c", g=4), po3[:, :, 0:64],
                       arec[:].unsqueeze(2).to_broadcast([128, 4, 64]), ALU.mult, [po.k, arec.k], ["mixA%d" % (n % 2)])
                    yield

            def gen_head(b, g, j):
                hb_ = HB[g % 2]
                n = g * 4 + j
                tsl = slice(j * 128, (j + 1) * 128)
                pq = nextps(); po_ = nextps(); pv = nextps(); pg = nextps()
                for (pp, c0, w) in ((pq, C_AQ, 512), (pv, C_MV, 512), (pg, C_GT, 16), (po_, C_MO, 512)):
                    for kc in range(8):
                        mm(pp[:, 0:w], hb_[:, kc, tsl], win[:, kc, c0:c0 + w], kc == 0, kc == 7, tile_keys(hb_, j) + [win.k], [pp.k])
                    yield
                qk_norm_rope(pq[:], pq.k, 8, qnB, n, qk_o[:, 0:512])
                yield
                for pi in range(4):
                    tr(PT[:, pi * 128:(pi + 1) * 128], qk_o[:, pi * 128:(pi + 1) * 128], identb[:], ["qk_o", identb.k], [PT.k])
                cp("scalar", QT[:], PT[:, 0:512].rearrange("p (a b) -> p a b", a=4), [PT.k], [QT.k])
                yield
                cp("scalar", mvs[:], pv[:], [pv.k], [mvs.k])
                gates_prep(pg[:, 0:16], pg.k)
                yield
                act(sig[:], po_[:], AF.Exp, [po_.k], [sig.k], scale=-1.0)
                act(sig[:], sig[:], AF.Ln, [sig.k], [sig.k], bias=1.0)
                act(sig[:], sig[:], AF.Exp, [sig.k], [sig.k], scale=-1.0)
                tt("gpsimd", sig[:], sig[:], mnB[:], ALU.mult, [sig.k, mnB.k], [sig.k])
                yield

            def gen_mlstm(b, g, j):
                n = g * 4 + j
                tsl = slice(j * 128, (j + 1) * 128)
                pS = nextps()
                for h in range(4):
                    mm(pS[:, h * 128:(h + 1) * 128], mkT[:, h, tsl], mqT[:, h, tsl], True, True, [mkT.k, mqT.k], [pS.k])
                chunk_weights(["f", "b"], "f")
                yield
                pS3 = pS[:].rearrange("p (h t) -> p h t", h=4)
                tt("vector", Sm["f"][:], pS3, mskf[:, :].unsqueeze(1).to_broadcast([128, 4, 128]), ALU.mult, [pS.k, mskf.k], [Sm["f"].k])
                tt("vector", Sm["b"][:], pS3, mskb[:, :].unsqueeze(1).to_broadcast([128, 4, 128]), ALU.mult, [pS.k, mskb.k], [Sm["b"].k])
                make_vw("f"); make_vw("b")
                yield
                state_update("f", j, CdF[:], CdF.k)
                yield
                cl = rr("cdl", CdL)
                dma("sync", cl[:], cdb_d[n], [("cdb", n)], [cl.k], cl.k)
                for di, d in enumerate(("f", "b")):
                    pa = nextps(); pb = nextps()
                    for h in range(4):
                        pp = pa if h < 2 else pb
                        o = pp[:, 0:260].rearrange("p (a b) -> p a b", a=2)[:, h % 2, 0:129]
                        mm(o, Sm[d][:, h, :], Vw[d][:, h, 0:129], True, False, [Sm[d].k, Vw[d].k], [pp.k])
                        if d == "f":
                            mm(o, mqT[:, h, tsl], CdF[:, h, 0:129], False, True, [mqT.k, CdF.k], [pp.k])
                        else:
                            mm(o, mqT[:, h, tsl], cl[:, h, 0:129], False, True, [mqT.k, cl.k], [pp.k])
                    yield
                    c4 = di * 4
                    for half, pp in enumerate((pa, pb)):
                        c0 = c4 + half * 2
                        den = pp[:, 0:260].rearrange("p (a b) -> p a b", a=2)[:, :, 128]
                        tt("vector", sc8[:, c0:c0 + 2], den, E8[:, c0:c0 + 2], ALU.mult, [pp.k, E8.k], [sc8.k])
                    ts("vector", sc8n[:, c4:c4 + 4], sc8[:, c4:c4 + 4], -1.0, None, ALU.mult, None, [sc8.k], [sc8n.k])
                    tt("vector", sc8[:, c4:c4 + 4], sc8[:, c4:c4 + 4], sc8n[:, c4:c4 + 4], ALU.max, [sc8.k, sc8n.k], [sc8.k])
                    ts("vector", sc8[:, c4:c4 + 4], sc8[:, c4:c4 + 4], 1.0, None, ALU.max, None, [sc8.k], [sc8.k])
                    recip(sc8[:, c4:c4 + 4], sc8[:, c4:c4 + 4], [sc8.k], [sc8.k])
                    tt("vector", sc8[:, c4:c4 + 4], sc8[:, c4:c4 + 4], E8[:, c4:c4 + 4], ALU.mult, [sc8.k, E8.k], [sc8.k])
                    yield
                    for h in range(4):
                        pp = pa if h < 2 else pb
                        src = pp[:, 0:260].rearrange("p (a b) -> p a b", a=2)[:, h % 2, 0:128]
                        if d == "f":
                            ts("vector", hs[:, h * 128:(h + 1) * 128], src, sc8[:, h:h + 1], None, ALU.mult, None, [pp.k, sc8.k], [hs.k])
                        else:
                            stt(hs[:, h * 128:(h + 1) * 128], src, sc8[:, 4 + h:5 + h], hs[:, h * 128:(h + 1) * 128], ALU.mult, ALU.add,
                                [pp.k, sc8.k, hs.k], [hs.k])
                    yield
                for h in range(4):
                    act(Sm["f"][:, h, :], hs[:, h * 128:(h + 1) * 128], AF.Square, [hs.k], [Sm["f"].k, hrs.k], accum=hrs[:, h:h + 1])
                act(hrs[:], hrs[:], AF.Ln, [hrs.k], [hrs.k], bias=EPS, scale=1.0 / 128)
                act(hrs[:], hrs[:], AF.Exp, [hrs.k], [hrs.k], scale=-0.5)
                yield
                tt("vector", hs[:].rearrange("p (h d) -> p h d", h=4), hs[:].rearrange("p (h d) -> p h d", h=4),
                   hrs[:].unsqueeze(2).to_broadcast([128, 4, 128]), ALU.mult, [hs.k, hrs.k], [hs.k])
                tt("vector", mixr[n % 2][:, 512:1024], hs[:], sig[:], ALU.mult, [hs.k, sig.k], ["mixM%d" % (n % 2)])
                yield

            def gen_tail(b, g, j):
                n = g * 4 + j
                row0 = b * SEQ + n * 128
                for kc in range(8):
                    tr(PT[:, kc * 128:(kc + 1) * 128], mixr[n % 2][:, kc * 128:(kc + 1) * 128], identb[:],
                       ["mixA%d" % (n % 2), "mixM%d" % (n % 2), identb.k], [PT.k])
                cp("scalar", mixT[:], PT[:].rearrange("p (a b) -> p a b", a=8), [PT.k], [mixT.k])
                xb = rr("xt", xt)
                dma("sync", xb[:], x_d[row0:row0 + 128, :], [], [xb.k], xb.k)
                xo = rr("xn", xn)
                yield
                for half in range(2):
                    p = nextps()
                    for kc in range(8):
                        mm(p[:], mixT[:, kc, :], wout[:, kc, half * 512:(half + 1) * 512], kc == 0, kc == 7, [mixT.k, wout.k], [p.k])
                    hsl = slice(half * 512, (half + 1) * 512)
                    tt("vector", xo[:, hsl], p[:], xb[:, hsl], ALU.add, [p.k, xb.k], [xo.k])
                    yield
                dma("sync", out_d[row0:row0 + 128, :], xo[:], [xo.k], ["outrows"], xo.k)
                yield
                s4 = rr("st", st4)
                hb2 = rr("h2b", h2b)
                act(hb2[:], xo[:], AF.Square, [xo.k], [hb2.k, s4.k], accum=s4[:, 0:1])
                act(s4[:, 1:2], s4[:, 0:1], AF.Ln, [s4.k], [s4.k], bias=EPS, scale=1.0 / D)
                act(s4[:, 2:3], s4[:, 1:2], AF.Exp, [s4.k], [s4.k], scale=-0.5)
                yield
                stt(t1[:], xo[:], s4[:, 2:3], A2[:], ALU.mult, ALU.mult, [xo.k, s4.k, A2.k], [t1.k])
                tt("vector", t1[:], t1[:], B2[:], ALU.add, [t1.k, B2.k], [t1.k])
                yield
                cp("scalar", hb2[:], h2[:], [h2.k], [hb2.k])
                dma("sync", hx2s_d[row0:row0 + 128, :], hb2[:], [hb2.k], ["hx2s"], hb2.k)
                for half in range(2):
                    p = nextps()
                    for q4 in range(4):
                        kc = half * 4 + q4
                        tr(p[:, q4 * 128:(q4 + 1) * 128], h2[:, kc * 128:(kc + 1) * 128], identf[:], [h2.k, identf.k], [p.k])
                    cp("scalar", h2T[:, half * 4:half * 4 + 4, :], p[:].rearrange("p (a b) -> p a b", a=4), [p.k], [h2T.k])
                    yield
                p = nextps()
                for kc in range(8):
                    mm(p[:, 0:16], h2T[:, kc, :], wr[:, kc, :], kc == 0, kc == 7, [h2T.k, wr.k], [p.k])
                S.op("vector", lambda e, p=p: e.tensor_reduce(out=rs1[:], in_=p[:, 0:16], axis=AX.X, op=ALU.max, negate=True),
                     reads=[p.k], writes=[rs1.k])
                act(rt[:], p[:, 0:16], AF.Exp, [p.k, rs1.k], [rt.k, rs2.k], bias=rs1[:, 0:1], accum=rs2[:, 0:1])
                yield
                recip(rs2[:], rs2[:], [rs2.k], [rs2.k])
                ts("vector", aff_all[:, n % (NT // 2), n // (NT // 2), b, :], rt[:], rs2[:, 0:1], None, ALU.mult, None, [rt.k, rs2.k], [(aff_all.k, n, b)])
                yield

            def p2_prep2(b, g):
                hb_ = HB[g % 2]
                hp = (NG + g - 1) if g > 0 else None
                hn = (g + 1) if g < NG - 1 else None
                feat_proj(C_MQ, 4, 512, mqT, 0, hp, hn, hb_)
                feat_proj(C_MK, 4, 512, mkT, 4, hp, hn, hb_)
                k_tokmajor(4)

            def p2_batch(b, ngroups):
                ntile = ngroups * 4
                if CFG.get("psv", 1) == 1:
                    al_A = RR(PB[0:2]); al_M = RR(PB[3:6]); al_T = RR([PB[2], PB[6]]); al_H = RR(PB[0:4])
                else:
                    al_A = RR(PB[0:3]); al_M = RR(PB[3:6]); al_T = RR(PB[6:7]); al_H = RR(PB[0:4])
                stage_a(xrows(b, 0), 4, A1, B1, HB[0])
                p2_prep2(b, 0)
                run([(gen_head(b, 0, 0), al_H)])
                for n in range(ntile):
                    g, j = n // 4, n % 4
                    gens = [(gen_mlstm(b, g, j), al_M, CFG.get("mw", 2)), (gen_attention(n), al_A)]
                    if n > 0:
                        gens.append((gen_tail(b, (n - 1) // 4, (n - 1) % 4), al_T))
                    if j == 0:
                        bg = []
                        if g + 1 < ngroups:
                            bg = [gen_stage_tile(xrows(b, g + 1), j_, A1, B1, HB[(g + 1) % 2]) for j_ in range(4)]
                    run(gens, bg)
                    if n + 1 < ntile:
                        if j == 3:
                            drain(bg)
                            p2_prep2(b, g + 1)
                        run([(gen_head(b, (n + 1) // 4, (n + 1) % 4), al_H)], bg)
                run([(gen_tail(b, (ntile - 1) // 4, (ntile - 1) % 4), al_T)])

            stop = CFG["stop"]
            for b in range(CFG["nb"]):
                if CFG.get("lm", 3) & 1:
                    load_mod(2, {"B1": B1} if CFG.get("lm", 3) & 4 else {"A1": A1, "B1": B1})
                if CFG.get("lm", 3) & 2:
                    mset("vector", Sst["f"][:], 0.0, [Sst["f"].k])
                    mset("vector", Sst["b"][:], 0.0, [Sst["b"].k])
                if stop == "mod":
                    continue
                p1_group_ctx(b)
                load_mod(b, {"A1": A1, "B1": B1, "A2": A2, "B2": B2})
                load_mod(b, {"G1": None})
                load_wout_scaled()
                if stop == "ctx":
                    continue
                p0(b)
                if stop == "p0":
                    continue
                g1s = list(range(NG - 1, -1, -1))
                if CFG["ng1"] is not None:
                    g1s = g1s[:CFG["ng1"]]
                stage_a(xrows(b, g1s[0]), 4, A1, B1, HB[g1s[0] % 2])
                for gi_, g in enumerate(g1s):
                    p1_group(b, g, g1s[gi_ + 1] if gi_ + 1 < len(g1s) else None)
                if stop == "p1":
                    continue
                p2_batch(b, NG if CFG["ng2"] is None else CFG["ng2"])
            S.emit()
        if CFG["stop"] in ("mod", "ctx", "p0", "p1", "p2"):
            return nc

        with ExitStack() as es:
            S = Sched(nc, "b")

            def sb(name, shape, dt=F32):
                return Buf(es.enter_context(nc.sbuf_tensor("z_" + name, list(shape), dt)), "z_" + name)

            def ps(name, shape, dt=F32):
                return Buf(es.enter_context(nc.psum_tensor(name, list(shape), dt)), name)

            PB = [ps("qb%d" % i, [128, 512]) for i in range(6)]
            PTS = [ps("qtb%d" % i, [128, 1024], BF16) for i in range(2)]
            pbi = [0]

            def nextps():
                p = PB[pbi[0] % 6]
                pbi[0] += 1
                return p

            G2 = [sb("G2_%d" % b, [128, D]) for b in range(NB)]
            for b in range(NB):
                S.op("sync", lambda e, b=b: e.dma_start(out=G2[b][:], in_=modsc_d[b, 5 * D:6 * D].partition_broadcast(128)),
                     writes=[G2[b].k], dma="prm2")
            S.group_keys.add("prm2")
            HSEQ = SEQ // 2
            affT = sb("affT", [64, HSEQ])
            mx = sb("mx", [64, CAP])
            ix = sb("ix", [64, CAP], U32)
            ixf = sb("ixf", [64, CAP])
            boff = sb("boff", [64, 1])
            antiI = sb("antiI", [128, 128])
            S.op("sync", lambda e: e.dma_start(out=boff[:], in_=boff_d), writes=[boff.k], dma="prm2")
            S.op("sync", lambda e: e.dma_start(out=antiI[:], in_=anti_d), writes=[antiI.k], dma="prm2")
            for m4 in range(4):
                p = nextps()
                for q in range(4):
                    m = m4 * 4 + q
                    S.op("tensor", lambda e, p=p, q=q, m=m: e.transpose(p[0:64, q * 128:(q + 1) * 128],
                                                                          aff_all[:, m, :, :, :].rearrange("p h b e -> p (h b e)"), identf[:]),
                         reads=[], writes=[p.k])
                S.op("vector", lambda e, p=p, m4=m4: e.tensor_copy(out=affT[:, m4 * 512:(m4 + 1) * 512], in_=p[0:64, :]),
                     reads=[p.k], writes=[affT.k])
            for k in range(CAP // 8):
                sl = slice(8 * k, 8 * k + 8)
                S.op("vector", lambda e, sl=sl: e.max(out=mx[:, sl], in_=affT[:]), reads=[affT.k], writes=[mx.k])
                S.op("vector", lambda e, sl=sl: e.max_index(out=ix[:, sl], in_max=mx[:, sl], in_values=affT[:]),
                     reads=[affT.k, mx.k], writes=[ix.k])
                if k < CAP // 8 - 1:
                    S.op("vector", lambda e, sl=sl: e.match_replace(out=affT[:], in_to_replace=mx[:, sl], in_values=affT[:], imm_value=-1.0),
                         reads=[affT.k, mx.k], writes=[affT.k])
            S.op("vector", lambda e: e.tensor_copy(out=ixf[:], in_=ix[:]), reads=[ix.k], writes=[ixf.k])
            S.op("vector", lambda e: e.tensor_scalar(out=ixf[:], in0=ixf[:], scalar1=boff[:, 0:1], scalar2=None, op0=ALU.add),
                 reads=[ixf.k, boff.k], writes=[ixf.k])
            idxT = sb("idxT", [128, 4, 32], I32)
            gateT = sb("gateT", [128, 4, 32])
            Tmx = sb("Tmx", [128, 4, 64])
            Tix = sb("Tix", [128, 4, 64])
            msk = sb("msk", [128, 4, 32])
            dif = sb("dif", [128, 4, 32])
            for (src, dst) in ((mx, Tmx), (ixf, Tix)):
                p = nextps()
                for q in range(4):
                    S.op("tensor", lambda e, p=p, q=q, src=src: e.transpose(p[:, q * 64:(q + 1) * 64], src[:, q * 128:(q + 1) * 128], identf[0:64, 0:64]),
                         reads=[src.k], writes=[p.k])
                S.op("vector", lambda e, p=p, dst=dst: e.tensor_copy(out=dst[:], in_=p[:, 0:256].rearrange("p (a b) -> p a b", a=4)),
                     reads=[p.k], writes=[dst.k])
            pB = nextps()
            for q in range(4):
                S.op("tensor", lambda e, q=q: e.matmul(pB[:, q * 64:q * 64 + 32], antiI[:], Tmx[:, 3 - q, 32:64], start=True, stop=True),
                     reads=[antiI.k, Tmx.k], writes=[pB.k])
                S.op("tensor", lambda e, q=q: e.matmul(pB[:, q * 64 + 32:q * 64 + 64], antiI[:], Tix[:, 3 - q, 32:64], start=True, stop=True),
                     reads=[antiI.k, Tix.k], writes=[pB.k])
            pB3 = pB[:, 0:256].rearrange("p (a b) -> p a b", a=4)
            S.op("vector", lambda e: e.tensor_tensor(out=msk[:], in0=Tmx[:, :, 0:32], in1=pB3[:, :, 0:32], op=ALU.is_ge),
                 reads=[Tmx.k, pB.k], writes=[msk.k])
            S.op("vector", lambda e: e.tensor_tensor(out=gateT[:], in0=Tmx[:, :, 0:32], in1=pB3[:, :, 0:32], op=ALU.max),
                 reads=[Tmx.k, pB.k], writes=[gateT.k])
            S.op("vector", lambda e: e.tensor_tensor(out=dif[:], in0=Tix[:, :, 0:32], in1=pB3[:, :, 32:64], op=ALU.subtract),
                 reads=[Tix.k, pB.k], writes=[dif.k])
            S.op("vector", lambda e: e.tensor_tensor(out=dif[:], in0=dif[:], in1=msk[:], op=ALU.mult),
                 reads=[dif.k, msk.k], writes=[dif.k])
            S.op("vector", lambda e: e.tensor_tensor(out=dif[:], in0=dif[:], in1=pB3[:, :, 32:64], op=ALU.add),
                 reads=[dif.k, pB.k], writes=[dif.k])
            S.op("vector", lambda e: e.tensor_copy(out=idxT[:], in_=dif[:]), reads=[dif.k], writes=[idxT.k])

            wgb = [sb("wg%d" % i, [128, 8, 512], BF16) for i in range(3)]
            wub = [sb("wu%d" % i, [128, 8, 512], BF16) for i in range(3)]
            wdb = [sb("wd%d" % i, [128, 4, D], BF16) for i in range(3)]
            xg = [sb("xg%d" % i, [128, D], BF16) for i in range(12)]
            xgTs = [sb("xgT%d" % i, [128, 8, 512], BF16) for i in range(2)]
            sgs = [sb("sg%d" % i, [128, 512]) for i in range(2)]
            hTe = sb("hTe", [128, 4, 512], BF16)
            ptc = [0]
            ys = [sb("ys%d" % i, [128, D]) for i in range(4)]

            stg = [sb("stg%d" % i, [128, 512]) for i in range(6)]
            stc = [0]

            def w_chunks(ex):
                wg_, wu_, wd_ = wgb[ex % 3], wub[ex % 3], wdb[ex % 3]
                out = []
                for kc in range(8):
                    out.append((wg_[:, kc, :], wg_.k, wg_d[ex, kc * 128:(kc + 1) * 128, :]))
                    out.append((wu_[:, kc, :], wu_.k, wu_d[ex, kc * 128:(kc + 1) * 128, :]))
                for fc in range(4):
                    for c0 in (0, 512):
                        out.append((wd_[:, fc, c0:c0 + 512], wd_.k, wd_d[ex, fc * 128:(fc + 1) * 128, c0:c0 + 512]))
                return out

            def load_chunk(ch):
                dst, dkey, src = ch
                st = stg[stc[0] % 6]
                eng = "scalar" if stc[0] % 2 == 0 else "gpsimd"
                stc[0] += 1
                S.op("sync", lambda e, st=st, src=src: e.dma_start(out=st[:], in_=src), writes=[st.k], dma=st.k)
                if eng == "scalar":
                    S.op("scalar", lambda e, st=st, dst=dst: e.copy(out=dst, in_=st[:]), reads=[st.k], writes=[dkey])
                else:
                    S.op("gpsimd", lambda e, st=st, dst=dst: e.tensor_copy(out=dst, in_=st[:]), reads=[st.k], writes=[dkey])

            def load_w(ex):
                for ch in w_chunks(ex):
                    load_chunk(ch)

            wstream = []
            for ex_ in range(2, NEXP):
                wstream.extend(w_chunks(ex_))

            def gen_wload(i):
                lo = 12 * (i - 1)
                for ch in wstream[lo:lo + 12] if i >= 1 else []:
                    load_chunk(ch)
                    yield

            items = [(ex, b) for ex in range(NEXP) for b in range(NB)]

            def gathers(i):
                ex, b = items[i]
                pe = b * 16 + ex
                for j in range(4):
                    xb = xg[(i % 3) * 4 + j]
                    S.op("gpsimd", lambda e, j=j, pe=pe, xb=xb: e.indirect_dma_start(
                        out=xb[:], out_offset=None, in_=hx2s_d,
                        in_offset=bass.IndirectOffsetOnAxis(ap=idxT[:, j, pe:pe + 1], axis=0)),
                        reads=[idxT.k], writes=[xb.k], dma=xb.k)

            def gen_trans(i):
                xgT = xgTs[i % 2]
                for j in range(4):
                    xb = xg[(i % 3) * 4 + j]
                    PT = PTS[ptc[0] % 2]
                    ptc[0] += 1
                    for kc in range(8):
                        S.op("tensor", lambda e, xb=xb, kc=kc, PT=PT: e.transpose(PT[:, kc * 128:(kc + 1) * 128], xb[:, kc * 128:(kc + 1) * 128], identb[:]),
                             reads=[xb.k], writes=[PT.k])
                    if j % 2 == 0:
                        S.op("scalar", lambda e, j=j, PT=PT, xgT=xgT: e.copy(out=xgT[:, :, j * 128:(j + 1) * 128], in_=PT[:].rearrange("p (a b) -> p a b", a=8)),
                             reads=[PT.k], writes=[xgT.k])
                    else:
                        S.op("vector", lambda e, j=j, PT=PT, xgT=xgT: e.tensor_copy(out=xgT[:, :, j * 128:(j + 1) * 128], in_=PT[:].rearrange("p (a b) -> p a b", a=8)),
                             reads=[PT.k], writes=[xgT.k])
                    yield

            def gen_ffn(i):
                ex, b = items[i]
                pe = b * 16 + ex
                xgT = xgTs[i % 2]
                wg_, wu_, wd_ = wgb[ex % 3], wub[ex % 3], wdb[ex % 3]
                for fc in range(4):
                    pg = nextps(); pu = nextps()
                    sg = sgs[fc % 2]
                    for kc in range(8):
                        S.op("tensor", lambda e, pg=pg, kc=kc, fc=fc: e.matmul(pg[:], wg_[:, kc, fc * 128:(fc + 1) * 128], xgT[:, kc, :],
                                                                             start=(kc == 0), stop=(kc == 7)),
                             reads=[wg_.k, xgT.k], writes=[pg.k])
                    for kc in range(8):
                        S.op("tensor", lambda e, pu=pu, kc=kc, fc=fc: e.matmul(pu[:], wu_[:, kc, fc * 128:(fc + 1) * 128], xgT[:, kc, :],
                                                                             start=(kc == 0), stop=(kc == 7)),
                             reads=[wu_.k, xgT.k], writes=[pu.k])
                    S.op("scalar", lambda e, pg=pg, sg=sg: e.activation(out=sg[:], in_=pg[:], func=AF.Silu), reads=[pg.k], writes=[sg.k])
                    S.op("vector", lambda e, pu=pu, fc=fc, sg=sg: e.tensor_tensor(out=hTe[:, fc, :], in0=pu[:], in1=sg[:], op=ALU.mult),
                         reads=[pu.k, sg.k], writes=[hTe.k])
                    yield
                for j in range(4):
                    for half in range(2):
                        p = nextps()
                        for fc in range(4):
                            S.op("tensor", lambda e, p=p, fc=fc, j=j, half=half: e.matmul(
                                p[:], hTe[:, fc, j * 128:(j + 1) * 128], wd_[:, fc, half * 512:(half + 1) * 512],
                                start=(fc == 0), stop=(fc == 3)), reads=[hTe.k, wd_.k], writes=[p.k])
                        S.op("vector", lambda e, p=p, j=j, half=half: e.scalar_tensor_tensor(
                            out=ys[j][:, half * 512:(half + 1) * 512], in0=p[:], scalar=gateT[:, j, pe:pe + 1],
                            in1=G2[b][:, half * 512:(half + 1) * 512], op0=ALU.mult, op1=ALU.mult),
                            reads=[p.k, gateT.k, G2[b].k], writes=[ys[j].k])
                    S.op("gpsimd", lambda e, j=j: e.indirect_dma_start(
                        out=out_d, out_offset=bass.IndirectOffsetOnAxis(ap=idxT[:, j, pe:pe + 1], axis=0),
                        in_=ys[j][:], in_offset=None, compute_op=ALU.add),
                        reads=[ys[j].k, idxT.k] + ["osc%d_%d_%d" % (b, (ex + 1) % 2, jj) for jj in range(4)],
                        writes=["osc%d_%d_%d" % (b, ex % 2, j)], dma=ys[j].k)
                    yield

            def run2(gens):
                live = list(gens)
                while live:
                    for g_ in list(live):
                        try:
                            next(g_)
                        except StopIteration:
                            live.remove(g_)

            load_w(0)
            load_w(1)
            gathers(0)
            gathers(1)
            run2([gen_trans(0)])
            for i in range(len(items)):
                ex, b = items[i]
                if i + 2 < len(items):
                    gathers(i + 2)
                if i + 1 < len(items):
                    run2([gen_ffn(i), gen_trans(i + 1), gen_wload(i)])
                else:
                    run2([gen_ffn(i), gen_wload(i)])
            S.emit()
    return nc


_CACHE = {}


def _consts():
    import ml_dtypes
    bf = ml_dtypes.bfloat16
    n = SEQ
    rows = n // 64
    row, col = np.meshgrid(np.arange(rows), np.arange(64), indexing="ij")
    n_freq = 16
    freqs = (10000.0 ** (-np.arange(n_freq, dtype=np.float32) / n_freq)).astype(np.float32)
    ang = np.concatenate([row.reshape(-1, 1).astype(np.float32) * freqs, col.reshape(-1, 1).astype(np.float32) * freqs], -1)
    s = np.arange(128)[:, None]
    t = np.arange(128)[None, :]
    c = {
        "rope_cos": np.cos(ang).astype(np.float32),
        "rope_sin": np.sin(ang).astype(np.float32),
        "ident_bf": np.eye(128, dtype=np.float32).astype(bf),
        "ident_f": np.eye(128, dtype=np.float32),
        "tri_f": (s > t).astype(np.float32),
        "tri_b": (s < t).astype(np.float32),
        "neg_ones": -np.ones((128, 128), np.float32),
        "mask_f": ((s <= t) * QS).astype(np.float32).astype(bf),
        "mask_b": ((s >= t) * QS).astype(np.float32).astype(bf),
        "amask_lo": (s >= t).astype(np.float32).astype(bf),
        "amask_hi": (s <= t).astype(np.float32).astype(bf),
        "boff": np.array([(p_ // 32) * (SEQ // 2) + ((p_ % 32) // 16) * SEQ for p_ in range(64)], np.float32).reshape(64, 1),
        "anti_ident": np.ascontiguousarray(np.eye(128, dtype=np.float32)[::-1]),
    }
    return c


def _perm_win(w_in):
    w = w_in
    aq = w[:, 0:512]
    ak = w[:, 512:640]
    av = w[:, 640:768]
    mq = w[:, 768:1280]
    mk = w[:, 1280:1792]
    mv = w[:, 1792:2304]
    mo = w[:, 2304:2816]
    gt = w[:, 2816:2832]
    heads = [aq[:, h * 64:(h + 1) * 64] for h in range(8)]
    aqp = np.concatenate([np.concatenate([heads[p], heads[4 + p]], 1) for p in range(4)], 1)
    return np.ascontiguousarray(np.concatenate([aqp, mo, ak, av, mv, gt, mq, mk], 1))


def kernel(x, c, ctx, c_ctx, w_mod, b_mod, norm1_w, norm2_w, w_in, b_gates, conv_qk, q_norm_w,
           k_norm_w, sink, mlstm_norm_w, w_out, w_router, w_gate, w_up, w_down):
    f = lambda a: np.ascontiguousarray(np.asarray(a, dtype=np.float32))
    x = f(x); c = f(c); ctx = f(ctx); c_ctx = f(c_ctx)
    if "nc" not in _CACHE:
        _CACHE["nc"] = build_program()
    nc = _CACHE["nc"]
    consts = _consts()
    shared = {
        "w_mod": f(w_mod)[0], "b_mod": f(b_mod), "norm1_w": f(norm1_w), "norm2_w": f(norm2_w),
        "w_in": _perm_win(f(w_in)[0]), "b_gates": f(b_gates), "conv_qk": f(conv_qk)[0],
        "q_norm_w": f(q_norm_w), "k_norm_w": f(k_norm_w), "sink": f(sink), "mlstm_norm_w": f(mlstm_norm_w),
        "w_out": f(w_out)[0], "w_router": f(w_router)[0], "w_gate": f(w_gate)[0], "w_up": f(w_up)[0],
        "w_down": f(w_down)[0],
    }
    shared.update(consts)
    if CFG["stop"] is not None:
        for k in ("w_gate", "w_up", "w_down"):
            shared.pop(k)
    in_maps = []
    for i in range(8):
        m = dict(shared)
        m["x"] = x[2 * i:2 * i + 2].reshape(NB * SEQ, D)
        m["ctx"] = ctx[2 * i:2 * i + 2].reshape(NB * CTX, D)
        m["cvec"] = np.ascontiguousarray(np.stack([c[2 * i], c[2 * i + 1], c_ctx], 0))
        in_maps.append(m)
    res = run_bass_kernel_spmd(nc, in_maps, core_ids=list(range(8)))
    _CACHE["last"] = res.results
    out = np.concatenate([np.asarray(r["out"]).reshape(NB, SEQ, D) for r in res.results], 0)
    return out.astype(np.float32)
```

```python
import numpy as np
from contextlib import ExitStack
import concourse.bass as bass
import concourse.mybir as mybir
from concourse.bass_utils import run_bass_kernel_spmd

F32 = mybir.dt.float32
BF16 = mybir.dt.bfloat16
I32 = mybir.dt.int32
U32 = mybir.dt.uint32
AF = mybir.ActivationFunctionType
ALU = mybir.AluOpType
AX = mybir.AxisListType

D = 1024
SEQ = 4096
NT = SEQ // 128
NG = SEQ // 512
CTX = 256
NB = 2
EPS = 1e-6
NEXP = 16
CAP = 512
QS = 128 ** -0.5
C_AQ, C_MO, C_AK, C_AV, C_MV, C_GT, C_MQ, C_MK = 0, 512, 1024, 1152, 1280, 1792, 1808, 2320
SEM_CHUNK = 16000
CFG = {"stop": None, "nb": NB, "ng1": None, "ng2": None}


class Sched:
    ENGS = ("tensor", "vector", "scalar", "gpsimd", "sync")

    def __init__(self, nc, tag):
        self.nc = nc
        self.tag = tag
        self.ops = []
        self.last_writer = {}
        self.readers = {}
        self.eng_count = {e: 0 for e in self.ENGS}
        self.dma_count = {}
        self.sem_names = set()
        self.group_keys = set()

    def _event_compute(self, eng):
        c = self.eng_count[eng]
        self.eng_count[eng] = c + 1
        name = "%sp_%s_%d" % (self.tag, eng, c // SEM_CHUNK)
        self.sem_names.add(name)
        return (name, (c % SEM_CHUNK) + 1, 1)

    def _event_dma(self, key):
        c = self.dma_count.get(key, 0)
        self.dma_count[key] = c + 1
        per = SEM_CHUNK // 16
        name = "%sd_%s_%d" % (self.tag, key, c // per)
        self.sem_names.add(name)
        return (name, ((c % per) + 1) * 16, 16)

    def op(self, eng, fn, reads=(), writes=(), dma=None):
        def expand(keys):
            out = []
            for k in keys:
                if isinstance(k, tuple) and len(k) > 0 and k[0] == "MULTI":
                    out.extend(k[1:])
                else:
                    out.append(k)
            return out
        reads = expand(reads)
        writes = expand(writes)
        waits = {}

        def need(ev):
            if ev is None:
                return
            n, v, _ = ev
            if waits.get(n, 0) < v:
                waits[n] = v

        for r in reads:
            need(self.last_writer.get(r))
        for w in writes:
            need(self.last_writer.get(w))
            for ev in self.readers.get(w, ()):
                need(ev)
        ev = self._event_dma(dma) if dma is not None else self._event_compute(eng)
        for r in reads:
            self.readers.setdefault(r, []).append(ev)
        for w in writes:
            self.last_writer[w] = ev
            self.readers[w] = []
        self.ops.append((eng, fn, waits, ev))

    def emit(self):
        nc = self.nc
        with ExitStack() as es:
            sems = {}
            for n in sorted(self.sem_names):
                sems[n] = es.enter_context(nc.semaphore(n))
            block = es.enter_context(nc.Block())
            final_waits = {}
            for (eng, fn, waits, ev) in self.ops:
                n, v, _ = ev
                if final_waits.get(n, 0) < v:
                    final_waits[n] = v
            gnames = set("%sd_%s_0" % (self.tag, k) for k in self.group_keys) if CFG.get("gk", 1) else set()

            def make(engname):
                def body(e):
                    seen = {}
                    for (eng, fn, waits, ev) in self.ops:
                        if eng != engname:
                            continue
                        for n, v in waits.items():
                            if engname == "tensor" and "p_tensor_" in n:
                                continue
                            if n in gnames:
                                v = final_waits[n]
                            if seen.get(n, 0) >= v:
                                continue
                            e.wait_ge(sems[n], v)
                            seen[n] = v
                        ins = fn(e)
                        ins.then_inc(sems[ev[0]], ev[2])
                    if engname == "sync":
                        for n, v in final_waits.items():
                            if seen.get(n, 0) >= v:
                                continue
                            e.wait_ge(sems[n], v)
                return body

            block.tensor(make("tensor"))
            block.vector(make("vector"))
            block.scalar(make("scalar"))
            block.gpsimd(make("gpsimd"))
            block.sync(make("sync"))


class Buf:
    def __init__(self, t, k):
        self.t = t
        self.k = k

    def __getitem__(self, idx):
        return self.t[idx]


def build_program():
    nc = bass.Bass("TRN2", target_bir_lowering=False)

    def din(name, shape, dt=F32):
        return nc.dram_tensor(name, list(shape), dt, kind="ExternalInput").ap()

    x_d = din("x", [NB * SEQ, D])
    ctx_d = din("ctx", [NB * CTX, D])
    cvec_d = din("cvec", [3, D])
    wmod_d = din("w_mod", [D, 6 * D])
    bmod_d = din("b_mod", [1, 6 * D])
    n1_d = din("norm1_w", [1, D])
    n2_d = din("norm2_w", [1, D])
    win_d = din("w_in", [D, 2832])
    bg_d = din("b_gates", [1, 16])
    conv_d = din("conv_qk", [3, D])
    qn_d = din("q_norm_w", [1, 64])
    kn_d = din("k_norm_w", [1, 64])
    sink_d = din("sink", [1, 8])
    mn_d = din("mlstm_norm_w", [1, 512])
    wout_d = din("w_out", [D, D])
    wr_d = din("w_router", [D, NEXP])
    if CFG["stop"] is None:
        wg_d = din("w_gate", [NEXP, D, 512])
        wu_d = din("w_up", [NEXP, D, 512])
        wd_d = din("w_down", [NEXP, 512, D])
    cos_d = din("rope_cos", [SEQ, 32])
    sin_d = din("rope_sin", [SEQ, 32])
    idb_d = din("ident_bf", [128, 128], BF16)
    idf_d = din("ident_f", [128, 128])
    trif_d = din("tri_f", [128, 128])
    trib_d = din("tri_b", [128, 128])
    nones_d = din("neg_ones", [128, 128])
    mskf_d = din("mask_f", [128, 128], BF16)
    mskb_d = din("mask_b", [128, 128], BF16)
    amlo_d = din("amask_lo", [128, 128], BF16)
    amhi_d = din("amask_hi", [128, 128], BF16)
    boff_d = din("boff", [64, 1])
    anti_d = din("anti_ident", [128, 128])
    out_d = nc.dram_tensor("out", [NB * SEQ, D], F32, kind="ExternalOutput").ap()
    hx2s_d = nc.dram_tensor("hx2s", [NB * SEQ, D], BF16).ap()
    modsc_d = nc.dram_tensor("modsc", [3, 6 * D], F32).ap()
    cdb_d = nc.dram_tensor("cdbs", [NT, 128, 4, 130], BF16).ap()

    with ExitStack() as es0:
        def sb0(name, shape, dt=F32):
            return Buf(es0.enter_context(nc.sbuf_tensor(name, list(shape), dt)), name)

        aff_all = sb0("aff_all", [128, NT // 2, 2, NB, NEXP])
        identb = sb0("identb", [128, 128], BF16)
        identf = sb0("identf", [128, 128])

        with ExitStack() as es:
            S = Sched(nc, "m")

            def sb(name, shape, dt=F32):
                return Buf(es.enter_context(nc.sbuf_tensor(name, list(shape), dt)), name)

            def ps(name, shape, dt=F32):
                return Buf(es.enter_context(nc.psum_tensor(name, list(shape), dt)), name)

            PB = [ps("mb%d" % i, [128, 512]) for i in range(4)]
            pbi = [0]

            def nextps():
                p = PB[pbi[0] % 4]
                pbi[0] += 1
                return p

            def dma(eng, out, in_, r, w, key, **kw):
                S.op(eng, lambda e: e.dma_start(out=out, in_=in_, **kw), reads=r, writes=w, dma=key)

            def mm(out, lhsT, rhs, start, stop, r, w):
                S.op("tensor", lambda e: e.matmul(out, lhsT, rhs, start=start, stop=stop), reads=r, writes=w)

            def tr(out, in_, ident, r, w):
                S.op("tensor", lambda e: e.transpose(out, in_, ident), reads=r, writes=w)

            def tt(eng, out, in0, in1, op, r, w):
                S.op(eng, lambda e: e.tensor_tensor(out=out, in0=in0, in1=in1, op=op), reads=r, writes=w)

            def cp(eng, out, in_, r, w):
                S.op(eng, lambda e: e.tensor_copy(out=out, in_=in_), reads=r, writes=w)

            PK = "prm0"
            S.group_keys.add(PK)

            def load_const(buf, src, eng="sync", **kw):
                dma(eng, buf[:], src, [], [buf.k], PK, **kw)

            load_const(identb, idb_d)
            load_const(identf, idf_d)
            c3 = sb("c3", [3, D]); load_const(c3, cvec_d)
            S.op("scalar", lambda e: e.activation(out=c3[:], in_=c3[:], func=AF.Silu), reads=[c3.k], writes=[c3.k])
            sT = sb("sT", [128, 8, 3])
            p = nextps()
            for kc in range(8):
                tr(p[:, kc * 4:kc * 4 + 3], c3[:, kc * 128:(kc + 1) * 128], identf[0:3, 0:3], [c3.k, identf.k], [p.k])
            cp("vector", sT[:], p[:, 0:32].rearrange("p (a b) -> p a b", a=8)[:, :, 0:3], [p.k], [sT.k])
            bm3 = sb("bm3", [3, 6 * D]); load_const(bm3, bmod_d[0].partition_broadcast(3))
            modrows = sb("modrows", [3, 6 * D])
            wmc = [sb("wmc%d" % i, [128, 8, 512]) for i in range(4)]
            for ci in range(12):
                wb = wmc[ci % 4]
                dma("sync", wb[:], wmod_d.rearrange("(c p) n -> p c n", p=128)[:, :, ci * 512:(ci + 1) * 512], [], [wb.k], wb.k)
                p = nextps()
                for kc in range(8):
                    mm(p[0:3, :], sT[:, kc, :], wb[:, kc, :], kc == 0, kc == 7, [sT.k, wb.k], [p.k])
                tt("vector", modrows[:, ci * 512:(ci + 1) * 512], p[0:3, :], bm3[:, ci * 512:(ci + 1) * 512], ALU.add,
                   [p.k, bm3.k], [modrows.k])
            dma("sync", modsc_d, modrows[:], [modrows.k], ["modsc"], modrows.k)

            S.emit()

        if CFG["stop"] == "mod0":
            return nc
        with ExitStack() as es:
            S = Sched(nc, "a")

            def sb(name, shape, dt=F32):
                return Buf(es.enter_context(nc.sbuf_tensor(name, list(shape), dt)), name)

            def ps(name, shape, dt=F32):
                return Buf(es.enter_context(nc.psum_tensor(name, list(shape), dt)), name)

            PB = [ps("pb%d" % i, [128, 512]) for i in range(7)]
            PT = ps("ptb", [128, 1024], BF16)
            class RR:
                def __init__(self, banks):
                    self.b = banks
                    self.i = 0

                def next(self):
                    p = self.b[self.i % len(self.b)]
                    self.i += 1
                    return p

            al_all = RR(PB)
            cur_al = [al_all]

            def nextps():
                return cur_al[0].next()

            def run(pairs, bg=None):
                live = list(pairs)
                while live:
                    for item in list(live):
                        cur_al[0] = item[1]
                        try:
                            for _ in range(item[2] if len(item) > 2 else 1):
                                next(item[0])
                        except StopIteration:
                            live.remove(item)
                    if bg:
                        cur_al[0] = al_all
                        try:
                            next(bg[0])
                        except StopIteration:
                            bg.pop(0)
                cur_al[0] = al_all

            def drain(bg):
                cur_al[0] = al_all
                while bg:
                    try:
                        next(bg[0])
                    except StopIteration:
                        bg.pop(0)

            def dma(eng, out, in_, r, w, key, **kw):
                S.op(eng, lambda e: e.dma_start(out=out, in_=in_, **kw), reads=r, writes=w, dma=key)

            def mm(out, lhsT, rhs, start, stop, r, w):
                S.op("tensor", lambda e: e.matmul(out, lhsT, rhs, start=start, stop=stop), reads=r, writes=w)

            def tr(out, in_, ident, r, w):
                S.op("tensor", lambda e: e.transpose(out, in_, ident), reads=r, writes=w)

            def act(out, in_, func, r, w, bias=None, scale=None, accum=None):
                kw = {}
                if bias is not None:
                    kw["bias"] = bias
                if scale is not None:
                    kw["scale"] = scale
                if accum is not None:
                    kw["accum_out"] = accum
                S.op("scalar", lambda e: e.activation(out=out, in_=in_, func=func, **kw), reads=r, writes=w)

            def tt(eng, out, in0, in1, op, r, w):
                S.op(eng, lambda e: e.tensor_tensor(out=out, in0=in0, in1=in1, op=op), reads=r, writes=w)

            def ts(eng, out, in0, s1, s2, op0, op1, r, w):
                if op1 is None:
                    S.op(eng, lambda e: e.tensor_scalar(out=out, in0=in0, scalar1=s1, scalar2=None, op0=op0), reads=r, writes=w)
                else:
                    S.op(eng, lambda e: e.tensor_scalar(out=out, in0=in0, scalar1=s1, scalar2=s2, op0=op0, op1=op1), reads=r, writes=w)

            def stt(out, in0, scalar, in1, op0, op1, r, w):
                S.op("vector", lambda e: e.scalar_tensor_tensor(out=out, in0=in0, scalar=scalar, in1=in1, op0=op0, op1=op1), reads=r, writes=w)

            def cp(eng, out, in_, r, w):
                if eng == "scalar":
                    S.op(eng, lambda e: e.copy(out=out, in_=in_), reads=r, writes=w)
                else:
                    S.op(eng, lambda e: e.tensor_copy(out=out, in_=in_), reads=r, writes=w)

            def red(out, in_, r, w, op=ALU.add):
                S.op("vector", lambda e: e.tensor_reduce(out=out, in_=in_, axis=AX.X, op=op), reads=r, writes=w)

            def recip(out, in_, r, w):
                S.op("vector", lambda e: e.reciprocal(out=out, in_=in_), reads=r, writes=w)

            def mset(eng, out, val, w):
                S.op(eng, lambda e: e.memset(out, val), writes=w)

            DBG = {}

            def dump(name, buf, ap, shape, dt=F32):
                if not CFG.get("dbg") or name in DBG:
                    return
                d = nc.dram_tensor("dbg_" + name, list(shape), dt, kind="ExternalOutput").ap()
                DBG[name] = d
                dma("sync", d, ap, [buf.k], ["dbg_" + name], "dbgk")

            PK = "prm"
            S.group_keys.update([PK])
            def load_const(buf, src, eng="sync", **kw):
                dma(eng, buf[:], src, [], [buf.k], PK, **kw)

            trif = sb("trif", [128, 128]); load_const(trif, trif_d)
            trib = sb("trib", [128, 128]); load_const(trib, trib_d)
            nones = sb("nones", [128, 128]); load_const(nones, nones_d)
            mskf = sb("mskf", [128, 128], BF16); load_const(mskf, mskf_d)
            mskb = sb("mskb", [128, 128], BF16); load_const(mskb, mskb_d)
            amlo = sb("amlo", [128, 128], BF16); load_const(amlo, amlo_d)
            amhi = sb("amhi", [128, 128], BF16); load_const(amhi, amhi_d)
            cosT = sb("cosT", [128, NT, 32]); load_const(cosT, cos_d.rearrange("(n p) f -> p n f", p=128))
            sinT = sb("sinT", [128, NT, 32]); load_const(sinT, sin_d.rearrange("(n p) f -> p n f", p=128))
            bgB = sb("bgB", [128, 16]); load_const(bgB, bg_d[0].partition_broadcast(128))
            qnB = sb("qnB", [128, 64]); load_const(qnB, qn_d[0].partition_broadcast(128))
            knB = sb("knB", [128, 64]); load_const(knB, kn_d[0].partition_broadcast(128))
            mnB = sb("mnB", [128, 512]); load_const(mnB, mn_d[0].partition_broadcast(128))
            esink = sb("esink", [128, 8]); load_const(esink, sink_d[0].partition_broadcast(128))
            act(esink[:], esink[:], AF.Exp, [esink.k], [esink.k])
            cwT = sb("cwT", [128, 8, 3])
            for jj in range(3):
                S.op("sync", lambda e, jj=jj: e.dma_start(out=cwT[:, :, jj], in_=conv_d[jj].rearrange("(c p) -> p c", p=128),
                                                         allow_slow_non_contiguous=True), writes=[cwT.k], dma="cwk")
            if CFG["stop"] == "s1":
                S.emit()
                return nc
            wr = sb("wr", [128, 8, NEXP]); load_const(wr, wr_d.rearrange("(c p) n -> p c n", p=128))
            win = sb("win", [128, 8, 2832], BF16)
            hTB = sb("hTB", [128, 8, 512], BF16)
            hTB.k = ("MULTI",) + tuple(("hTB", i_, j_) for i_ in range(4) for j_ in range(4))
            hTBf = hTB[:].rearrange("p a b -> p (a b)").bitcast(F32)
            stg_ap = [hTBf[:, i_ * 512:(i_ + 1) * 512] for i_ in range(4)]
            stg_k = [("MULTI",) + tuple(("hTB", i_, j_) for j_ in range(4)) for i_ in range(4)]
            stc1 = [0]
            for kc in range(8):
                for c0 in range(0, 2832, 512):
                    w_ = min(512, 2832 - c0)
                    si = stc1[0] % 4
                    eng = ("scalar", "gpsimd", "vector")[stc1[0] % 3]
                    stc1[0] += 1
                    dma("sync", stg_ap[si][:, 0:w_], win_d[kc * 128:(kc + 1) * 128, c0:c0 + w_], [], [stg_k[si]], "stg%d" % si)
                    cp(eng, win[:, kc, c0:c0 + w_], stg_ap[si][:, 0:w_], [stg_k[si]], [win.k])
            wout = sb("wout", [128, 8, D], BF16)

            def load_wout_scaled():
                for kc in range(8):
                    for c0 in range(0, D, 512):
                        si = stc1[0] % 4
                        stc1[0] += 1
                        dma("sync", stg_ap[si], wout_d[kc * 128:(kc + 1) * 128, c0:c0 + 512], [], [stg_k[si]], "stg%d" % si)
                        tt("vector", wout[:, kc, c0:c0 + 512], stg_ap[si], t1[:, c0:c0 + 512], ALU.mult, [stg_k[si], t1.k], [wout.k])

            if CFG["stop"] == "s2":
                S.emit()
                return nc
            A1 = sb("A1", [128, D]); B1 = sb("B1", [128, D])
            A2 = sb("A2", [128, D]); B2 = sb("B2", [128, D])
            t1 = sb("t1", [128, D])
            t1.k = ("MULTI", "t1a", "t1b")

            def load_mod(row, want):
                def bc(i):
                    return modsc_d[row, i * D:(i + 1) * D].partition_broadcast(128)
                if "A1" in want:
                    dma("sync", t1[:], bc(1), [], [t1.k], "t1d")
                    dma("sync", A1[:], n1_d[0].partition_broadcast(128), [], [A1.k], A1.k)
                    stt(A1[:], t1[:], 1.0, A1[:], ALU.add, ALU.mult, [t1.k, A1.k], [A1.k])
                if "B1" in want:
                    dma("sync", B1[:], bc(0), [], [B1.k], B1.k)
                if "G1" in want:
                    dma("sync", t1[:], bc(2), [], [t1.k], "t1d")
                if "A2" in want:
                    dma("sync", t1[:], bc(4), [], [t1.k], "t1d")
                    dma("sync", A2[:], n2_d[0].partition_broadcast(128), [], [A2.k], A2.k)
                    stt(A2[:], t1[:], 1.0, A2[:], ALU.add, ALU.mult, [t1.k, A2.k], [A2.k])
                if "B2" in want:
                    dma("sync", B2[:], bc(3), [], [B2.k], B2.k)

            xt = [sb("xt%d" % i, [128, D]) for i in range(2)]
            hx = [sb("hx%d" % i, [128, D], BF16) for i in range(2)]
            hT = sb("hT", [128, 8, 512], BF16)
            hT.k = ("MULTI",) + tuple(("hT", j_) for j_ in range(4))
            HB = [hT, hTB]

            def tile_keys(hb_, j_):
                if hb_ is hT:
                    return [("hT", j_)]
                return [("hTB", i_, j_) for i_ in range(4)]
            st4 = [sb("st4_%d" % i, [128, 4]) for i in range(2)]
            HS = sb("HS", [128, 8, 2 * NG])
            KT = sb("KT", [128, SEQ + CTX], BF16)
            V = sb("V", [128, NT + 2, 2, 65], BF16)
            KT.k = ("MULTI",) + tuple(("KT", i_) for i_ in range(NT + 2))
            V.k = ("MULTI",) + tuple(("V", i_) for i_ in range(NT + 2))
            mset("vector", V[:], 1.0, [V.k])
            CdS = [sb("CdS%d" % i, [128, 4, 130], BF16) for i in range(2)]
            CdL = [sb("CdL%d" % i, [128, 4, 130], BF16) for i in range(2)]
            Sst = {"f": sb("Sf", [128, 4, 130]), "b": sb("Sb", [128, 4, 130])}
            Sdec = sb("Sdec", [128, 4, 130])
            CdF = sb("CdF", [128, 4, 130], BF16)
            mqT = sb("mqT", [128, 4, 512], BF16)
            mkT = sb("mkT", [128, 4, 512], BF16)
            mqT.k = ("MULTI",) + tuple(("mqT", c_) for c_ in range(4))
            mkT.k = ("MULTI",) + tuple(("mkT", c_) for c_ in range(4))
            Kt = sb("Kt", [128, 4, 512], BF16)
            qk_sq = sb("qk_sq", [128, 512])
            qk_t = sb("qk_t", [128, 512])
            qk_a = sb("qk_a", [128, 256])
            qk_b = sb("qk_b", [128, 256])
            qk_o = sb("qk_o", [128, 512], BF16)
            qk_ss = sb("qk_ss", [128, 10])
            QT = sb("QT", [128, 4, 128], BF16)
            PTs = sb("PTs", [128, 5, 512], BF16)
            arec = sb("arec", [128, 4])
            hrs = sb("hrs", [128, 4])
            mixr = [sb("mix%d" % i, [128, D], BF16) for i in range(2)]
            cv = Buf(t1.t, t1.k)
            mixT = sb("mixT", [128, 8, 128], BF16)
            mvs = sb("mvs", [128, 512])
            sig = sb("sig", [128, 512])
            gt = sb("gt", [128, 16])
            l1 = sb("l1", [128, 8])
            g8 = sb("g8", [128, 8])
            ws8 = sb("ws8", [128, 8])
            E8 = sb("E8", [128, 8])
            dec4 = sb("dec4", [128, 4])
            Vw = {"f": sb("Vwf", [128, 4, 130], BF16), "b": sb("Vwb", [128, 4, 130], BF16)}
            Sm = {"f": sb("Smf", [128, 4, 128], BF16), "b": sb("Smb", [128, 4, 128], BF16)}
            sc8 = sb("sc8", [128, 8])
            sc8n = sb("sc8n", [128, 8])
            hs = sb("hs", [128, 512])
            xn = [sb("xn%d" % i, [128, D]) for i in range(1)]
            h2 = t1
            h2b = [sb("h2b%d" % i, [128, D], BF16) for i in range(1)]
            h2T = sb("h2T", [128, 8, 128])
            rt = sb("rt", [128, 16])
            rs1 = sb("rs1", [128, 1])
            rs2 = sb("rs2", [128, 1])
            if CFG["stop"] == "s3":
                S.emit()
                return nc
            ring = {"xt": 0, "hx": 0, "xn": 0, "h2b": 0, "st": 0, "cds": 0, "cdl": 0}

            def rr(name, lst):
                b = lst[ring[name] % len(lst)]
                ring[name] += 1
                return b

            def norm_mod(src_ap_rows, npart, A, Bm, out_buf, xbufs, xname):
                xb = rr(xname, xbufs)
                dma("sync", xb[0:npart, :], src_ap_rows, [], [xb.k], xb.k)
                s4 = rr("st", st4)
                act(out_buf[0:npart, :], xb[0:npart, :], AF.Square, [xb.k], [out_buf.k, s4.k], accum=s4[0:npart, 0:1])
                act(s4[0:npart, 1:2], s4[0:npart, 0:1], AF.Ln, [s4.k], [s4.k], bias=EPS, scale=1.0 / D)
                act(s4[0:npart, 2:3], s4[0:npart, 1:2], AF.Exp, [s4.k], [s4.k], scale=-0.5)
                stt(out_buf[0:npart, :], xb[0:npart, :], s4[0:npart, 2:3], A[0:npart, :], ALU.mult, ALU.mult,
                    [xb.k, s4.k, A.k], [out_buf.k])
                tt("vector", out_buf[0:npart, :], out_buf[0:npart, :], Bm[0:npart, :], ALU.add, [out_buf.k, Bm.k], [out_buf.k])
                return xb

            def gen_stage_tile(src_rows_fn, j, A, Bm, hb_=None):
                hb_ = hT if hb_ is None else hb_
                hb = rr("hx", hx)
                xb = rr("xt", xt)
                dma("sync", xb[:], src_rows_fn(j), [], [xb.k], xb.k)
                s4 = rr("st", st4)
                yield
                act(hb[:], xb[:], AF.Square, [xb.k], [hb.k, s4.k], accum=s4[:, 0:1])
                act(s4[:, 1:2], s4[:, 0:1], AF.Ln, [s4.k], [s4.k], bias=EPS, scale=1.0 / D)
                act(s4[:, 2:3], s4[:, 1:2], AF.Exp, [s4.k], [s4.k], scale=-0.5)
                yield
                stt(hb[:], xb[:], s4[:, 2:3], A[:], ALU.mult, ALU.mult, [xb.k, s4.k, A.k], [hb.k])
                yield
                tt("vector", hb[:], hb[:], Bm[:], ALU.add, [hb.k, Bm.k], [hb.k])
                yield
                for kc in range(8):
                    tr(PT[:, kc * 128:(kc + 1) * 128], hb[:, kc * 128:(kc + 1) * 128], identb[:], [hb.k, identb.k], [PT.k])
                cp("scalar", hb_[:, :, j * 128:(j + 1) * 128], PT[:].rearrange("p (a b) -> p a b", a=8), [PT.k], tile_keys(hb_, j))
                yield

            def stage_a(src_rows_fn, ntile, A, Bm, hb_=None):
                for j0 in range(0, ntile, 2):
                    run([(gen_stage_tile(src_rows_fn, j, A, Bm, hb_), al_all) for j in range(j0, min(j0 + 2, ntile))])

            def gen_feat_chunk(col0, cc, N, dst, cw0, hprev, hnext, hb_=None):
                hb_ = hT if hb_ is None else hb_
                HTK = [hb_.k]
                p = nextps()
                base = (cc % 2) * 512
                ck = "t1a" if cc % 2 == 0 else "t1b"
                cvv = lambda a_, b_: t1[:, base + a_:base + b_]
                for kc in range(8):
                    mm(p[:, 0:N], win[:, kc, col0 + cc * 128: col0 + (cc + 1) * 128], hb_[:, kc, 0:N], kc == 0, kc == 7,
                       [win.k] + HTK, [p.k])
                yield
                act(cvv(0, N), p[:, 0:N], AF.Identity, [p.k, cwT.k], [ck], scale=cwT[:, cw0 + cc, 1:2])
                yield
                stt(cvv(1, N), p[:, 0:N - 1], cwT[:, cw0 + cc, 0:1], cvv(1, N), ALU.mult, ALU.add, [p.k, cwT.k, ck], [ck])
                stt(cvv(0, N - 1), p[:, 1:N], cwT[:, cw0 + cc, 2:3], cvv(0, N - 1), ALU.mult, ALU.add, [p.k, cwT.k, ck], [ck])
                if hprev is not None:
                    stt(cvv(0, 1), HS[:, cw0 + cc, hprev:hprev + 1], cwT[:, cw0 + cc, 0:1], cvv(0, 1), ALU.mult, ALU.add,
                        [HS.k, cwT.k, ck], [ck])
                if hnext is not None:
                    stt(cvv(N - 1, N), HS[:, cw0 + cc, hnext:hnext + 1], cwT[:, cw0 + cc, 2:3], cvv(N - 1, N), ALU.mult, ALU.add,
                        [HS.k, cwT.k, ck], [ck])
                yield
                act(dst[:, cc, 0:N], cvv(0, N), AF.Silu, [ck], [dst.k[1 + cc]])
                yield

            def feat_proj(col0, nchunks, N, dst, cw0, hprev, hnext, hb_=None):
                for c0 in range(0, nchunks, 2):
                    run([(gen_feat_chunk(col0, cc, N, dst, cw0, hprev, hnext, hb_), al_all) for cc in range(c0, c0 + 2)])

            def k_tokmajor(ntile):
                for j in range(ntile):
                    for h in range(4):
                        tr(PT[:, h * 128:(h + 1) * 128], mkT[:, h, j * 128:(j + 1) * 128], identb[:], [mkT.k, identb.k], [PT.k])
                    cp("vector", Kt[:, j, :], PT[:, 0:512], [PT.k], [Kt.k])

            def qk_norm_rope(src, srck, H, wB, tile_n, out_ap):
                W = H * 64
                s3 = lambda a: a.rearrange("p (h d) -> p h d", h=H)
                act(qk_sq[:, 0:W], src, AF.Square, [srck], [qk_sq.k])
                red(qk_ss[:, 0:H], s3(qk_sq[:, 0:W]), [qk_sq.k], [qk_ss.k])
                act(qk_ss[:, 0:H], qk_ss[:, 0:H], AF.Ln, [qk_ss.k], [qk_ss.k], bias=EPS, scale=1.0 / 64)
                act(qk_ss[:, 0:H], qk_ss[:, 0:H], AF.Exp, [qk_ss.k], [qk_ss.k], scale=-0.5)
                tt("vector", s3(qk_t[:, 0:W]), s3(src), qk_ss[:, 0:H].unsqueeze(2).to_broadcast([128, H, 64]), ALU.mult,
                   [srck, qk_ss.k], [qk_t.k])
                wbc = wB[:, :].unsqueeze(1).to_broadcast([128, H, 64])
                if tile_n is None:
                    tt("gpsimd", s3(out_ap), s3(qk_t[:, 0:W]), wbc, ALU.mult, [qk_t.k, wB.k], [out_ap.tensor.name if False else "qk_o"])
                    return
                tt("gpsimd", s3(qk_t[:, 0:W]), s3(qk_t[:, 0:W]), wbc, ALU.mult, [qk_t.k, wB.k], [qk_t.k])
                x1 = s3(qk_t[:, 0:W])[:, :, 0:32]
                x2 = s3(qk_t[:, 0:W])[:, :, 32:64]
                cb = cosT[:, tile_n, :].unsqueeze(1).to_broadcast([128, H, 32])
                sbn = sinT[:, tile_n, :].unsqueeze(1).to_broadcast([128, H, 32])
                h3 = lambda a: a.rearrange("p (h d) -> p h d", h=H)
                a_ = h3(qk_a[:, 0:H * 32]); b_ = h3(qk_b[:, 0:H * 32])
                o3 = s3(out_ap)
                tt("vector", a_, x1, cb, ALU.mult, [qk_t.k, cosT.k], [qk_a.k])
                tt("gpsimd", b_, x2, sbn, ALU.mult, [qk_t.k, sinT.k], [qk_b.k])
                tt("vector", o3[:, :, 0:32], a_, b_, ALU.subtract, [qk_a.k, qk_b.k], ["qk_o"])
                tt("vector", a_, x2, cb, ALU.mult, [qk_t.k, cosT.k, "qk_o"], [qk_a.k])
                tt("gpsimd", b_, x1, sbn, ALU.mult, [qk_t.k, sinT.k, "qk_o"], [qk_b.k])
                tt("vector", o3[:, :, 32:64], a_, b_, ALU.add, [qk_a.k, qk_b.k], ["qk_o"])

            def gates_prep(psg_ap, psgk):
                tt("vector", gt[:], psg_ap, bgB[:], ALU.add, [psgk, bgB.k], [gt.k])
                g3 = gt[:].rearrange("p (a b) -> p a b", a=2)
                act(l1[:].rearrange("p (a b) -> p a b", a=2), g3[:, :, 4:8], AF.Exp, [gt.k], [l1.k], scale=-1.0)
                act(l1[:], l1[:], AF.Ln, [l1.k], [l1.k], bias=1.0)
                cp("gpsimd", g8[:].rearrange("p (a b) -> p a b", a=2), g3[:, :, 0:4], [gt.k], [g8.k])

            def chunk_weights(dirs, need_tot):
                p = nextps()
                if "f" in dirs:
                    mm(p[:, 0:4], trif[:], l1[:, 0:4], True, True, [trif.k, l1.k], [p.k])
                if "b" in dirs:
                    mm(p[:, 4:8], trib[:], l1[:, 4:8], True, True, [trib.k, l1.k], [p.k])
                c0 = 0 if need_tot == "f" else 4
                mm(p[:, 8:12], nones[:], l1[:, c0:c0 + 4], True, True, [nones.k, l1.k], [p.k])
                lo, hi = (0, 8) if len(dirs) == 2 else ((0, 4) if "f" in dirs else (4, 8))
                tt("vector", ws8[:, lo:hi], g8[:, lo:hi], p[:, lo:hi], ALU.subtract, [g8.k, p.k], [ws8.k])
                act(ws8[:, lo:hi], ws8[:, lo:hi], AF.Exp, [ws8.k], [ws8.k])
                act(E8[:, lo:hi], p[:, lo:hi], AF.Exp, [p.k], [E8.k])
                act(dec4[:], p[:, 8:12], AF.Exp, [p.k], [dec4.k])

            def make_vw(d):
                c0 = 0 if d == "f" else 4
                tt("vector", Vw[d][:, :, 0:128], mvs[:].rearrange("p (h d) -> p h d", h=4),
                   ws8[:, c0:c0 + 4].unsqueeze(2).to_broadcast([128, 4, 128]), ALU.mult, [mvs.k, ws8.k], [Vw[d].k])
                cp("gpsimd", Vw[d][:, :, 128:129], ws8[:, c0:c0 + 4].unsqueeze(2), [ws8.k], [Vw[d].k])

            def state_update(d, j, cd_out_ap, cd_key):
                St = Sst[d]
                decb = dec4[:].unsqueeze(2).to_broadcast([128, 4, 130])
                tt("vector", Sdec[:], St[:], decb, ALU.mult, [St.k, dec4.k], [Sdec.k])
                ts("vector", cd_out_ap, Sdec[:], QS, None, ALU.mult, None, [Sdec.k], [cd_key])
                pa = nextps(); pb = nextps()
                for h in range(4):
                    pp = pa if h < 2 else pb
                    o = pp[:, 0:260].rearrange("p (a b) -> p a b", a=2)[:, h % 2, 0:129]
                    mm(o, Kt[:, j, h * 128:(h + 1) * 128], Vw[d][:, h, 0:129], True, True, [Kt.k, Vw[d].k], [pp.k])
                tt("vector", St[:, 0:2, 0:129], Sdec[:, 0:2, 0:129], pa[:, 0:260].rearrange("p (a b) -> p a b", a=2)[:, :, 0:129],
                   ALU.add, [Sdec.k, pa.k], [St.k])
                tt("vector", St[:, 2:4, 0:129], Sdec[:, 2:4, 0:129], pb[:, 0:260].rearrange("p (a b) -> p a b", a=2)[:, :, 0:129],
                   ALU.add, [Sdec.k, pb.k], [St.k])

            def p1_tile(j, kv_col, v_idx, rope_n, dirs_states, cdb_idx):
                pX = nextps(); pY = nextps()
                for kc in range(8):
                    mm(pX[:], hT[:, kc, j * 128:(j + 1) * 128], win[:, kc, C_AK:C_AK + 512], kc == 0, kc == 7, [hT.k, win.k], [pX.k])
                for kc in range(8):
                    mm(pY[:, 0:272], hT[:, kc, j * 128:(j + 1) * 128], win[:, kc, C_AK + 512:C_AK + 784], kc == 0, kc == 7,
                       [hT.k, win.k], [pY.k])
                qk_norm_rope(pX[:, 0:128], pX.k, 2, knB, rope_n, qk_o[:, 0:128])
                tr(PT[:, 0:128], qk_o[:, 0:128], identb[:], ["qk_o", identb.k], [PT.k])
                cp("scalar", KT[:, kv_col:kv_col + 128], PT[:, 0:128], [PT.k], [KT.k])
                cp("scalar", V[:, v_idx, :, 0:64], pX[:, 128:256].rearrange("p (a b) -> p a b", a=2), [pX.k], [V.k])
                cp("scalar", mvs[:, 0:256], pX[:, 256:512], [pX.k], [mvs.k])
                cp("scalar", mvs[:, 256:512], pY[:, 0:256], [pY.k], [mvs.k])
                gates_prep(pY[:, 256:272], pY.k)
                for d in dirs_states:
                    chunk_weights([d], d)
                    make_vw(d)
                    if d == "b" and cdb_idx is not None:
                        cs = rr("cds", CdS)
                        state_update(d, j, cs[:], cs.k)
                        dma("sync", cdb_d[cdb_idx], cs[:], [cs.k], [("cdb", cdb_idx)], cs.k)
                    else:
                        state_update(d, j, CdF[:], CdF.k)

            def p1_group_ctx(b):
                stage_a(lambda j: ctx_d[b * CTX + j * 128: b * CTX + (j + 1) * 128, :], 2, A1, B1)
                feat_proj(C_MK, 4, 256, mkT, 4, None, None)
                k_tokmajor(2)
                for j in (0, 1):
                    p1_tile(j, SEQ + j * 128, NT + j, None, ["f"], None)
                for j in (1, 0):
                    p1_tile(j, SEQ + j * 128, NT + j, None, ["b"], None)

            def gen_p1_proj(j, pX, pY, hb_):
                for kc in range(8):
                    mm(pX[:], hb_[:, kc, j * 128:(j + 1) * 128], win[:, kc, C_AK:C_AK + 512], kc == 0, kc == 7, [hb_.k, win.k], [pX.k])
                yield
                for kc in range(8):
                    mm(pY[:, 0:272], hb_[:, kc, j * 128:(j + 1) * 128], win[:, kc, C_AK + 512:C_AK + 784], kc == 0, kc == 7,
                       [hb_.k, win.k], [pY.k])
                yield

            def gen_p1_k(j, pX, kv_col, v_idx, rope_n):
                cp("scalar", V[:, v_idx, :, 0:64], pX[:, 128:256].rearrange("p (a b) -> p a b", a=2), [pX.k], [V.k[1 + v_idx]])
                yield
                qk_norm_rope(pX[:, 0:128], pX.k, 2, knB, rope_n, qk_o[:, 0:128])
                yield
                tr(PT[:, 0:128], qk_o[:, 0:128], identb[:], ["qk_o", identb.k], [PT.k])
                cp("scalar", KT[:, kv_col:kv_col + 128], PT[:, 0:128], [PT.k], [KT.k[1 + v_idx]])
                yield

            def gen_p1_s(j, pX, pY, d, cdb_idx):
                cp("scalar", mvs[:, 0:256], pX[:, 256:512], [pX.k], [mvs.k])
                cp("scalar", mvs[:, 256:512], pY[:, 0:256], [pY.k], [mvs.k])
                gates_prep(pY[:, 256:272], pY.k)
                yield
                chunk_weights([d], d)
                yield
                make_vw(d)
                yield
                cs = rr("cds", CdS)
                state_update(d, j, cs[:], cs.k)
                dma("sync", cdb_d[cdb_idx], cs[:], [cs.k], [("cdb", cdb_idx)], cs.k)
                yield

            def xrows(b, g):
                return lambda j: x_d[b * SEQ + g * 512 + j * 128: b * SEQ + g * 512 + (j + 1) * 128, :]

            def p1_group(b, g, gnext):
                hb_ = HB[g % 2]
                feat_proj(C_MK, 4, 512, mkT, 4, (NG + g - 1) if g > 0 else None, (g + 1) if g < NG - 1 else None, hb_)
                k_tokmajor(4)
                al_S = RR(PB[4:7])
                pairs = [(PB[0], PB[1]), (PB[2], PB[3])]
                js = (3, 2, 1, 0)
                bg = []
                if gnext is not None:
                    bg = [gen_stage_tile(xrows(b, gnext), i_, A1, B1, HB[gnext % 2]) for i_ in range(4)]
                run([(gen_p1_proj(js[0], pairs[0][0], pairs[0][1], hb_), al_all)])
                for i, j in enumerate(js):
                    n = g * 4 + j
                    pX, pY = pairs[i % 2]
                    gens = [(gen_p1_k(j, pX, n * 128, n, n), al_S), (gen_p1_s(j, pX, pY, "b", n), al_S)]
                    if i + 1 < len(js):
                        gens.append((gen_p1_proj(js[i + 1], pairs[(i + 1) % 2][0], pairs[(i + 1) % 2][1], hb_), al_all))
                    run(gens, bg)
                drain(bg)

            def p0(b):
                hb = rr("hx", hx)
                xb = rr("xt", xt)
                xv = x_d[b * SEQ:(b + 1) * SEQ, :].rearrange("(g t) d -> t g d", t=512)
                dma("sync", xb[0:NG, :], xv[0], [], [xb.k], xb.k)
                dma("sync", xb[NG:2 * NG, :], xv[511], [], [xb.k], xb.k)
                s4 = rr("st", st4)
                act(hb[0:2 * NG, :], xb[0:2 * NG, :], AF.Square, [xb.k], [hb.k, s4.k], accum=s4[0:2 * NG, 0:1])
                act(s4[0:2 * NG, 1:2], s4[0:2 * NG, 0:1], AF.Ln, [s4.k], [s4.k], bias=EPS, scale=1.0 / D)
                act(s4[0:2 * NG, 2:3], s4[0:2 * NG, 1:2], AF.Exp, [s4.k], [s4.k], scale=-0.5)
                stt(t1[0:2 * NG, :], xb[0:2 * NG, :], s4[0:2 * NG, 2:3], A1[0:2 * NG, :], ALU.mult, ALU.mult, [xb.k, s4.k, A1.k], [t1.k])
                tt("gpsimd", hb[0:2 * NG, :], t1[0:2 * NG, :], B1[0:2 * NG, :], ALU.add, [t1.k, B1.k], [hb.k])
                for kc in range(8):
                    tr(PT[:, kc * 128:kc * 128 + 2 * NG], hb[0:2 * NG, kc * 128:(kc + 1) * 128], identb[0:2 * NG, 0:2 * NG], [hb.k, identb.k], [PT.k])
                cp("scalar", hT[:, :, 0:2 * NG], PT[:].rearrange("p (a b) -> p a b", a=8)[:, :, 0:2 * NG], [PT.k], [hT.k])
                p = nextps()
                for cc in range(8):
                    for kc in range(8):
                        mm(p[:, cc * 2 * NG:(cc + 1) * 2 * NG], win[:, kc, C_MQ + cc * 128:C_MQ + (cc + 1) * 128], hT[:, kc, 0:2 * NG],
                           kc == 0, kc == 7, [win.k, hT.k], [p.k])
                cp("vector", HS[:], p[:, 0:16 * NG].rearrange("p (a b) -> p a b", a=8), [p.k], [HS.k])

            def gen_attention(n):
                blocks = []
                if n > 0:
                    blocks.append((n - 1) * 128)
                blocks.append(n * 128)
                if n < NT - 1:
                    blocks.append((n + 1) * 128)
                blocks += [SEQ, SEQ + 128]
                for kv in range(2):
                    pr = slice(64 * kv, 64 * kv + 64)
                    for bi, col in enumerate(blocks):
                        p = nextps()
                        mm(p[:], KT[pr, col:col + 128], QT[pr, :, :], True, True, [KT.k, QT.k], [p.k])
                        act(PTs[:, bi, :], p[:], AF.Exp, [p.k], [PTs.k], scale=0.125)
                        if col == (n - 1) * 128:
                            m = amlo
                        elif col == (n + 1) * 128 and col < SEQ:
                            m = amhi
                        else:
                            m = None
                        if m is not None:
                            tt("gpsimd", PTs[:, bi, :].rearrange("p (g q) -> p g q", g=4),
                               PTs[:, bi, :].rearrange("p (g q) -> p g q", g=4),
                               m[:, :].unsqueeze(1).to_broadcast([128, 4, 128]), ALU.mult, [PTs.k, m.k], [PTs.k])
                        yield
                    po = nextps()
                    for gi in range(4):
                        for bi, col in enumerate(blocks):
                            vi = col // 128
                            mm(po[:, gi * 65:(gi + 1) * 65], PTs[:, bi, gi * 128:(gi + 1) * 128], V[:, vi, kv, :],
                               bi == 0, bi == len(blocks) - 1, [PTs.k, V.k], [po.k])
                        yield
                    po3 = po[:, 0:260].rearrange("p (g c) -> p g c", g=4)
                    tt("vector", arec[:], po3[:, :, 64], esink[:, kv * 4:kv * 4 + 4], ALU.add, [po.k, esink.k], [arec.k])
                    recip(arec[:], arec[:], [arec.k], [arec.k])
                    tt("vector", mixr[n % 2][:, kv * 256:(kv + 1) * 256].rearrange("p (g c) -> p g c", g=4), po3[:, :, 0:64],
                       arec[:].unsqueeze(2).to_broadcast([128, 4, 64]), ALU.mult, [po.k, arec.k], ["mixA%d" % (n % 2)])
                    yield

            def gen_head(b, g, j):
                hb_ = HB[g % 2]
                n = g * 4 + j
                tsl = slice(j * 128, (j + 1) * 128)
                pq = nextps(); po_ = nextps(); pv = nextps(); pg = nextps()
                for (pp, c0, w) in ((pq, C_AQ, 512), (pv, C_MV, 512), (pg, C_GT, 16), (po_, C_MO, 512)):
                    for kc in range(8):
                        mm(pp[:, 0:w], hb_[:, kc, tsl], win[:, kc, c0:c0 + w], kc == 0, kc == 7, tile_keys(hb_, j) + [win.k], [pp.k])
                    yield
                qk_norm_rope(pq[:], pq.k, 8, qnB, n, qk_o[:, 0:512])
                yield
                for pi in range(4):
                    tr(PT[:, pi * 128:(pi + 1) * 128], qk_o[:, pi * 128:(pi + 1) * 128], identb[:], ["qk_o", identb.k], [PT.k])
                cp("scalar", QT[:], PT[:, 0:512].rearrange("p (a b) -> p a b", a=4), [PT.k], [QT.k])
                yield
                cp("scalar", mvs[:], pv[:], [pv.k], [mvs.k])
                gates_prep(pg[:, 0:16], pg.k)
                yield
                act(sig[:], po_[:], AF.Exp, [po_.k], [sig.k], scale=-1.0)
                act(sig[:], sig[:], AF.Ln, [sig.k], [sig.k], bias=1.0)
                act(sig[:], sig[:], AF.Exp, [sig.k], [sig.k], scale=-1.0)
                tt("gpsimd", sig[:], sig[:], mnB[:], ALU.mult, [sig.k, mnB.k], [sig.k])
                yield

            def gen_mlstm(b, g, j):
                n = g * 4 + j
                tsl = slice(j * 128, (j + 1) * 128)
                pS = nextps()
                for h in range(4):
                    mm(pS[:, h * 128:(h + 1) * 128], mkT[:, h, tsl], mqT[:, h, tsl], True, True, [mkT.k, mqT.k], [pS.k])
                chunk_weights(["f", "b"], "f")
                yield
                pS3 = pS[:].rearrange("p (h t) -> p h t", h=4)
                tt("vector", Sm["f"][:], pS3, mskf[:, :].unsqueeze(1).to_broadcast([128, 4, 128]), ALU.mult, [pS.k, mskf.k], [Sm["f"].k])
                tt("vector", Sm["b"][:], pS3, mskb[:, :].unsqueeze(1).to_broadcast([128, 4, 128]), ALU.mult, [pS.k, mskb.k], [Sm["b"].k])
                make_vw("f"); make_vw("b")
                yield
                state_update("f", j, CdF[:], CdF.k)
                yield
                cl = rr("cdl", CdL)
                dma("sync", cl[:], cdb_d[n], [("cdb", n)], [cl.k], cl.k)
                for di, d in enumerate(("f", "b")):
                    pa = nextps(); pb = nextps()
                    for h in range(4):
                        pp = pa if h < 2 else pb
                        o = pp[:, 0:260].rearrange("p (a b) -> p a b", a=2)[:, h % 2, 0:129]
                        mm(o, Sm[d][:, h, :], Vw[d][:, h, 0:129], True, False, [Sm[d].k, Vw[d].k], [pp.k])
                        if d == "f":
                            mm(o, mqT[:, h, tsl], CdF[:, h, 0:129], False, True, [mqT.k, CdF.k], [pp.k])
                        else:
                            mm(o, mqT[:, h, tsl], cl[:, h, 0:129], False, True, [mqT.k, cl.k], [pp.k])
                    yield
                    c4 = di * 4
                    for half, pp in enumerate((pa, pb)):
                        c0 = c4 + half * 2
                        den = pp[:, 0:260].rearrange("p (a b) -> p a b", a=2)[:, :, 128]
                        tt("vector", sc8[:, c0:c0 + 2], den, E8[:, c0:c0 + 2], ALU.mult, [pp.k, E8.k], [sc8.k])
                    ts("vector", sc8n[:, c4:c4 + 4], sc8[:, c4:c4 + 4], -1.0, None, ALU.mult, None, [sc8.k], [sc8n.k])
                    tt("vector", sc8[:, c4:c4 + 4], sc8[:, c4:c4 + 4], sc8n[:, c4:c4 + 4], ALU.max, [sc8.k, sc8n.k], [sc8.k])
                    ts("vector", sc8[:, c4:c4 + 4], sc8[:, c4:c4 + 4], 1.0, None, ALU.max, None, [sc8.k], [sc8.k])
                    recip(sc8[:, c4:c4 + 4], sc8[:, c4:c4 + 4], [sc8.k], [sc8.k])
                    tt("vector", sc8[:, c4:c4 + 4], sc8[:, c4:c4 + 4], E8[:, c4:c4 + 4], ALU.mult, [sc8.k, E8.k], [sc8.k])
                    yield
                    for h in range(4):
                        pp = pa if h < 2 else pb
                        src = pp[:, 0:260].rearrange("p (a b) -> p a b", a=2)[:, h % 2, 0:128]
                        if d == "f":
                            ts("vector", hs[:, h * 128:(h + 1) * 128], src, sc8[:, h:h + 1], None, ALU.mult, None, [pp.k, sc8.k], [hs.k])
                        else:
                            stt(hs[:, h * 128:(h + 1) * 128], src, sc8[:, 4 + h:5 + h], hs[:, h * 128:(h + 1) * 128], ALU.mult, ALU.add,
                                [pp.k, sc8.k, hs.k], [hs.k])
                    yield
                for h in range(4):
                    act(Sm["f"][:, h, :], hs[:, h * 128:(h + 1) * 128], AF.Square, [hs.k], [Sm["f"].k, hrs.k], accum=hrs[:, h:h + 1])
                act(hrs[:], hrs[:], AF.Ln, [hrs.k], [hrs.k], bias=EPS, scale=1.0 / 128)
                act(hrs[:], hrs[:], AF.Exp, [hrs.k], [hrs.k], scale=-0.5)
                yield
                tt("vector", hs[:].rearrange("p (h d) -> p h d", h=4), hs[:].rearrange("p (h d) -> p h d", h=4),
                   hrs[:].unsqueeze(2).to_broadcast([128, 4, 128]), ALU.mult, [hs.k, hrs.k], [hs.k])
                tt("vector", mixr[n % 2][:, 512:1024], hs[:], sig[:], ALU.mult, [hs.k, sig.k], ["mixM%d" % (n % 2)])
                yield

            def gen_tail(b, g, j):
                n = g * 4 + j
                row0 = b * SEQ + n * 128
                for kc in range(8):
                    tr(PT[:, kc * 128:(kc + 1) * 128], mixr[n % 2][:, kc * 128:(kc + 1) * 128], identb[:],
                       ["mixA%d" % (n % 2), "mixM%d" % (n % 2), identb.k], [PT.k])
                cp("scalar", mixT[:], PT[:].rearrange("p (a b) -> p a b", a=8), [PT.k], [mixT.k])
                xb = rr("xt", xt)
                dma("sync", xb[:], x_d[row0:row0 + 128, :], [], [xb.k], xb.k)
                xo = rr("xn", xn)
                yield
                for half in range(2):
                    p = nextps()
                    for kc in range(8):
                        mm(p[:], mixT[:, kc, :], wout[:, kc, half * 512:(half + 1) * 512], kc == 0, kc == 7, [mixT.k, wout.k], [p.k])
                    hsl = slice(half * 512, (half + 1) * 512)
                    tt("vector", xo[:, hsl], p[:], xb[:, hsl], ALU.add, [p.k, xb.k], [xo.k])
                    yield
                dma("sync", out_d[row0:row0 + 128, :], xo[:], [xo.k], ["outrows"], xo.k)
                yield
                s4 = rr("st", st4)
                hb2 = rr("h2b", h2b)
                act(hb2[:], xo[:], AF.Square, [xo.k], [hb2.k, s4.k], accum=s4[:, 0:1])
                act(s4[:, 1:2], s4[:, 0:1], AF.Ln, [s4.k], [s4.k], bias=EPS, scale=1.0 / D)
                act(s4[:, 2:3], s4[:, 1:2], AF.Exp, [s4.k], [s4.k], scale=-0.5)
                yield
                stt(t1[:], xo[:], s4[:, 2:3], A2[:], ALU.mult, ALU.mult, [xo.k, s4.k, A2.k], [t1.k])
                tt("vector", t1[:], t1[:], B2[:], ALU.add, [t1.k, B2.k], [t1.k])
                yield
                cp("scalar", hb2[:], h2[:], [h2.k], [hb2.k])
                dma("sync", hx2s_d[row0:row0 + 128, :], hb2[:], [hb2.k], ["hx2s"], hb2.k)
                for half in range(2):
                    p = nextps()
                    for q4 in range(4):
                        kc = half * 4 + q4
                        tr(p[:, q4 * 128:(q4 + 1) * 128], h2[:, kc * 128:(kc + 1) * 128], identf[:], [h2.k, identf.k], [p.k])
                    cp("scalar", h2T[:, half * 4:half * 4 + 4, :], p[:].rearrange("p (a b) -> p a b", a=4), [p.k], [h2T.k])
                    yield
                p = nextps()
                for kc in range(8):
                    mm(p[:, 0:16], h2T[:, kc, :], wr[:, kc, :], kc == 0, kc == 7, [h2T.k, wr.k], [p.k])
                S.op("vector", lambda e, p=p: e.tensor_reduce(out=rs1[:], in_=p[:, 0:16], axis=AX.X, op=ALU.max, negate=True),
                     reads=[p.k], writes=[rs1.k])
                act(rt[:], p[:, 0:16], AF.Exp, [p.k, rs1.k], [rt.k, rs2.k], bias=rs1[:, 0:1], accum=rs2[:, 0:1])
                yield
                recip(rs2[:], rs2[:], [rs2.k], [rs2.k])
                ts("vector", aff_all[:, n % (NT // 2), n // (NT // 2), b, :], rt[:], rs2[:, 0:1], None, ALU.mult, None, [rt.k, rs2.k], [(aff_all.k, n, b)])
                yield

            def p2_prep2(b, g):
                hb_ = HB[g % 2]
                hp = (NG + g - 1) if g > 0 else None
                hn = (g + 1) if g < NG - 1 else None
                feat_proj(C_MQ, 4, 512, mqT, 0, hp, hn, hb_)
                feat_proj(C_MK, 4, 512, mkT, 4, hp, hn, hb_)
                k_tokmajor(4)

            def p2_batch(b, ngroups):
                ntile = ngroups * 4
                if CFG.get("psv", 1) == 1:
                    al_A = RR(PB[0:2]); al_M = RR(PB[3:6]); al_T = RR([PB[2], PB[6]]); al_H = RR(PB[0:4])
                else:
                    al_A = RR(PB[0:3]); al_M = RR(PB[3:6]); al_T = RR(PB[6:7]); al_H = RR(PB[0:4])
                stage_a(xrows(b, 0), 4, A1, B1, HB[0])
                p2_prep2(b, 0)
                run([(gen_head(b, 0, 0), al_H)])
                for n in range(ntile):
                    g, j = n // 4, n % 4
                    gens = [(gen_mlstm(b, g, j), al_M, CFG.get("mw", 1)), (gen_attention(n), al_A)]
                    if n > 0:
                        gens.append((gen_tail(b, (n - 1) // 4, (n - 1) % 4), al_T))
                    if j == 0:
                        bg = []
                        if g + 1 < ngroups:
                            bg = [gen_stage_tile(xrows(b, g + 1), j_, A1, B1, HB[(g + 1) % 2]) for j_ in range(4)]
                    run(gens, bg)
                    if n + 1 < ntile:
                        if j == 3:
                            drain(bg)
                            p2_prep2(b, g + 1)
                        run([(gen_head(b, (n + 1) // 4, (n + 1) % 4), al_H)], bg)
                run([(gen_tail(b, (ntile - 1) // 4, (ntile - 1) % 4), al_T)])

            stop = CFG["stop"]
            for b in range(CFG["nb"]):
                if CFG.get("lm", 3) & 1:
                    load_mod(2, {"B1": B1} if CFG.get("lm", 3) & 4 else {"A1": A1, "B1": B1})
                if CFG.get("lm", 3) & 2:
                    mset("vector", Sst["f"][:], 0.0, [Sst["f"].k])
                    mset("vector", Sst["b"][:], 0.0, [Sst["b"].k])
                if stop == "mod":
                    continue
                p1_group_ctx(b)
                load_mod(b, {"A1": A1, "B1": B1, "A2": A2, "B2": B2})
                load_mod(b, {"G1": None})
                load_wout_scaled()
                if stop == "ctx":
                    continue
                p0(b)
                if stop == "p0":
                    continue
                g1s = list(range(NG - 1, -1, -1))
                if CFG["ng1"] is not None:
                    g1s = g1s[:CFG["ng1"]]
                stage_a(xrows(b, g1s[0]), 4, A1, B1, HB[g1s[0] % 2])
                for gi_, g in enumerate(g1s):
                    p1_group(b, g, g1s[gi_ + 1] if gi_ + 1 < len(g1s) else None)
                if stop == "p1":
                    continue
                p2_batch(b, NG if CFG["ng2"] is None else CFG["ng2"])
            S.emit()
        if CFG["stop"] in ("mod", "ctx", "p0", "p1", "p2"):
            return nc

        with ExitStack() as es:
            S = Sched(nc, "b")

            def sb(name, shape, dt=F32):
                return Buf(es.enter_context(nc.sbuf_tensor("z_" + name, list(shape), dt)), "z_" + name)

            def ps(name, shape, dt=F32):
                return Buf(es.enter_context(nc.psum_tensor(name, list(shape), dt)), name)

            PB = [ps("qb%d" % i, [128, 512]) for i in range(6)]
            PTS = [ps("qtb%d" % i, [128, 1024], BF16) for i in range(2)]
            pbi = [0]

            def nextps():
                p = PB[pbi[0] % 6]
                pbi[0] += 1
                return p

            G2 = [sb("G2_%d" % b, [128, D]) for b in range(NB)]
            for b in range(NB):
                S.op("sync", lambda e, b=b: e.dma_start(out=G2[b][:], in_=modsc_d[b, 5 * D:6 * D].partition_broadcast(128)),
                     writes=[G2[b].k], dma="prm2")
            S.group_keys.add("prm2")
            HSEQ = SEQ // 2
            affT = sb("affT", [64, HSEQ])
            mx = sb("mx", [64, CAP])
            ix = sb("ix", [64, CAP], U32)
            ixf = sb("ixf", [64, CAP])
            boff = sb("boff", [64, 1])
            antiI = sb("antiI", [128, 128])
            S.op("sync", lambda e: e.dma_start(out=boff[:], in_=boff_d), writes=[boff.k], dma="prm2")
            S.op("sync", lambda e: e.dma_start(out=antiI[:], in_=anti_d), writes=[antiI.k], dma="prm2")
            for m4 in range(4):
                p = nextps()
                for q in range(4):
                    m = m4 * 4 + q
                    S.op("tensor", lambda e, p=p, q=q, m=m: e.transpose(p[0:64, q * 128:(q + 1) * 128],
                                                                          aff_all[:, m, :, :, :].rearrange("p h b e -> p (h b e)"), identf[:]),
                         reads=[], writes=[p.k])
                S.op("vector", lambda e, p=p, m4=m4: e.tensor_copy(out=affT[:, m4 * 512:(m4 + 1) * 512], in_=p[0:64, :]),
                     reads=[p.k], writes=[affT.k])
            for k in range(CAP // 8):
                sl = slice(8 * k, 8 * k + 8)
                S.op("vector", lambda e, sl=sl: e.max(out=mx[:, sl], in_=affT[:]), reads=[affT.k], writes=[mx.k])
                S.op("vector", lambda e, sl=sl: e.max_index(out=ix[:, sl], in_max=mx[:, sl], in_values=affT[:]),
                     reads=[affT.k, mx.k], writes=[ix.k])
                if k < CAP // 8 - 1:
                    S.op("vector", lambda e, sl=sl: e.match_replace(out=affT[:], in_to_replace=mx[:, sl], in_values=affT[:], imm_value=-1.0),
                         reads=[affT.k, mx.k], writes=[affT.k])
            S.op("vector", lambda e: e.tensor_copy(out=ixf[:], in_=ix[:]), reads=[ix.k], writes=[ixf.k])
            S.op("vector", lambda e: e.tensor_scalar(out=ixf[:], in0=ixf[:], scalar1=boff[:, 0:1], scalar2=None, op0=ALU.add),
                 reads=[ixf.k, boff.k], writes=[ixf.k])
            idxT = sb("idxT", [128, 4, 32], I32)
            gateT = sb("gateT", [128, 4, 32])
            Tmx = sb("Tmx", [128, 4, 64])
            Tix = sb("Tix", [128, 4, 64])
            msk = sb("msk", [128, 4, 32])
            dif = sb("dif", [128, 4, 32])
            for (src, dst) in ((mx, Tmx), (ixf, Tix)):
                p = nextps()
                for q in range(4):
                    S.op("tensor", lambda e, p=p, q=q, src=src: e.transpose(p[:, q * 64:(q + 1) * 64], src[:, q * 128:(q + 1) * 128], identf[0:64, 0:64]),
                         reads=[src.k], writes=[p.k])
                S.op("vector", lambda e, p=p, dst=dst: e.tensor_copy(out=dst[:], in_=p[:, 0:256].rearrange("p (a b) -> p a b", a=4)),
                     reads=[p.k], writes=[dst.k])
            pB = nextps()
            for q in range(4):
                S.op("tensor", lambda e, q=q: e.matmul(pB[:, q * 64:q * 64 + 32], antiI[:], Tmx[:, 3 - q, 32:64], start=True, stop=True),
                     reads=[antiI.k, Tmx.k], writes=[pB.k])
                S.op("tensor", lambda e, q=q: e.matmul(pB[:, q * 64 + 32:q * 64 + 64], antiI[:], Tix[:, 3 - q, 32:64], start=True, stop=True),
                     reads=[antiI.k, Tix.k], writes=[pB.k])
            pB3 = pB[:, 0:256].rearrange("p (a b) -> p a b", a=4)
            S.op("vector", lambda e: e.tensor_tensor(out=msk[:], in0=Tmx[:, :, 0:32], in1=pB3[:, :, 0:32], op=ALU.is_ge),
                 reads=[Tmx.k, pB.k], writes=[msk.k])
            S.op("vector", lambda e: e.tensor_tensor(out=gateT[:], in0=Tmx[:, :, 0:32], in1=pB3[:, :, 0:32], op=ALU.max),
                 reads=[Tmx.k, pB.k], writes=[gateT.k])
            S.op("vector", lambda e: e.tensor_tensor(out=dif[:], in0=Tix[:, :, 0:32], in1=pB3[:, :, 32:64], op=ALU.subtract),
                 reads=[Tix.k, pB.k], writes=[dif.k])
            S.op("vector", lambda e: e.tensor_tensor(out=dif[:], in0=dif[:], in1=msk[:], op=ALU.mult),
                 reads=[dif.k, msk.k], writes=[dif.k])
            S.op("vector", lambda e: e.tensor_tensor(out=dif[:], in0=dif[:], in1=pB3[:, :, 32:64], op=ALU.add),
                 reads=[dif.k, pB.k], writes=[dif.k])
            S.op("vector", lambda e: e.tensor_copy(out=idxT[:], in_=dif[:]), reads=[dif.k], writes=[idxT.k])

            wgb = [sb("wg%d" % i, [128, 8, 512], BF16) for i in range(3)]
            wub = [sb("wu%d" % i, [128, 8, 512], BF16) for i in range(3)]
            wdb = [sb("wd%d" % i, [128, 4, D], BF16) for i in range(3)]
            xg = [sb("xg%d" % i, [128, D], BF16) for i in range(12)]
            xgTs = [sb("xgT%d" % i, [128, 8, 512], BF16) for i in range(2)]
            sgs = [sb("sg%d" % i, [128, 512]) for i in range(2)]
            hTe = sb("hTe", [128, 4, 512], BF16)
            ptc = [0]
            ys = [sb("ys%d" % i, [128, D]) for i in range(4)]

            stg = [sb("stg%d" % i, [128, 512]) for i in range(6)]
            stc = [0]

            def w_chunks(ex):
                wg_, wu_, wd_ = wgb[ex % 3], wub[ex % 3], wdb[ex % 3]
                out = []
                for kc in range(8):
                    out.append((wg_[:, kc, :], wg_.k, wg_d[ex, kc * 128:(kc + 1) * 128, :]))
                    out.append((wu_[:, kc, :], wu_.k, wu_d[ex, kc * 128:(kc + 1) * 128, :]))
                for fc in range(4):
                    for c0 in (0, 512):
                        out.append((wd_[:, fc, c0:c0 + 512], wd_.k, wd_d[ex, fc * 128:(fc + 1) * 128, c0:c0 + 512]))
                return out

            def load_chunk(ch):
                dst, dkey, src = ch
                st = stg[stc[0] % 6]
                eng = "scalar" if stc[0] % 2 == 0 else "gpsimd"
                stc[0] += 1
                S.op("sync", lambda e, st=st, src=src: e.dma_start(out=st[:], in_=src), writes=[st.k], dma=st.k)
                if eng == "scalar":
                    S.op("scalar", lambda e, st=st, dst=dst: e.copy(out=dst, in_=st[:]), reads=[st.k], writes=[dkey])
                else:
                    S.op("gpsimd", lambda e, st=st, dst=dst: e.tensor_copy(out=dst, in_=st[:]), reads=[st.k], writes=[dkey])

            def load_w(ex):
                for ch in w_chunks(ex):
                    load_chunk(ch)

            wstream = []
            for ex_ in range(2, NEXP):
                wstream.extend(w_chunks(ex_))

            def gen_wload(i):
                lo = 12 * (i - 1)
                for ch in wstream[lo:lo + 12] if i >= 1 else []:
                    load_chunk(ch)
                    yield

            items = [(ex, b) for ex in range(NEXP) for b in range(NB)]

            def gathers(i):
                ex, b = items[i]
                pe = b * 16 + ex
                for j in range(4):
                    xb = xg[(i % 3) * 4 + j]
                    S.op("gpsimd", lambda e, j=j, pe=pe, xb=xb: e.indirect_dma_start(
                        out=xb[:], out_offset=None, in_=hx2s_d,
                        in_offset=bass.IndirectOffsetOnAxis(ap=idxT[:, j, pe:pe + 1], axis=0)),
                        reads=[idxT.k], writes=[xb.k], dma=xb.k)

            def gen_trans(i):
                xgT = xgTs[i % 2]
                for j in range(4):
                    xb = xg[(i % 3) * 4 + j]
                    PT = PTS[ptc[0] % 2]
                    ptc[0] += 1
                    for kc in range(8):
                        S.op("tensor", lambda e, xb=xb, kc=kc, PT=PT: e.transpose(PT[:, kc * 128:(kc + 1) * 128], xb[:, kc * 128:(kc + 1) * 128], identb[:]),
                             reads=[xb.k], writes=[PT.k])
                    if j % 2 == 0:
                        S.op("scalar", lambda e, j=j, PT=PT, xgT=xgT: e.copy(out=xgT[:, :, j * 128:(j + 1) * 128], in_=PT[:].rearrange("p (a b) -> p a b", a=8)),
                             reads=[PT.k], writes=[xgT.k])
                    else:
                        S.op("vector", lambda e, j=j, PT=PT, xgT=xgT: e.tensor_copy(out=xgT[:, :, j * 128:(j + 1) * 128], in_=PT[:].rearrange("p (a b) -> p a b", a=8)),
                             reads=[PT.k], writes=[xgT.k])
                    yield

            def gen_ffn(i):
                ex, b = items[i]
                pe = b * 16 + ex
                xgT = xgTs[i % 2]
                wg_, wu_, wd_ = wgb[ex % 3], wub[ex % 3], wdb[ex % 3]
                for fc in range(4):
                    pg = nextps(); pu = nextps()
                    sg = sgs[fc % 2]
                    for kc in range(8):
                        S.op("tensor", lambda e, pg=pg, kc=kc, fc=fc: e.matmul(pg[:], wg_[:, kc, fc * 128:(fc + 1) * 128], xgT[:, kc, :],
                                                                             start=(kc == 0), stop=(kc == 7)),
                             reads=[wg_.k, xgT.k], writes=[pg.k])
                    for kc in range(8):
                        S.op("tensor", lambda e, pu=pu, kc=kc, fc=fc: e.matmul(pu[:], wu_[:, kc, fc * 128:(fc + 1) * 128], xgT[:, kc, :],
                                                                             start=(kc == 0), stop=(kc == 7)),
                             reads=[wu_.k, xgT.k], writes=[pu.k])
                    S.op("scalar", lambda e, pg=pg, sg=sg: e.activation(out=sg[:], in_=pg[:], func=AF.Silu), reads=[pg.k], writes=[sg.k])
                    S.op("vector", lambda e, pu=pu, fc=fc, sg=sg: e.tensor_tensor(out=hTe[:, fc, :], in0=pu[:], in1=sg[:], op=ALU.mult),
                         reads=[pu.k, sg.k], writes=[hTe.k])
                    yield
                for j in range(4):
                    for half in range(2):
                        p = nextps()
                        for fc in range(4):
                            S.op("tensor", lambda e, p=p, fc=fc, j=j, half=half: e.matmul(
                                p[:], hTe[:, fc, j * 128:(j + 1) * 128], wd_[:, fc, half * 512:(half + 1) * 512],
                                start=(fc == 0), stop=(fc == 3)), reads=[hTe.k, wd_.k], writes=[p.k])
                        S.op("vector", lambda e, p=p, j=j, half=half: e.scalar_tensor_tensor(
                            out=ys[j][:, half * 512:(half + 1) * 512], in0=p[:], scalar=gateT[:, j, pe:pe + 1],
                            in1=G2[b][:, half * 512:(half + 1) * 512], op0=ALU.mult, op1=ALU.mult),
                            reads=[p.k, gateT.k, G2[b].k], writes=[ys[j].k])
                    S.op("gpsimd", lambda e, j=j: e.indirect_dma_start(
                        out=out_d, out_offset=bass.IndirectOffsetOnAxis(ap=idxT[:, j, pe:pe + 1], axis=0),
                        in_=ys[j][:], in_offset=None, compute_op=ALU.add),
                        reads=[ys[j].k, idxT.k] + ["osc%d_%d_%d" % (b, (ex + 1) % 2, jj) for jj in range(4)],
                        writes=["osc%d_%d_%d" % (b, ex % 2, j)], dma=ys[j].k)
                    yield

            def run2(gens):
                live = list(gens)
                while live:
                    for g_ in list(live):
                        try:
                            next(g_)
                        except StopIteration:
                            live.remove(g_)

            load_w(0)
            load_w(1)
            gathers(0)
            gathers(1)
            run2([gen_trans(0)])
            for i in range(len(items)):
                ex, b = items[i]
                if i + 2 < len(items):
                    gathers(i + 2)
                if i + 1 < len(items):
                    run2([gen_ffn(i), gen_trans(i + 1), gen_wload(i)])
                else:
                    run2([gen_ffn(i), gen_wload(i)])
            S.emit()
    return nc


_CACHE = {}


def _consts():
    import ml_dtypes
    bf = ml_dtypes.bfloat16
    n = SEQ
    rows = n // 64
    row, col = np.meshgrid(np.arange(rows), np.arange(64), indexing="ij")
    n_freq = 16
    freqs = (10000.0 ** (-np.arange(n_freq, dtype=np.float32) / n_freq)).astype(np.float32)
    ang = np.concatenate([row.reshape(-1, 1).astype(np.float32) * freqs, col.reshape(-1, 1).astype(np.float32) * freqs], -1)
    s = np.arange(128)[:, None]
    t = np.arange(128)[None, :]
    c = {
        "rope_cos": np.cos(ang).astype(np.float32),
        "rope_sin": np.sin(ang).astype(np.float32),
        "ident_bf": np.eye(128, dtype=np.float32).astype(bf),
        "ident_f": np.eye(128, dtype=np.float32),
        "tri_f": (s > t).astype(np.float32),
        "tri_b": (s < t).astype(np.float32),
        "neg_ones": -np.ones((128, 128), np.float32),
        "mask_f": ((s <= t) * QS).astype(np.float32).astype(bf),
        "mask_b": ((s >= t) * QS).astype(np.float32).astype(bf),
        "amask_lo": (s >= t).astype(np.float32).astype(bf),
        "amask_hi": (s <= t).astype(np.float32).astype(bf),
        "boff": np.array([(p_ // 32) * (SEQ // 2) + ((p_ % 32) // 16) * SEQ for p_ in range(64)], np.float32).reshape(64, 1),
        "anti_ident": np.ascontiguousarray(np.eye(128, dtype=np.float32)[::-1]),
    }
    return c


def _perm_win(w_in):
    w = w_in
    aq = w[:, 0:512]
    ak = w[:, 512:640]
    av = w[:, 640:768]
    mq = w[:, 768:1280]
    mk = w[:, 1280:1792]
    mv = w[:, 1792:2304]
    mo = w[:, 2304:2816]
    gt = w[:, 2816:2832]
    heads = [aq[:, h * 64:(h + 1) * 64] for h in range(8)]
    aqp = np.concatenate([np.concatenate([heads[p], heads[4 + p]], 1) for p in range(4)], 1)
    return np.ascontiguousarray(np.concatenate([aqp, mo, ak, av, mv, gt, mq, mk], 1))


def kernel(x, c, ctx, c_ctx, w_mod, b_mod, norm1_w, norm2_w, w_in, b_gates, conv_qk, q_norm_w,
           k_norm_w, sink, mlstm_norm_w, w_out, w_router, w_gate, w_up, w_down):
    f = lambda a: np.ascontiguousarray(np.asarray(a, dtype=np.float32))
    x = f(x); c = f(c); ctx = f(ctx); c_ctx = f(c_ctx)
    if "nc" not in _CACHE:
        _CACHE["nc"] = build_program()
    nc = _CACHE["nc"]
    consts = _consts()
    shared = {
        "w_mod": f(w_mod)[0], "b_mod": f(b_mod), "norm1_w": f(norm1_w), "norm2_w": f(norm2_w),
        "w_in": _perm_win(f(w_in)[0]), "b_gates": f(b_gates), "conv_qk": f(conv_qk)[0],
        "q_norm_w": f(q_norm_w), "k_norm_w": f(k_norm_w), "sink": f(sink), "mlstm_norm_w": f(mlstm_norm_w),
        "w_out": f(w_out)[0], "w_router": f(w_router)[0], "w_gate": f(w_gate)[0], "w_up": f(w_up)[0],
        "w_down": f(w_down)[0],
    }
    shared.update(consts)
    if CFG["stop"] is not None:
        for k in ("w_gate", "w_up", "w_down"):
            shared.pop(k)
    in_maps = []
    for i in range(8):
        m = dict(shared)
        m["x"] = x[2 * i:2 * i + 2].reshape(NB * SEQ, D)
        m["ctx"] = ctx[2 * i:2 * i + 2].reshape(NB * CTX, D)
        m["cvec"] = np.ascontiguousarray(np.stack([c[2 * i], c[2 * i + 1], c_ctx], 0))
        in_maps.append(m)
    res = run_bass_kernel_spmd(nc, in_maps, core_ids=list(range(8)))
    _CACHE["last"] = res.results
    out = np.concatenate([np.asarray(r["out"]).reshape(NB, SEQ, D) for r in res.results], 0)
    return out.astype(np.float32)
```

```python
import numpy as np
from contextlib import ExitStack
import concourse.bass as bass
import concourse.mybir as mybir
from concourse.bass_utils import run_bass_kernel_spmd

F32 = mybir.dt.float32
BF16 = mybir.dt.bfloat16
I32 = mybir.dt.int32
U32 = mybir.dt.uint32
AF = mybir.ActivationFunctionType
ALU = mybir.AluOpType
AX = mybir.AxisListType

D = 1024
SEQ = 4096
NT = SEQ // 128
NG = SEQ // 512
CTX = 256
NB = 2
EPS = 1e-6
NEXP = 16
CAP = 512
QS = 128 ** -0.5
C_AQ, C_MO, C_AK, C_AV, C_MV, C_GT, C_MQ, C_MK = 0, 512, 1024, 1152, 1280, 1792, 1808, 2320
SEM_CHUNK = 16000
CFG = {"stop": None, "nb": NB, "ng1": None, "ng2": None}


class Sched:
    ENGS = ("tensor", "vector", "scalar", "gpsimd", "sync")

    def __init__(self, nc, tag):
        self.nc = nc
        self.tag = tag
        self.ops = []
        self.last_writer = {}
        self.readers = {}
        self.eng_count = {e: 0 for e in self.ENGS}
        self.dma_count = {}
        self.sem_names = set()
        self.group_keys = set()

    def _event_compute(self, eng):
        c = self.eng_count[eng]
        self.eng_count[eng] = c + 1
        name = "%sp_%s_%d" % (self.tag, eng, c // SEM_CHUNK)
        self.sem_names.add(name)
        return (name, (c % SEM_CHUNK) + 1, 1)

    def _event_dma(self, key):
        c = self.dma_count.get(key, 0)
        self.dma_count[key] = c + 1
        per = SEM_CHUNK // 16
        name = "%sd_%s_%d" % (self.tag, key, c // per)
        self.sem_names.add(name)
        return (name, ((c % per) + 1) * 16, 16)

    def op(self, eng, fn, reads=(), writes=(), dma=None):
        def expand(keys):
            out = []
            for k in keys:
                if isinstance(k, tuple) and len(k) > 0 and k[0] == "MULTI":
                    out.extend(k[1:])
                else:
                    out.append(k)
            return out
        reads = expand(reads)
        writes = expand(writes)
        waits = {}

        def need(ev):
            if ev is None:
                return
            n, v, _ = ev
            if waits.get(n, 0) < v:
                waits[n] = v

        for r in reads:
            need(self.last_writer.get(r))
        for w in writes:
            need(self.last_writer.get(w))
            for ev in self.readers.get(w, ()):
                need(ev)
        ev = self._event_dma(dma) if dma is not None else self._event_compute(eng)
        for r in reads:
            self.readers.setdefault(r, []).append(ev)
        for w in writes:
            self.last_writer[w] = ev
            self.readers[w] = []
        self.ops.append((eng, fn, waits, ev))

    def emit(self):
        nc = self.nc
        with ExitStack() as es:
            sems = {}
            for n in sorted(self.sem_names):
                sems[n] = es.enter_context(nc.semaphore(n))
            block = es.enter_context(nc.Block())
            final_waits = {}
            for (eng, fn, waits, ev) in self.ops:
                n, v, _ = ev
                if final_waits.get(n, 0) < v:
                    final_waits[n] = v
            gnames = set("%sd_%s_0" % (self.tag, k) for k in self.group_keys) if CFG.get("gk", 1) else set()

            def make(engname):
                def body(e):
                    seen = {}
                    for (eng, fn, waits, ev) in self.ops:
                        if eng != engname:
                            continue
                        for n, v in waits.items():
                            if engname == "tensor" and "p_tensor_" in n:
                                continue
                            if n in gnames:
                                v = final_waits[n]
                            if seen.get(n, 0) >= v:
                                continue
                            e.wait_ge(sems[n], v)
                            seen[n] = v
                        ins = fn(e)
                        ins.then_inc(sems[ev[0]], ev[2])
                    if engname == "sync":
                        for n, v in final_waits.items():
                            if seen.get(n, 0) >= v:
                                continue
                            e.wait_ge(sems[n], v)
                return body

            block.tensor(make("tensor"))
            block.vector(make("vector"))
            block.scalar(make("scalar"))
            block.gpsimd(make("gpsimd"))
            block.sync(make("sync"))


class Buf:
    def __init__(self, t, k):
        self.t = t
        self.k = k

    def __getitem__(self, idx):
        return self.t[idx]


def build_program():
    nc = bass.Bass("TRN2", target_bir_lowering=False)

    def din(name, shape, dt=F32):
        return nc.dram_tensor(name, list(shape), dt, kind="ExternalInput").ap()

    x_d = din("x", [NB * SEQ, D])
    ctx_d = din("ctx", [NB * CTX, D])
    cvec_d = din("cvec", [3, D])
    wmod_d = din("w_mod", [D, 6 * D])
    bmod_d = din("b_mod", [1, 6 * D])
    n1_d = din("norm1_w", [1, D])
    n2_d = din("norm2_w", [1, D])
    win_d = din("w_in", [D, 2832])
    bg_d = din("b_gates", [1, 16])
    conv_d = din("conv_qk", [3, D])
    qn_d = din("q_norm_w", [1, 64])
    kn_d = din("k_norm_w", [1, 64])
    sink_d = din("sink", [1, 8])
    mn_d = din("mlstm_norm_w", [1, 512])
    wout_d = din("w_out", [D, D])
    wr_d = din("w_router", [D, NEXP])
    if CFG["stop"] is None:
        wg_d = din("w_gate", [NEXP, D, 512])
        wu_d = din("w_up", [NEXP, D, 512])
        wd_d = din("w_down", [NEXP, 512, D])
    cos_d = din("rope_cos", [SEQ, 32])
    sin_d = din("rope_sin", [SEQ, 32])
    idb_d = din("ident_bf", [128, 128], BF16)
    idf_d = din("ident_f", [128, 128])
    trif_d = din("tri_f", [128, 128])
    trib_d = din("tri_b", [128, 128])
    nones_d = din("neg_ones", [128, 128])
    mskf_d = din("mask_f", [128, 128], BF16)
    mskb_d = din("mask_b", [128, 128], BF16)
    amlo_d = din("amask_lo", [128, 128], BF16)
    amhi_d = din("amask_hi", [128, 128], BF16)
    boff_d = din("boff", [64, 1])
    anti_d = din("anti_ident", [128, 128])
    out_d = nc.dram_tensor("out", [NB * SEQ, D], F32, kind="ExternalOutput").ap()
    hx2s_d = nc.dram_tensor("hx2s", [NB * SEQ, D], BF16).ap()
    modsc_d = nc.dram_tensor("modsc", [3, 6 * D], F32).ap()
    cdb_d = nc.dram_tensor("cdbs", [NT, 128, 4, 130], BF16).ap()

    with ExitStack() as es0:
        def sb0(name, shape, dt=F32):
            return Buf(es0.enter_context(nc.sbuf_tensor(name, list(shape), dt)), name)

        aff_all = sb0("aff_all", [128, NT // 2, 2, NB, NEXP])
        identb = sb0("identb", [128, 128], BF16)
        identf = sb0("identf", [128, 128])

        with ExitStack() as es:
            S = Sched(nc, "m")

            def sb(name, shape, dt=F32):
                return Buf(es.enter_context(nc.sbuf_tensor(name, list(shape), dt)), name)

            def ps(name, shape, dt=F32):
                return Buf(es.enter_context(nc.psum_tensor(name, list(shape), dt)), name)

            PB = [ps("mb%d" % i, [128, 512]) for i in range(4)]
            pbi = [0]

            def nextps():
                p = PB[pbi[0] % 4]
                pbi[0] += 1
                return p

            def dma(eng, out, in_, r, w, key, **kw):
                S.op(eng, lambda e: e.dma_start(out=out, in_=in_, **kw), reads=r, writes=w, dma=key)

            def mm(out, lhsT, rhs, start, stop, r, w):
                S.op("tensor", lambda e: e.matmul(out, lhsT, rhs, start=start, stop=stop), reads=r, writes=w)

            def tr(out, in_, ident, r, w):
                S.op("tensor", lambda e: e.transpose(out, in_, ident), reads=r, writes=w)

            def tt(eng, out, in0, in1, op, r, w):
                S.op(eng, lambda e: e.tensor_tensor(out=out, in0=in0, in1=in1, op=op), reads=r, writes=w)

            def cp(eng, out, in_, r, w):
                S.op(eng, lambda e: e.tensor_copy(out=out, in_=in_), reads=r, writes=w)

            PK = "prm0"
            S.group_keys.add(PK)

            def load_const(buf, src, eng="sync", **kw):
                dma(eng, buf[:], src, [], [buf.k], PK, **kw)

            load_const(identb, idb_d)
            load_const(identf, idf_d)
            c3 = sb("c3", [3, D]); load_const(c3, cvec_d)
            S.op("scalar", lambda e: e.activation(out=c3[:], in_=c3[:], func=AF.Silu), reads=[c3.k], writes=[c3.k])
            sT = sb("sT", [128, 8, 3])
            p = nextps()
            for kc in range(8):
                tr(p[:, kc * 4:kc * 4 + 3], c3[:, kc * 128:(kc + 1) * 128], identf[0:3, 0:3], [c3.k, identf.k], [p.k])
            cp("vector", sT[:], p[:, 0:32].rearrange("p (a b) -> p a b", a=8)[:, :, 0:3], [p.k], [sT.k])
            bm3 = sb("bm3", [3, 6 * D]); load_const(bm3, bmod_d[0].partition_broadcast(3))
            modrows = sb("modrows", [3, 6 * D])
            wmc = [sb("wmc%d" % i, [128, 8, 512]) for i in range(4)]
            for ci in range(12):
                wb = wmc[ci % 4]
                dma("sync", wb[:], wmod_d.rearrange("(c p) n -> p c n", p=128)[:, :, ci * 512:(ci + 1) * 512], [], [wb.k], wb.k)
                p = nextps()
                for kc in range(8):
                    mm(p[0:3, :], sT[:, kc, :], wb[:, kc, :], kc == 0, kc == 7, [sT.k, wb.k], [p.k])
                tt("vector", modrows[:, ci * 512:(ci + 1) * 512], p[0:3, :], bm3[:, ci * 512:(ci + 1) * 512], ALU.add,
                   [p.k, bm3.k], [modrows.k])
            dma("sync", modsc_d, modrows[:], [modrows.k], ["modsc"], modrows.k)

            S.emit()

        if CFG["stop"] == "mod0":
            return nc
        with ExitStack() as es:
            S = Sched(nc, "a")

            def sb(name, shape, dt=F32):
                return Buf(es.enter_context(nc.sbuf_tensor(name, list(shape), dt)), name)

            def ps(name, shape, dt=F32):
                return Buf(es.enter_context(nc.psum_tensor(name, list(shape), dt)), name)

            PB = [ps("pb%d" % i, [128, 512]) for i in range(7)]
            PT = ps("ptb", [128, 1024], BF16)
            class RR:
                def __init__(self, banks):
                    self.b = banks
                    self.i = 0

                def next(self):
                    p = self.b[self.i % len(self.b)]
                    self.i += 1
                    return p

            al_all = RR(PB)
            cur_al = [al_all]

            def nextps():
                return cur_al[0].next()

            def run(pairs, bg=None):
                live = list(pairs)
                while live:
                    for item in list(live):
                        cur_al[0] = item[1]
                        try:
                            next(item[0])
                        except StopIteration:
                            live.remove(item)
                    if bg:
                        cur_al[0] = al_all
                        try:
                            next(bg[0])
                        except StopIteration:
                            bg.pop(0)
                cur_al[0] = al_all

            def drain(bg):
                cur_al[0] = al_all
                while bg:
                    try:
                        next(bg[0])
                    except StopIteration:
                        bg.pop(0)

            def dma(eng, out, in_, r, w, key, **kw):
                S.op(eng, lambda e: e.dma_start(out=out, in_=in_, **kw), reads=r, writes=w, dma=key)

            def mm(out, lhsT, rhs, start, stop, r, w):
                S.op("tensor", lambda e: e.matmul(out, lhsT, rhs, start=start, stop=stop), reads=r, writes=w)

            def tr(out, in_, ident, r, w):
                S.op("tensor", lambda e: e.transpose(out, in_, ident), reads=r, writes=w)

            def act(out, in_, func, r, w, bias=None, scale=None, accum=None):
                kw = {}
                if bias is not None:
                    kw["bias"] = bias
                if scale is not None:
                    kw["scale"] = scale
                if accum is not None:
                    kw["accum_out"] = accum
                S.op("scalar", lambda e: e.activation(out=out, in_=in_, func=func, **kw), reads=r, writes=w)

            def tt(eng, out, in0, in1, op, r, w):
                S.op(eng, lambda e: e.tensor_tensor(out=out, in0=in0, in1=in1, op=op), reads=r, writes=w)

            def ts(eng, out, in0, s1, s2, op0, op1, r, w):
                if op1 is None:
                    S.op(eng, lambda e: e.tensor_scalar(out=out, in0=in0, scalar1=s1, scalar2=None, op0=op0), reads=r, writes=w)
                else:
                    S.op(eng, lambda e: e.tensor_scalar(out=out, in0=in0, scalar1=s1, scalar2=s2, op0=op0, op1=op1), reads=r, writes=w)

            def stt(out, in0, scalar, in1, op0, op1, r, w):
                S.op("vector", lambda e: e.scalar_tensor_tensor(out=out, in0=in0, scalar=scalar, in1=in1, op0=op0, op1=op1), reads=r, writes=w)

            def cp(eng, out, in_, r, w):
                if eng == "scalar":
                    S.op(eng, lambda e: e.copy(out=out, in_=in_), reads=r, writes=w)
                else:
                    S.op(eng, lambda e: e.tensor_copy(out=out, in_=in_), reads=r, writes=w)

            def red(out, in_, r, w, op=ALU.add):
                S.op("vector", lambda e: e.tensor_reduce(out=out, in_=in_, axis=AX.X, op=op), reads=r, writes=w)

            def recip(out, in_, r, w):
                S.op("vector", lambda e: e.reciprocal(out=out, in_=in_), reads=r, writes=w)

            def mset(eng, out, val, w):
                S.op(eng, lambda e: e.memset(out, val), writes=w)

            DBG = {}

            def dump(name, buf, ap, shape, dt=F32):
                if not CFG.get("dbg") or name in DBG:
                    return
                d = nc.dram_tensor("dbg_" + name, list(shape), dt, kind="ExternalOutput").ap()
                DBG[name] = d
                dma("sync", d, ap, [buf.k], ["dbg_" + name], "dbgk")

            PK = "prm"
            S.group_keys.update([PK])
            def load_const(buf, src, eng="sync", **kw):
                dma(eng, buf[:], src, [], [buf.k], PK, **kw)

            trif = sb("trif", [128, 128]); load_const(trif, trif_d)
            trib = sb("trib", [128, 128]); load_const(trib, trib_d)
            nones = sb("nones", [128, 128]); load_const(nones, nones_d)
            mskf = sb("mskf", [128, 128], BF16); load_const(mskf, mskf_d)
            mskb = sb("mskb", [128, 128], BF16); load_const(mskb, mskb_d)
            amlo = sb("amlo", [128, 128], BF16); load_const(amlo, amlo_d)
            amhi = sb("amhi", [128, 128], BF16); load_const(amhi, amhi_d)
            cosT = sb("cosT", [128, NT, 32]); load_const(cosT, cos_d.rearrange("(n p) f -> p n f", p=128))
            sinT = sb("sinT", [128, NT, 32]); load_const(sinT, sin_d.rearrange("(n p) f -> p n f", p=128))
            bgB = sb("bgB", [128, 16]); load_const(bgB, bg_d[0].partition_broadcast(128))
            qnB = sb("qnB", [128, 64]); load_const(qnB, qn_d[0].partition_broadcast(128))
            knB = sb("knB", [128, 64]); load_const(knB, kn_d[0].partition_broadcast(128))
            mnB = sb("mnB", [128, 512]); load_const(mnB, mn_d[0].partition_broadcast(128))
            esink = sb("esink", [128, 8]); load_const(esink, sink_d[0].partition_broadcast(128))
            act(esink[:], esink[:], AF.Exp, [esink.k], [esink.k])
            cwT = sb("cwT", [128, 8, 3])
            for jj in range(3):
                S.op("sync", lambda e, jj=jj: e.dma_start(out=cwT[:, :, jj], in_=conv_d[jj].rearrange("(c p) -> p c", p=128),
                                                         allow_slow_non_contiguous=True), writes=[cwT.k], dma="cwk")
            if CFG["stop"] == "s1":
                S.emit()
                return nc
            wr = sb("wr", [128, 8, NEXP]); load_const(wr, wr_d.rearrange("(c p) n -> p c n", p=128))
            win = sb("win", [128, 8, 2832], BF16)
            hTB = sb("hTB", [128, 8, 512], BF16)
            hTB.k = ("MULTI",) + tuple(("hTB", i_, j_) for i_ in range(4) for j_ in range(4))
            hTBf = hTB[:].rearrange("p a b -> p (a b)").bitcast(F32)
            stg_ap = [hTBf[:, i_ * 512:(i_ + 1) * 512] for i_ in range(4)]
            stg_k = [("MULTI",) + tuple(("hTB", i_, j_) for j_ in range(4)) for i_ in range(4)]
            stc1 = [0]
            for kc in range(8):
                for c0 in range(0, 2832, 512):
                    w_ = min(512, 2832 - c0)
                    si = stc1[0] % 4
                    eng = ("scalar", "gpsimd", "vector")[stc1[0] % 3]
                    stc1[0] += 1
                    dma("sync", stg_ap[si][:, 0:w_], win_d[kc * 128:(kc + 1) * 128, c0:c0 + w_], [], [stg_k[si]], "stg%d" % si)
                    cp(eng, win[:, kc, c0:c0 + w_], stg_ap[si][:, 0:w_], [stg_k[si]], [win.k])
            wout = sb("wout", [128, 8, D], BF16)

            def load_wout_scaled():
                for kc in range(8):
                    for c0 in range(0, D, 512):
                        si = stc1[0] % 4
                        stc1[0] += 1
                        dma("sync", stg_ap[si], wout_d[kc * 128:(kc + 1) * 128, c0:c0 + 512], [], [stg_k[si]], "stg%d" % si)
                        tt("vector", wout[:, kc, c0:c0 + 512], stg_ap[si], t1[:, c0:c0 + 512], ALU.mult, [stg_k[si], t1.k], [wout.k])

            if CFG["stop"] == "s2":
                S.emit()
                return nc
            A1 = sb("A1", [128, D]); B1 = sb("B1", [128, D])
            A2 = sb("A2", [128, D]); B2 = sb("B2", [128, D])
            t1 = sb("t1", [128, D])
            t1.k = ("MULTI", "t1a", "t1b")

            def load_mod(row, want):
                def bc(i):
                    return modsc_d[row, i * D:(i + 1) * D].partition_broadcast(128)
                if "A1" in want:
                    dma("sync", t1[:], bc(1), [], [t1.k], "t1d")
                    dma("sync", A1[:], n1_d[0].partition_broadcast(128), [], [A1.k], A1.k)
                    stt(A1[:], t1[:], 1.0, A1[:], ALU.add, ALU.mult, [t1.k, A1.k], [A1.k])
                if "B1" in want:
                    dma("sync", B1[:], bc(0), [], [B1.k], B1.k)
                if "G1" in want:
                    dma("sync", t1[:], bc(2), [], [t1.k], "t1d")
                if "A2" in want:
                    dma("sync", t1[:], bc(4), [], [t1.k], "t1d")
                    dma("sync", A2[:], n2_d[0].partition_broadcast(128), [], [A2.k], A2.k)
                    stt(A2[:], t1[:], 1.0, A2[:], ALU.add, ALU.mult, [t1.k, A2.k], [A2.k])
                if "B2" in want:
                    dma("sync", B2[:], bc(3), [], [B2.k], B2.k)

            xt = [sb("xt%d" % i, [128, D]) for i in range(2)]
            hx = [sb("hx%d" % i, [128, D], BF16) for i in range(2)]
            hT = sb("hT", [128, 8, 512], BF16)
            hT.k = ("MULTI",) + tuple(("hT", j_) for j_ in range(4))
            HB = [hT, hTB]

            def tile_keys(hb_, j_):
                if hb_ is hT:
                    return [("hT", j_)]
                return [("hTB", i_, j_) for i_ in range(4)]
            st4 = [sb("st4_%d" % i, [128, 4]) for i in range(2)]
            HS = sb("HS", [128, 8, 2 * NG])
            KT = sb("KT", [128, SEQ + CTX], BF16)
            V = sb("V", [128, NT + 2, 2, 65], BF16)
            KT.k = ("MULTI",) + tuple(("KT", i_) for i_ in range(NT + 2))
            V.k = ("MULTI",) + tuple(("V", i_) for i_ in range(NT + 2))
            mset("vector", V[:], 1.0, [V.k])
            CdS = [sb("CdS%d" % i, [128, 4, 130], BF16) for i in range(2)]
            CdL = [sb("CdL%d" % i, [128, 4, 130], BF16) for i in range(2)]
            Sst = {"f": sb("Sf", [128, 4, 130]), "b": sb("Sb", [128, 4, 130])}
            Sdec = sb("Sdec", [128, 4, 130])
            CdF = sb("CdF", [128, 4, 130], BF16)
            mqT = sb("mqT", [128, 4, 512], BF16)
            mkT = sb("mkT", [128, 4, 512], BF16)
            mqT.k = ("MULTI",) + tuple(("mqT", c_) for c_ in range(4))
            mkT.k = ("MULTI",) + tuple(("mkT", c_) for c_ in range(4))
            Kt = sb("Kt", [128, 4, 512], BF16)
            qk_sq = sb("qk_sq", [128, 512])
            qk_t = sb("qk_t", [128, 512])
            qk_a = sb("qk_a", [128, 256])
            qk_b = sb("qk_b", [128, 256])
            qk_o = sb("qk_o", [128, 512], BF16)
            qk_ss = sb("qk_ss", [128, 10])
            QT = sb("QT", [128, 4, 128], BF16)
            PTs = sb("PTs", [128, 5, 512], BF16)
            arec = sb("arec", [128, 4])
            hrs = sb("hrs", [128, 4])
            mixr = [sb("mix%d" % i, [128, D], BF16) for i in range(2)]
            cv = Buf(t1.t, t1.k)
            mixT = sb("mixT", [128, 8, 128], BF16)
            mvs = sb("mvs", [128, 512])
            sig = sb("sig", [128, 512])
            gt = sb("gt", [128, 16])
            l1 = sb("l1", [128, 8])
            g8 = sb("g8", [128, 8])
            ws8 = sb("ws8", [128, 8])
            E8 = sb("E8", [128, 8])
            dec4 = sb("dec4", [128, 4])
            Vw = {"f": sb("Vwf", [128, 4, 130], BF16), "b": sb("Vwb", [128, 4, 130], BF16)}
            Sm = {"f": sb("Smf", [128, 4, 128], BF16), "b": sb("Smb", [128, 4, 128], BF16)}
            sc8 = sb("sc8", [128, 8])
            sc8n = sb("sc8n", [128, 8])
            hs = sb("hs", [128, 512])
            xn = [sb("xn%d" % i, [128, D]) for i in range(1)]
            h2 = t1
            h2b = [sb("h2b%d" % i, [128, D], BF16) for i in range(1)]
            h2T = sb("h2T", [128, 8, 128])
            rt = sb("rt", [128, 16])
            rs1 = sb("rs1", [128, 1])
            rs2 = sb("rs2", [128, 1])
            if CFG["stop"] == "s3":
                S.emit()
                return nc
            ring = {"xt": 0, "hx": 0, "xn": 0, "h2b": 0, "st": 0, "cds": 0, "cdl": 0}

            def rr(name, lst):
                b = lst[ring[name] % len(lst)]
                ring[name] += 1
                return b

            def norm_mod(src_ap_rows, npart, A, Bm, out_buf, xbufs, xname):
                xb = rr(xname, xbufs)
                dma("sync", xb[0:npart, :], src_ap_rows, [], [xb.k], xb.k)
                s4 = rr("st", st4)
                act(out_buf[0:npart, :], xb[0:npart, :], AF.Square, [xb.k], [out_buf.k, s4.k], accum=s4[0:npart, 0:1])
                act(s4[0:npart, 1:2], s4[0:npart, 0:1], AF.Ln, [s4.k], [s4.k], bias=EPS, scale=1.0 / D)
                act(s4[0:npart, 2:3], s4[0:npart, 1:2], AF.Exp, [s4.k], [s4.k], scale=-0.5)
                stt(out_buf[0:npart, :], xb[0:npart, :], s4[0:npart, 2:3], A[0:npart, :], ALU.mult, ALU.mult,
                    [xb.k, s4.k, A.k], [out_buf.k])
                tt("vector", out_buf[0:npart, :], out_buf[0:npart, :], Bm[0:npart, :], ALU.add, [out_buf.k, Bm.k], [out_buf.k])
                return xb

            def gen_stage_tile(src_rows_fn, j, A, Bm, hb_=None):
                hb_ = hT if hb_ is None else hb_
                hb = rr("hx", hx)
                xb = rr("xt", xt)
                dma("sync", xb[:], src_rows_fn(j), [], [xb.k], xb.k)
                s4 = rr("st", st4)
                yield
                act(hb[:], xb[:], AF.Square, [xb.k], [hb.k, s4.k], accum=s4[:, 0:1])
                act(s4[:, 1:2], s4[:, 0:1], AF.Ln, [s4.k], [s4.k], bias=EPS, scale=1.0 / D)
                act(s4[:, 2:3], s4[:, 1:2], AF.Exp, [s4.k], [s4.k], scale=-0.5)
                yield
                stt(hb[:], xb[:], s4[:, 2:3], A[:], ALU.mult, ALU.mult, [xb.k, s4.k, A.k], [hb.k])
                yield
                tt("vector", hb[:], hb[:], Bm[:], ALU.add, [hb.k, Bm.k], [hb.k])
                yield
                for kc in range(8):
                    tr(PT[:, kc * 128:(kc + 1) * 128], hb[:, kc * 128:(kc + 1) * 128], identb[:], [hb.k, identb.k], [PT.k])
                cp("scalar", hb_[:, :, j * 128:(j + 1) * 128], PT[:].rearrange("p (a b) -> p a b", a=8), [PT.k], tile_keys(hb_, j))
                yield

            def stage_a(src_rows_fn, ntile, A, Bm, hb_=None):
                for j0 in range(0, ntile, 2):
                    run([(gen_stage_tile(src_rows_fn, j, A, Bm, hb_), al_all) for j in range(j0, min(j0 + 2, ntile))])

            def gen_feat_chunk(col0, cc, N, dst, cw0, hprev, hnext, hb_=None):
                hb_ = hT if hb_ is None else hb_
                HTK = [hb_.k]
                p = nextps()
                base = (cc % 2) * 512
                ck = "t1a" if cc % 2 == 0 else "t1b"
                cvv = lambda a_, b_: t1[:, base + a_:base + b_]
                for kc in range(8):
                    mm(p[:, 0:N], win[:, kc, col0 + cc * 128: col0 + (cc + 1) * 128], hb_[:, kc, 0:N], kc == 0, kc == 7,
                       [win.k] + HTK, [p.k])
                yield
                act(cvv(0, N), p[:, 0:N], AF.Identity, [p.k, cwT.k], [ck], scale=cwT[:, cw0 + cc, 1:2])
                yield
                stt(cvv(1, N), p[:, 0:N - 1], cwT[:, cw0 + cc, 0:1], cvv(1, N), ALU.mult, ALU.add, [p.k, cwT.k, ck], [ck])
                stt(cvv(0, N - 1), p[:, 1:N], cwT[:, cw0 + cc, 2:3], cvv(0, N - 1), ALU.mult, ALU.add, [p.k, cwT.k, ck], [ck])
                if hprev is not None:
                    stt(cvv(0, 1), HS[:, cw0 + cc, hprev:hprev + 1], cwT[:, cw0 + cc, 0:1], cvv(0, 1), ALU.mult, ALU.add,
                        [HS.k, cwT.k, ck], [ck])
                if hnext is not None:
                    stt(cvv(N - 1, N), HS[:, cw0 + cc, hnext:hnext + 1], cwT[:, cw0 + cc, 2:3], cvv(N - 1, N), ALU.mult, ALU.add,
                        [HS.k, cwT.k, ck], [ck])
                yield
                act(dst[:, cc, 0:N], cvv(0, N), AF.Silu, [ck], [dst.k[1 + cc]])
                yield

            def feat_proj(col0, nchunks, N, dst, cw0, hprev, hnext, hb_=None):
                for c0 in range(0, nchunks, 2):
                    run([(gen_feat_chunk(col0, cc, N, dst, cw0, hprev, hnext, hb_), al_all) for cc in range(c0, c0 + 2)])

            def k_tokmajor(ntile):
                for j in range(ntile):
                    for h in range(4):
                        tr(PT[:, h * 128:(h + 1) * 128], mkT[:, h, j * 128:(j + 1) * 128], identb[:], [mkT.k, identb.k], [PT.k])
                    cp("vector", Kt[:, j, :], PT[:, 0:512], [PT.k], [Kt.k])

            def qk_norm_rope(src, srck, H, wB, tile_n, out_ap):
                W = H * 64
                s3 = lambda a: a.rearrange("p (h d) -> p h d", h=H)
                act(qk_sq[:, 0:W], src, AF.Square, [srck], [qk_sq.k])
                red(qk_ss[:, 0:H], s3(qk_sq[:, 0:W]), [qk_sq.k], [qk_ss.k])
                act(qk_ss[:, 0:H], qk_ss[:, 0:H], AF.Ln, [qk_ss.k], [qk_ss.k], bias=EPS, scale=1.0 / 64)
                act(qk_ss[:, 0:H], qk_ss[:, 0:H], AF.Exp, [qk_ss.k], [qk_ss.k], scale=-0.5)
                tt("vector", s3(qk_t[:, 0:W]), s3(src), qk_ss[:, 0:H].unsqueeze(2).to_broadcast([128, H, 64]), ALU.mult,
                   [srck, qk_ss.k], [qk_t.k])
                wbc = wB[:, :].unsqueeze(1).to_broadcast([128, H, 64])
                if tile_n is None:
                    tt("gpsimd", s3(out_ap), s3(qk_t[:, 0:W]), wbc, ALU.mult, [qk_t.k, wB.k], [out_ap.tensor.name if False else "qk_o"])
                    return
                tt("gpsimd", s3(qk_t[:, 0:W]), s3(qk_t[:, 0:W]), wbc, ALU.mult, [qk_t.k, wB.k], [qk_t.k])
                x1 = s3(qk_t[:, 0:W])[:, :, 0:32]
                x2 = s3(qk_t[:, 0:W])[:, :, 32:64]
                cb = cosT[:, tile_n, :].unsqueeze(1).to_broadcast([128, H, 32])
                sbn = sinT[:, tile_n, :].unsqueeze(1).to_broadcast([128, H, 32])
                h3 = lambda a: a.rearrange("p (h d) -> p h d", h=H)
                a_ = h3(qk_a[:, 0:H * 32]); b_ = h3(qk_b[:, 0:H * 32])
                o3 = s3(out_ap)
                tt("vector", a_, x1, cb, ALU.mult, [qk_t.k, cosT.k], [qk_a.k])
                tt("gpsimd", b_, x2, sbn, ALU.mult, [qk_t.k, sinT.k], [qk_b.k])
                tt("vector", o3[:, :, 0:32], a_, b_, ALU.subtract, [qk_a.k, qk_b.k], ["qk_o"])
                tt("vector", a_, x2, cb, ALU.mult, [qk_t.k, cosT.k, "qk_o"], [qk_a.k])
                tt("gpsimd", b_, x1, sbn, ALU.mult, [qk_t.k, sinT.k, "qk_o"], [qk_b.k])
                tt("vector", o3[:, :, 32:64], a_, b_, ALU.add, [qk_a.k, qk_b.k], ["qk_o"])

            def gates_prep(psg_ap, psgk):
                tt("vector", gt[:], psg_ap, bgB[:], ALU.add, [psgk, bgB.k], [gt.k])
                g3 = gt[:].rearrange("p (a b) -> p a b", a=2)
                act(l1[:].rearrange("p (a b) -> p a b", a=2), g3[:, :, 4:8], AF.Exp, [gt.k], [l1.k], scale=-1.0)
                act(l1[:], l1[:], AF.Ln, [l1.k], [l1.k], bias=1.0)
                cp("gpsimd", g8[:].rearrange("p (a b) -> p a b", a=2), g3[:, :, 0:4], [gt.k], [g8.k])

            def chunk_weights(dirs, need_tot):
                p = nextps()
                if "f" in dirs:
                    mm(p[:, 0:4], trif[:], l1[:, 0:4], True, True, [trif.k, l1.k], [p.k])
                if "b" in dirs:
                    mm(p[:, 4:8], trib[:], l1[:, 4:8], True, True, [trib.k, l1.k], [p.k])
                c0 = 0 if need_tot == "f" else 4
                mm(p[:, 8:12], nones[:], l1[:, c0:c0 + 4], True, True, [nones.k, l1.k], [p.k])
                lo, hi = (0, 8) if len(dirs) == 2 else ((0, 4) if "f" in dirs else (4, 8))
                tt("vector", ws8[:, lo:hi], g8[:, lo:hi], p[:, lo:hi], ALU.subtract, [g8.k, p.k], [ws8.k])
                act(ws8[:, lo:hi], ws8[:, lo:hi], AF.Exp, [ws8.k], [ws8.k])
                act(E8[:, lo:hi], p[:, lo:hi], AF.Exp, [p.k], [E8.k])
                act(dec4[:], p[:, 8:12], AF.Exp, [p.k], [dec4.k])

            def make_vw(d):
                c0 = 0 if d == "f" else 4
                tt("vector", Vw[d][:, :, 0:128], mvs[:].rearrange("p (h d) -> p h d", h=4),
                   ws8[:, c0:c0 + 4].unsqueeze(2).to_broadcast([128, 4, 128]), ALU.mult, [mvs.k, ws8.k], [Vw[d].k])
                cp("gpsimd", Vw[d][:, :, 128:129], ws8[:, c0:c0 + 4].unsqueeze(2), [ws8.k], [Vw[d].k])

            def state_update(d, j, cd_out_ap, cd_key):
                St = Sst[d]
                decb = dec4[:].unsqueeze(2).to_broadcast([128, 4, 130])
                tt("vector", Sdec[:], St[:], decb, ALU.mult, [St.k, dec4.k], [Sdec.k])
                ts("vector", cd_out_ap, Sdec[:], QS, None, ALU.mult, None, [Sdec.k], [cd_key])
                pa = nextps(); pb = nextps()
                for h in range(4):
                    pp = pa if h < 2 else pb
                    o = pp[:, 0:260].rearrange("p (a b) -> p a b", a=2)[:, h % 2, 0:129]
                    mm(o, Kt[:, j, h * 128:(h + 1) * 128], Vw[d][:, h, 0:129], True, True, [Kt.k, Vw[d].k], [pp.k])
                tt("vector", St[:, 0:2, 0:129], Sdec[:, 0:2, 0:129], pa[:, 0:260].rearrange("p (a b) -> p a b", a=2)[:, :, 0:129],
                   ALU.add, [Sdec.k, pa.k], [St.k])
                tt("vector", St[:, 2:4, 0:129], Sdec[:, 2:4, 0:129], pb[:, 0:260].rearrange("p (a b) -> p a b", a=2)[:, :, 0:129],
                   ALU.add, [Sdec.k, pb.k], [St.k])

            def p1_tile(j, kv_col, v_idx, rope_n, dirs_states, cdb_idx):
                pX = nextps(); pY = nextps()
                for kc in range(8):
                    mm(pX[:], hT[:, kc, j * 128:(j + 1) * 128], win[:, kc, C_AK:C_AK + 512], kc == 0, kc == 7, [hT.k, win.k], [pX.k])
                for kc in range(8):
                    mm(pY[:, 0:272], hT[:, kc, j * 128:(j + 1) * 128], win[:, kc, C_AK + 512:C_AK + 784], kc == 0, kc == 7,
                       [hT.k, win.k], [pY.k])
                qk_norm_rope(pX[:, 0:128], pX.k, 2, knB, rope_n, qk_o[:, 0:128])
                tr(PT[:, 0:128], qk_o[:, 0:128], identb[:], ["qk_o", identb.k], [PT.k])
                cp("scalar", KT[:, kv_col:kv_col + 128], PT[:, 0:128], [PT.k], [KT.k])
                cp("scalar", V[:, v_idx, :, 0:64], pX[:, 128:256].rearrange("p (a b) -> p a b", a=2), [pX.k], [V.k])
                cp("scalar", mvs[:, 0:256], pX[:, 256:512], [pX.k], [mvs.k])
                cp("scalar", mvs[:, 256:512], pY[:, 0:256], [pY.k], [mvs.k])
                gates_prep(pY[:, 256:272], pY.k)
                for d in dirs_states:
                    chunk_weights([d], d)
                    make_vw(d)
                    if d == "b" and cdb_idx is not None:
                        cs = rr("cds", CdS)
                        state_update(d, j, cs[:], cs.k)
                        dma("sync", cdb_d[cdb_idx], cs[:], [cs.k], [("cdb", cdb_idx)], cs.k)
                    else:
                        state_update(d, j, CdF[:], CdF.k)

            def p1_group_ctx(b):
                stage_a(lambda j: ctx_d[b * CTX + j * 128: b * CTX + (j + 1) * 128, :], 2, A1, B1)
                feat_proj(C_MK, 4, 256, mkT, 4, None, None)
                k_tokmajor(2)
                for j in (0, 1):
                    p1_tile(j, SEQ + j * 128, NT + j, None, ["f"], None)
                for j in (1, 0):
                    p1_tile(j, SEQ + j * 128, NT + j, None, ["b"], None)

            def gen_p1_proj(j, pX, pY, hb_):
                for kc in range(8):
                    mm(pX[:], hb_[:, kc, j * 128:(j + 1) * 128], win[:, kc, C_AK:C_AK + 512], kc == 0, kc == 7, [hb_.k, win.k], [pX.k])
                yield
                for kc in range(8):
                    mm(pY[:, 0:272], hb_[:, kc, j * 128:(j + 1) * 128], win[:, kc, C_AK + 512:C_AK + 784], kc == 0, kc == 7,
                       [hb_.k, win.k], [pY.k])
                yield

            def gen_p1_k(j, pX, kv_col, v_idx, rope_n):
                cp("scalar", V[:, v_idx, :, 0:64], pX[:, 128:256].rearrange("p (a b) -> p a b", a=2), [pX.k], [V.k[1 + v_idx]])
                yield
                qk_norm_rope(pX[:, 0:128], pX.k, 2, knB, rope_n, qk_o[:, 0:128])
                yield
                tr(PT[:, 0:128], qk_o[:, 0:128], identb[:], ["qk_o", identb.k], [PT.k])
                cp("scalar", KT[:, kv_col:kv_col + 128], PT[:, 0:128], [PT.k], [KT.k[1 + v_idx]])
                yield

            def gen_p1_s(j, pX, pY, d, cdb_idx):
                cp("scalar", mvs[:, 0:256], pX[:, 256:512], [pX.k], [mvs.k])
                cp("scalar", mvs[:, 256:512], pY[:, 0:256], [pY.k], [mvs.k])
                gates_prep(pY[:, 256:272], pY.k)
                yield
                chunk_weights([d], d)
                yield
                make_vw(d)
                yield
                cs = rr("cds", CdS)
                state_update(d, j, cs[:], cs.k)
                dma("sync", cdb_d[cdb_idx], cs[:], [cs.k], [("cdb", cdb_idx)], cs.k)
                yield

            def xrows(b, g):
                return lambda j: x_d[b * SEQ + g * 512 + j * 128: b * SEQ + g * 512 + (j + 1) * 128, :]

            def p1_group(b, g, gnext):
                hb_ = HB[g % 2]
                feat_proj(C_MK, 4, 512, mkT, 4, (NG + g - 1) if g > 0 else None, (g + 1) if g < NG - 1 else None, hb_)
                k_tokmajor(4)
                al_S = RR(PB[4:7])
                pairs = [(PB[0], PB[1]), (PB[2], PB[3])]
                js = (3, 2, 1, 0)
                bg = []
                if gnext is not None:
                    bg = [gen_stage_tile(xrows(b, gnext), i_, A1, B1, HB[gnext % 2]) for i_ in range(4)]
                run([(gen_p1_proj(js[0], pairs[0][0], pairs[0][1], hb_), al_all)])
                for i, j in enumerate(js):
                    n = g * 4 + j
                    pX, pY = pairs[i % 2]
                    gens = [(gen_p1_k(j, pX, n * 128, n, n), al_S), (gen_p1_s(j, pX, pY, "b", n), al_S)]
                    if i + 1 < len(js):
                        gens.append((gen_p1_proj(js[i + 1], pairs[(i + 1) % 2][0], pairs[(i + 1) % 2][1], hb_), al_all))
                    run(gens, bg)
                drain(bg)

            def p0(b):
                hb = rr("hx", hx)
                xb = rr("xt", xt)
                xv = x_d[b * SEQ:(b + 1) * SEQ, :].rearrange("(g t) d -> t g d", t=512)
                dma("sync", xb[0:NG, :], xv[0], [], [xb.k], xb.k)
                dma("sync", xb[NG:2 * NG, :], xv[511], [], [xb.k], xb.k)
                s4 = rr("st", st4)
                act(hb[0:2 * NG, :], xb[0:2 * NG, :], AF.Square, [xb.k], [hb.k, s4.k], accum=s4[0:2 * NG, 0:1])
                act(s4[0:2 * NG, 1:2], s4[0:2 * NG, 0:1], AF.Ln, [s4.k], [s4.k], bias=EPS, scale=1.0 / D)
                act(s4[0:2 * NG, 2:3], s4[0:2 * NG, 1:2], AF.Exp, [s4.k], [s4.k], scale=-0.5)
                stt(t1[0:2 * NG, :], xb[0:2 * NG, :], s4[0:2 * NG, 2:3], A1[0:2 * NG, :], ALU.mult, ALU.mult, [xb.k, s4.k, A1.k], [t1.k])
                tt("gpsimd", hb[0:2 * NG, :], t1[0:2 * NG, :], B1[0:2 * NG, :], ALU.add, [t1.k, B1.k], [hb.k])
                for kc in range(8):
                    tr(PT[:, kc * 128:kc * 128 + 2 * NG], hb[0:2 * NG, kc * 128:(kc + 1) * 128], identb[0:2 * NG, 0:2 * NG], [hb.k, identb.k], [PT.k])
                cp("scalar", hT[:, :, 0:2 * NG], PT[:].rearrange("p (a b) -> p a b", a=8)[:, :, 0:2 * NG], [PT.k], [hT.k])
                p = nextps()
                for cc in range(8):
                    for kc in range(8):
                        mm(p[:, cc * 2 * NG:(cc + 1) * 2 * NG], win[:, kc, C_MQ + cc * 128:C_MQ + (cc + 1) * 128], hT[:, kc, 0:2 * NG],
                           kc == 0, kc == 7, [win.k, hT.k], [p.k])
                cp("vector", HS[:], p[:, 0:16 * NG].rearrange("p (a b) -> p a b", a=8), [p.k], [HS.k])

            def gen_attention(n):
                blocks = []
                if n > 0:
                    blocks.append((n - 1) * 128)
                blocks.append(n * 128)
                if n < NT - 1:
                    blocks.append((n + 1) * 128)
                blocks += [SEQ, SEQ + 128]
                for kv in range(2):
                    pr = slice(64 * kv, 64 * kv + 64)
                    for bi, col in enumerate(blocks):
                        p = nextps()
                        mm(p[:], KT[pr, col:col + 128], QT[pr, :, :], True, True, [KT.k, QT.k], [p.k])
                        act(PTs[:, bi, :], p[:], AF.Exp, [p.k], [PTs.k], scale=0.125)
                        if col == (n - 1) * 128:
                            m = amlo
                        elif col == (n + 1) * 128 and col < SEQ:
                            m = amhi
                        else:
                            m = None
                        if m is not None:
                            tt("gpsimd", PTs[:, bi, :].rearrange("p (g q) -> p g q", g=4),
                               PTs[:, bi, :].rearrange("p (g q) -> p g q", g=4),
                               m[:, :].unsqueeze(1).to_broadcast([128, 4, 128]), ALU.mult, [PTs.k, m.k], [PTs.k])
                        if bi % 2 == 1 or bi == len(blocks) - 1:
                            yield
                    po = nextps()
                    for gi in range(4):
                        for bi, col in enumerate(blocks):
                            vi = col // 128
                            mm(po[:, gi * 65:(gi + 1) * 65], PTs[:, bi, gi * 128:(gi + 1) * 128], V[:, vi, kv, :],
                               bi == 0, bi == len(blocks) - 1, [PTs.k, V.k], [po.k])
                        if gi % 2 == 1:
                            yield
                    po3 = po[:, 0:260].rearrange("p (g c) -> p g c", g=4)
                    tt("vector", arec[:], po3[:, :, 64], esink[:, kv * 4:kv * 4 + 4], ALU.add, [po.k, esink.k], [arec.k])
                    recip(arec[:], arec[:], [arec.k], [arec.k])
                    tt("vector", mixr[n % 2][:, kv * 256:(kv + 1) * 256].rearrange("p (g c) -> p g c", g=4), po3[:, :, 0:64],
                       arec[:].unsqueeze(2).to_broadcast([128, 4, 64]), ALU.mult, [po.k, arec.k], ["mixA%d" % (n % 2)])
                    yield

            def gen_head(b, g, j):
                hb_ = HB[g % 2]
                n = g * 4 + j
                tsl = slice(j * 128, (j + 1) * 128)
                pq = nextps(); po_ = nextps(); pv = nextps(); pg = nextps()
                for (pp, c0, w) in ((pq, C_AQ, 512), (pv, C_MV, 512), (pg, C_GT, 16), (po_, C_MO, 512)):
                    for kc in range(8):
                        mm(pp[:, 0:w], hb_[:, kc, tsl], win[:, kc, c0:c0 + w], kc == 0, kc == 7, tile_keys(hb_, j) + [win.k], [pp.k])
                    yield
                qk_norm_rope(pq[:], pq.k, 8, qnB, n, qk_o[:, 0:512])
                yield
                for pi in range(4):
                    tr(PT[:, pi * 128:(pi + 1) * 128], qk_o[:, pi * 128:(pi + 1) * 128], identb[:], ["qk_o", identb.k], [PT.k])
                cp("scalar", QT[:], PT[:, 0:512].rearrange("p (a b) -> p a b", a=4), [PT.k], [QT.k])
                yield
                cp("scalar", mvs[:], pv[:], [pv.k], [mvs.k])
                gates_prep(pg[:, 0:16], pg.k)
                yield
                act(sig[:], po_[:], AF.Exp, [po_.k], [sig.k], scale=-1.0)
                act(sig[:], sig[:], AF.Ln, [sig.k], [sig.k], bias=1.0)
                act(sig[:], sig[:], AF.Exp, [sig.k], [sig.k], scale=-1.0)
                tt("gpsimd", sig[:], sig[:], mnB[:], ALU.mult, [sig.k, mnB.k], [sig.k])
                yield

            def gen_mlstm(b, g, j):
                n = g * 4 + j
                tsl = slice(j * 128, (j + 1) * 128)
                chunk_weights(["f", "b"], "f")
                yield
                make_vw("f"); make_vw("b")
                pS = nextps()
                for h in range(4):
                    mm(pS[:, h * 128:(h + 1) * 128], mkT[:, h, tsl], mqT[:, h, tsl], True, True, [mkT.k, mqT.k], [pS.k])
                pS3 = pS[:].rearrange("p (h t) -> p h t", h=4)
                tt("vector", Sm["f"][:], pS3, mskf[:, :].unsqueeze(1).to_broadcast([128, 4, 128]), ALU.mult, [pS.k, mskf.k], [Sm["f"].k])
                tt("vector", Sm["b"][:], pS3, mskb[:, :].unsqueeze(1).to_broadcast([128, 4, 128]), ALU.mult, [pS.k, mskb.k], [Sm["b"].k])
                yield
                state_update("f", j, CdF[:], CdF.k)
                yield
                cl = rr("cdl", CdL)
                dma("sync", cl[:], cdb_d[n], [("cdb", n)], [cl.k], cl.k)
                for di, d in enumerate(("f", "b")):
                    pa = nextps(); pb = nextps()
                    for h in range(4):
                        pp = pa if h < 2 else pb
                        o = pp[:, 0:260].rearrange("p (a b) -> p a b", a=2)[:, h % 2, 0:129]
                        mm(o, Sm[d][:, h, :], Vw[d][:, h, 0:129], True, False, [Sm[d].k, Vw[d].k], [pp.k])
                        if d == "f":
                            mm(o, mqT[:, h, tsl], CdF[:, h, 0:129], False, True, [mqT.k, CdF.k], [pp.k])
                        else:
                            mm(o, mqT[:, h, tsl], cl[:, h, 0:129], False, True, [mqT.k, cl.k], [pp.k])
                    yield
                    c4 = di * 4
                    for half, pp in enumerate((pa, pb)):
                        c0 = c4 + half * 2
                        den = pp[:, 0:260].rearrange("p (a b) -> p a b", a=2)[:, :, 128]
                        tt("vector", sc8[:, c0:c0 + 2], den, E8[:, c0:c0 + 2], ALU.mult, [pp.k, E8.k], [sc8.k])
                    ts("vector", sc8n[:, c4:c4 + 4], sc8[:, c4:c4 + 4], -1.0, None, ALU.mult, None, [sc8.k], [sc8n.k])
                    tt("vector", sc8[:, c4:c4 + 4], sc8[:, c4:c4 + 4], sc8n[:, c4:c4 + 4], ALU.max, [sc8.k, sc8n.k], [sc8.k])
                    ts("vector", sc8[:, c4:c4 + 4], sc8[:, c4:c4 + 4], 1.0, None, ALU.max, None, [sc8.k], [sc8.k])
                    recip(sc8[:, c4:c4 + 4], sc8[:, c4:c4 + 4], [sc8.k], [sc8.k])
                    tt("vector", sc8[:, c4:c4 + 4], sc8[:, c4:c4 + 4], E8[:, c4:c4 + 4], ALU.mult, [sc8.k, E8.k], [sc8.k])
                    yield
                    for h in range(4):
                        pp = pa if h < 2 else pb
                        src = pp[:, 0:260].rearrange("p (a b) -> p a b", a=2)[:, h % 2, 0:128]
                        if d == "f":
                            ts("vector", hs[:, h * 128:(h + 1) * 128], src, sc8[:, h:h + 1], None, ALU.mult, None, [pp.k, sc8.k], [hs.k])
                        else:
                            stt(hs[:, h * 128:(h + 1) * 128], src, sc8[:, 4 + h:5 + h], hs[:, h * 128:(h + 1) * 128], ALU.mult, ALU.add,
                                [pp.k, sc8.k, hs.k], [hs.k])
                    yield
                for h in range(4):
                    act(Sm["f"][:, h, :], hs[:, h * 128:(h + 1) * 128], AF.Square, [hs.k], [Sm["f"].k, hrs.k], accum=hrs[:, h:h + 1])
                act(hrs[:], hrs[:], AF.Ln, [hrs.k], [hrs.k], bias=EPS, scale=1.0 / 128)
                act(hrs[:], hrs[:], AF.Exp, [hrs.k], [hrs.k], scale=-0.5)
                yield
                tt("vector", hs[:].rearrange("p (h d) -> p h d", h=4), hs[:].rearrange("p (h d) -> p h d", h=4),
                   hrs[:].unsqueeze(2).to_broadcast([128, 4, 128]), ALU.mult, [hs.k, hrs.k], [hs.k])
                tt("vector", mixr[n % 2][:, 512:1024], hs[:], sig[:], ALU.mult, [hs.k, sig.k], ["mixM%d" % (n % 2)])
                yield

            def gen_tail(b, g, j):
                n = g * 4 + j
                row0 = b * SEQ + n * 128
                for kc in range(8):
                    tr(PT[:, kc * 128:(kc + 1) * 128], mixr[n % 2][:, kc * 128:(kc + 1) * 128], identb[:],
                       ["mixA%d" % (n % 2), "mixM%d" % (n % 2), identb.k], [PT.k])
                cp("scalar", mixT[:], PT[:].rearrange("p (a b) -> p a b", a=8), [PT.k], [mixT.k])
                xb = rr("xt", xt)
                dma("sync", xb[:], x_d[row0:row0 + 128, :], [], [xb.k], xb.k)
                xo = rr("xn", xn)
                yield
                for half in range(2):
                    p = nextps()
                    for kc in range(8):
                        mm(p[:], mixT[:, kc, :], wout[:, kc, half * 512:(half + 1) * 512], kc == 0, kc == 7, [mixT.k, wout.k], [p.k])
                    hsl = slice(half * 512, (half + 1) * 512)
                    tt("vector", xo[:, hsl], p[:], xb[:, hsl], ALU.add, [p.k, xb.k], [xo.k])
                    yield
                dma("sync", out_d[row0:row0 + 128, :], xo[:], [xo.k], ["outrows"], xo.k)
                yield
                s4 = rr("st", st4)
                hb2 = rr("h2b", h2b)
                act(hb2[:], xo[:], AF.Square, [xo.k], [hb2.k, s4.k], accum=s4[:, 0:1])
                act(s4[:, 1:2], s4[:, 0:1], AF.Ln, [s4.k], [s4.k], bias=EPS, scale=1.0 / D)
                act(s4[:, 2:3], s4[:, 1:2], AF.Exp, [s4.k], [s4.k], scale=-0.5)
                yield
                stt(t1[:], xo[:], s4[:, 2:3], A2[:], ALU.mult, ALU.mult, [xo.k, s4.k, A2.k], [t1.k])
                tt("vector", t1[:], t1[:], B2[:], ALU.add, [t1.k, B2.k], [t1.k])
                yield
                cp("scalar", hb2[:], h2[:], [h2.k], [hb2.k])
                dma("sync", hx2s_d[row0:row0 + 128, :], hb2[:], [hb2.k], ["hx2s"], hb2.k)
                for half in range(2):
                    p = nextps()
                    for q4 in range(4):
                        kc = half * 4 + q4
                        tr(p[:, q4 * 128:(q4 + 1) * 128], h2[:, kc * 128:(kc + 1) * 128], identf[:], [h2.k, identf.k], [p.k])
                    cp("scalar", h2T[:, half * 4:half * 4 + 4, :], p[:].rearrange("p (a b) -> p a b", a=4), [p.k], [h2T.k])
                    yield
                p = nextps()
                for kc in range(8):
                    mm(p[:, 0:16], h2T[:, kc, :], wr[:, kc, :], kc == 0, kc == 7, [h2T.k, wr.k], [p.k])
                S.op("vector", lambda e, p=p: e.tensor_reduce(out=rs1[:], in_=p[:, 0:16], axis=AX.X, op=ALU.max, negate=True),
                     reads=[p.k], writes=[rs1.k])
                act(rt[:], p[:, 0:16], AF.Exp, [p.k, rs1.k], [rt.k, rs2.k], bias=rs1[:, 0:1], accum=rs2[:, 0:1])
                yield
                recip(rs2[:], rs2[:], [rs2.k], [rs2.k])
                ts("vector", aff_all[:, n % (NT // 2), n // (NT // 2), b, :], rt[:], rs2[:, 0:1], None, ALU.mult, None, [rt.k, rs2.k], [(aff_all.k, n, b)])
                yield

            def p2_prep2(b, g):
                hb_ = HB[g % 2]
                hp = (NG + g - 1) if g > 0 else None
                hn = (g + 1) if g < NG - 1 else None
                feat_proj(C_MQ, 4, 512, mqT, 0, hp, hn, hb_)
                feat_proj(C_MK, 4, 512, mkT, 4, hp, hn, hb_)
                k_tokmajor(4)

            def p2_batch(b, ngroups):
                ntile = ngroups * 4
                if CFG.get("psv", 1) == 1:
                    al_A = RR(PB[0:2]); al_M = RR(PB[3:6]); al_T = RR([PB[2], PB[6]]); al_H = RR(PB[0:4])
                else:
                    al_A = RR(PB[0:3]); al_M = RR(PB[3:6]); al_T = RR(PB[6:7]); al_H = RR(PB[0:4])
                stage_a(xrows(b, 0), 4, A1, B1, HB[0])
                p2_prep2(b, 0)
                run([(gen_head(b, 0, 0), al_H)])
                for n in range(ntile):
                    g, j = n // 4, n % 4
                    gens = [(gen_attention(n), al_A), (gen_mlstm(b, g, j), al_M)]
                    if n > 0:
                        gens.append((gen_tail(b, (n - 1) // 4, (n - 1) % 4), al_T))
                    if j == 0:
                        bg = []
                        if g + 1 < ngroups:
                            bg = [gen_stage_tile(xrows(b, g + 1), j_, A1, B1, HB[(g + 1) % 2]) for j_ in range(4)]
                    run(gens, bg)
                    if n + 1 < ntile:
                        if j == 3:
                            drain(bg)
                            p2_prep2(b, g + 1)
                        run([(gen_head(b, (n + 1) // 4, (n + 1) % 4), al_H)], bg)
                run([(gen_tail(b, (ntile - 1) // 4, (ntile - 1) % 4), al_T)])

            stop = CFG["stop"]
            for b in range(CFG["nb"]):
                if CFG.get("lm", 3) & 1:
                    load_mod(2, {"B1": B1} if CFG.get("lm", 3) & 4 else {"A1": A1, "B1": B1})
                if CFG.get("lm", 3) & 2:
                    mset("vector", Sst["f"][:], 0.0, [Sst["f"].k])
                    mset("vector", Sst["b"][:], 0.0, [Sst["b"].k])
                if stop == "mod":
                    continue
                p1_group_ctx(b)
                load_mod(b, {"A1": A1, "B1": B1, "A2": A2, "B2": B2})
                load_mod(b, {"G1": None})
                load_wout_scaled()
                if stop == "ctx":
                    continue
                p0(b)
                if stop == "p0":
                    continue
                g1s = list(range(NG - 1, -1, -1))
                if CFG["ng1"] is not None:
                    g1s = g1s[:CFG["ng1"]]
                stage_a(xrows(b, g1s[0]), 4, A1, B1, HB[g1s[0] % 2])
                for gi_, g in enumerate(g1s):
                    p1_group(b, g, g1s[gi_ + 1] if gi_ + 1 < len(g1s) else None)
                if stop == "p1":
                    continue
                p2_batch(b, NG if CFG["ng2"] is None else CFG["ng2"])
            S.emit()
        if CFG["stop"] in ("mod", "ctx", "p0", "p1", "p2"):
            return nc

        with ExitStack() as es:
            S = Sched(nc, "b")

            def sb(name, shape, dt=F32):
                return Buf(es.enter_context(nc.sbuf_tensor("z_" + name, list(shape), dt)), "z_" + name)

            def ps(name, shape, dt=F32):
                return Buf(es.enter_context(nc.psum_tensor(name, list(shape), dt)), name)

            PB = [ps("qb%d" % i, [128, 512]) for i in range(6)]
            PTS = [ps("qtb%d" % i, [128, 1024], BF16) for i in range(2)]
            pbi = [0]

            def nextps():
                p = PB[pbi[0] % 6]
                pbi[0] += 1
                return p

            G2 = [sb("G2_%d" % b, [128, D]) for b in range(NB)]
            for b in range(NB):
                S.op("sync", lambda e, b=b: e.dma_start(out=G2[b][:], in_=modsc_d[b, 5 * D:6 * D].partition_broadcast(128)),
                     writes=[G2[b].k], dma="prm2")
            S.group_keys.add("prm2")
            HSEQ = SEQ // 2
            affT = sb("affT", [64, HSEQ])
            mx = sb("mx", [64, CAP])
            ix = sb("ix", [64, CAP], U32)
            ixf = sb("ixf", [64, CAP])
            boff = sb("boff", [64, 1])
            antiI = sb("antiI", [128, 128])
            S.op("sync", lambda e: e.dma_start(out=boff[:], in_=boff_d), writes=[boff.k], dma="prm2")
            S.op("sync", lambda e: e.dma_start(out=antiI[:], in_=anti_d), writes=[antiI.k], dma="prm2")
            for m4 in range(4):
                p = nextps()
                for q in range(4):
                    m = m4 * 4 + q
                    S.op("tensor", lambda e, p=p, q=q, m=m: e.transpose(p[0:64, q * 128:(q + 1) * 128],
                                                                          aff_all[:, m, :, :, :].rearrange("p h b e -> p (h b e)"), identf[:]),
                         reads=[], writes=[p.k])
                S.op("vector", lambda e, p=p, m4=m4: e.tensor_copy(out=affT[:, m4 * 512:(m4 + 1) * 512], in_=p[0:64, :]),
                     reads=[p.k], writes=[affT.k])
            for k in range(CAP // 8):
                sl = slice(8 * k, 8 * k + 8)
                S.op("vector", lambda e, sl=sl: e.max(out=mx[:, sl], in_=affT[:]), reads=[affT.k], writes=[mx.k])
                S.op("vector", lambda e, sl=sl: e.max_index(out=ix[:, sl], in_max=mx[:, sl], in_values=affT[:]),
                     reads=[affT.k, mx.k], writes=[ix.k])
                if k < CAP // 8 - 1:
                    S.op("vector", lambda e, sl=sl: e.match_replace(out=affT[:], in_to_replace=mx[:, sl], in_values=affT[:], imm_value=-1.0),
                         reads=[affT.k, mx.k], writes=[affT.k])
            S.op("vector", lambda e: e.tensor_copy(out=ixf[:], in_=ix[:]), reads=[ix.k], writes=[ixf.k])
            S.op("vector", lambda e: e.tensor_scalar(out=ixf[:], in0=ixf[:], scalar1=boff[:, 0:1], scalar2=None, op0=ALU.add),
                 reads=[ixf.k, boff.k], writes=[ixf.k])
            idxT = sb("idxT", [128, 4, 32], I32)
            gateT = sb("gateT", [128, 4, 32])
            Tmx = sb("Tmx", [128, 4, 64])
            Tix = sb("Tix", [128, 4, 64])
            msk = sb("msk", [128, 4, 32])
            dif = sb("dif", [128, 4, 32])
            for (src, dst) in ((mx, Tmx), (ixf, Tix)):
                p = nextps()
                for q in range(4):
                    S.op("tensor", lambda e, p=p, q=q, src=src: e.transpose(p[:, q * 64:(q + 1) * 64], src[:, q * 128:(q + 1) * 128], identf[0:64, 0:64]),
                         reads=[src.k], writes=[p.k])
                S.op("vector", lambda e, p=p, dst=dst: e.tensor_copy(out=dst[:], in_=p[:, 0:256].rearrange("p (a b) -> p a b", a=4)),
                     reads=[p.k], writes=[dst.k])
            pB = nextps()
            for q in range(4):
                S.op("tensor", lambda e, q=q: e.matmul(pB[:, q * 64:q * 64 + 32], antiI[:], Tmx[:, 3 - q, 32:64], start=True, stop=True),
                     reads=[antiI.k, Tmx.k], writes=[pB.k])
                S.op("tensor", lambda e, q=q: e.matmul(pB[:, q * 64 + 32:q * 64 + 64], antiI[:], Tix[:, 3 - q, 32:64], start=True, stop=True),
                     reads=[antiI.k, Tix.k], writes=[pB.k])
            pB3 = pB[:, 0:256].rearrange("p (a b) -> p a b", a=4)
            S.op("vector", lambda e: e.tensor_tensor(out=msk[:], in0=Tmx[:, :, 0:32], in1=pB3[:, :, 0:32], op=ALU.is_ge),
                 reads=[Tmx.k, pB.k], writes=[msk.k])
            S.op("vector", lambda e: e.tensor_tensor(out=gateT[:], in0=Tmx[:, :, 0:32], in1=pB3[:, :, 0:32], op=ALU.max),
                 reads=[Tmx.k, pB.k], writes=[gateT.k])
            S.op("vector", lambda e: e.tensor_tensor(out=dif[:], in0=Tix[:, :, 0:32], in1=pB3[:, :, 32:64], op=ALU.subtract),
                 reads=[Tix.k, pB.k], writes=[dif.k])
            S.op("vector", lambda e: e.tensor_tensor(out=dif[:], in0=dif[:], in1=msk[:], op=ALU.mult),
                 reads=[dif.k, msk.k], writes=[dif.k])
            S.op("vector", lambda e: e.tensor_tensor(out=dif[:], in0=dif[:], in1=pB3[:, :, 32:64], op=ALU.add),
                 reads=[dif.k, pB.k], writes=[dif.k])
            S.op("vector", lambda e: e.tensor_copy(out=idxT[:], in_=dif[:]), reads=[dif.k], writes=[idxT.k])

            wgb = [sb("wg%d" % i, [128, 8, 512], BF16) for i in range(3)]
            wub = [sb("wu%d" % i, [128, 8, 512], BF16) for i in range(3)]
            wdb = [sb("wd%d" % i, [128, 4, D], BF16) for i in range(3)]
            xg = [sb("xg%d" % i, [128, D], BF16) for i in range(12)]
            xgTs = [sb("xgT%d" % i, [128, 8, 512], BF16) for i in range(2)]
            sgs = [sb("sg%d" % i, [128, 512]) for i in range(2)]
            hTe = sb("hTe", [128, 4, 512], BF16)
            ptc = [0]
            ys = [sb("ys%d" % i, [128, D]) for i in range(4)]

            stg = [sb("stg%d" % i, [128, 512]) for i in range(6)]
            stc = [0]

            def w_chunks(ex):
                wg_, wu_, wd_ = wgb[ex % 3], wub[ex % 3], wdb[ex % 3]
                out = []
                for kc in range(8):
                    out.append((wg_[:, kc, :], wg_.k, wg_d[ex, kc * 128:(kc + 1) * 128, :]))
                    out.append((wu_[:, kc, :], wu_.k, wu_d[ex, kc * 128:(kc + 1) * 128, :]))
                for fc in range(4):
                    for c0 in (0, 512):
                        out.append((wd_[:, fc, c0:c0 + 512], wd_.k, wd_d[ex, fc * 128:(fc + 1) * 128, c0:c0 + 512]))
                return out

            def load_chunk(ch):
                dst, dkey, src = ch
                st = stg[stc[0] % 6]
                eng = "scalar" if stc[0] % 2 == 0 else "gpsimd"
                stc[0] += 1
                S.op("sync", lambda e, st=st, src=src: e.dma_start(out=st[:], in_=src), writes=[st.k], dma=st.k)
                if eng == "scalar":
                    S.op("scalar", lambda e, st=st, dst=dst: e.copy(out=dst, in_=st[:]), reads=[st.k], writes=[dkey])
                else:
                    S.op("gpsimd", lambda e, st=st, dst=dst: e.tensor_copy(out=dst, in_=st[:]), reads=[st.k], writes=[dkey])

            def load_w(ex):
                for ch in w_chunks(ex):
                    load_chunk(ch)

            wstream = []
            for ex_ in range(2, NEXP):
                wstream.extend(w_chunks(ex_))

            def gen_wload(i):
                lo = 12 * (i - 1)
                for ch in wstream[lo:lo + 12] if i >= 1 else []:
                    load_chunk(ch)
                    yield

            items = [(ex, b) for ex in range(NEXP) for b in range(NB)]

            def gathers(i):
                ex, b = items[i]
                pe = b * 16 + ex
                for j in range(4):
                    xb = xg[(i % 3) * 4 + j]
                    S.op("gpsimd", lambda e, j=j, pe=pe, xb=xb: e.indirect_dma_start(
                        out=xb[:], out_offset=None, in_=hx2s_d,
                        in_offset=bass.IndirectOffsetOnAxis(ap=idxT[:, j, pe:pe + 1], axis=0)),
                        reads=[idxT.k], writes=[xb.k], dma=xb.k)

            def gen_trans(i):
                xgT = xgTs[i % 2]
                for j in range(4):
                    xb = xg[(i % 3) * 4 + j]
                    PT = PTS[ptc[0] % 2]
                    ptc[0] += 1
                    for kc in range(8):
                        S.op("tensor", lambda e, xb=xb, kc=kc, PT=PT: e.transpose(PT[:, kc * 128:(kc + 1) * 128], xb[:, kc * 128:(kc + 1) * 128], identb[:]),
                             reads=[xb.k], writes=[PT.k])
                    if j % 2 == 0:
                        S.op("scalar", lambda e, j=j, PT=PT, xgT=xgT: e.copy(out=xgT[:, :, j * 128:(j + 1) * 128], in_=PT[:].rearrange("p (a b) -> p a b", a=8)),
                             reads=[PT.k], writes=[xgT.k])
                    else:
                        S.op("vector", lambda e, j=j, PT=PT, xgT=xgT: e.tensor_copy(out=xgT[:, :, j * 128:(j + 1) * 128], in_=PT[:].rearrange("p (a b) -> p a b", a=8)),
                             reads=[PT.k], writes=[xgT.k])
                    yield

            def gen_ffn(i):
                ex, b = items[i]
                pe = b * 16 + ex
                xgT = xgTs[i % 2]
                wg_, wu_, wd_ = wgb[ex % 3], wub[ex % 3], wdb[ex % 3]
                for fc in range(4):
                    pg = nextps(); pu = nextps()
                    sg = sgs[fc % 2]
                    for kc in range(8):
                        S.op("tensor", lambda e, pg=pg, kc=kc, fc=fc: e.matmul(pg[:], wg_[:, kc, fc * 128:(fc + 1) * 128], xgT[:, kc, :],
                                                                             start=(kc == 0), stop=(kc == 7)),
                             reads=[wg_.k, xgT.k], writes=[pg.k])
                    for kc in range(8):
                        S.op("tensor", lambda e, pu=pu, kc=kc, fc=fc: e.matmul(pu[:], wu_[:, kc, fc * 128:(fc + 1) * 128], xgT[:, kc, :],
                                                                             start=(kc == 0), stop=(kc == 7)),
                             reads=[wu_.k, xgT.k], writes=[pu.k])
                    S.op("scalar", lambda e, pg=pg, sg=sg: e.activation(out=sg[:], in_=pg[:], func=AF.Silu), reads=[pg.k], writes=[sg.k])
                    S.op("vector", lambda e, pu=pu, fc=fc, sg=sg: e.tensor_tensor(out=hTe[:, fc, :], in0=pu[:], in1=sg[:], op=ALU.mult),
                         reads=[pu.k, sg.k], writes=[hTe.k])
                    yield
                for j in range(4):
                    for half in range(2):
                        p = nextps()
                        for fc in range(4):
                            S.op("tensor", lambda e, p=p, fc=fc, j=j, half=half: e.matmul(
                                p[:], hTe[:, fc, j * 128:(j + 1) * 128], wd_[:, fc, half * 512:(half + 1) * 512],
                                start=(fc == 0), stop=(fc == 3)), reads=[hTe.k, wd_.k], writes=[p.k])
                        S.op("vector", lambda e, p=p, j=j, half=half: e.scalar_tensor_tensor(
                            out=ys[j][:, half * 512:(half + 1) * 512], in0=p[:], scalar=gateT[:, j, pe:pe + 1],
                            in1=G2[b][:, half * 512:(half + 1) * 512], op0=ALU.mult, op1=ALU.mult),
                            reads=[p.k, gateT.k, G2[b].k], writes=[ys[j].k])
                    S.op("gpsimd", lambda e, j=j: e.indirect_dma_start(
                        out=out_d, out_offset=bass.IndirectOffsetOnAxis(ap=idxT[:, j, pe:pe + 1], axis=0),
                        in_=ys[j][:], in_offset=None, compute_op=ALU.add),
                        reads=[ys[j].k, idxT.k] + ["osc%d_%d_%d" % (b, (ex + 1) % 2, jj) for jj in range(4)],
                        writes=["osc%d_%d_%d" % (b, ex % 2, j)], dma=ys[j].k)
                    yield

            def run2(gens):
                live = list(gens)
                while live:
                    for g_ in list(live):
                        try:
                            next(g_)
                        except StopIteration:
                            live.remove(g_)

            load_w(0)
            load_w(1)
            gathers(0)
            gathers(1)
            run2([gen_trans(0)])
            for i in range(len(items)):
                ex, b = items[i]
                if i + 2 < len(items):
                    gathers(i + 2)
                if i + 1 < len(items):
                    run2([gen_ffn(i), gen_trans(i + 1), gen_wload(i)])
                else:
                    run2([gen_ffn(i), gen_wload(i)])
            S.emit()
    return nc


_CACHE = {}


def _consts():
    import ml_dtypes
    bf = ml_dtypes.bfloat16
    n = SEQ
    rows = n // 64
    row, col = np.meshgrid(np.arange(rows), np.arange(64), indexing="ij")
    n_freq = 16
    freqs = (10000.0 ** (-np.arange(n_freq, dtype=np.float32) / n_freq)).astype(np.float32)
    ang = np.concatenate([row.reshape(-1, 1).astype(np.float32) * freqs, col.reshape(-1, 1).astype(np.float32) * freqs], -1)
    s = np.arange(128)[:, None]
    t = np.arange(128)[None, :]
    c = {
        "rope_cos": np.cos(ang).astype(np.float32),
        "rope_sin": np.sin(ang).astype(np.float32),
        "ident_bf": np.eye(128, dtype=np.float32).astype(bf),
        "ident_f": np.eye(128, dtype=np.float32),
        "tri_f": (s > t).astype(np.float32),
        "tri_b": (s < t).astype(np.float32),
        "neg_ones": -np.ones((128, 128), np.float32),
        "mask_f": ((s <= t) * QS).astype(np.float32).astype(bf),
        "mask_b": ((s >= t) * QS).astype(np.float32).astype(bf),
        "amask_lo": (s >= t).astype(np.float32).astype(bf),
        "amask_hi": (s <= t).astype(np.float32).astype(bf),
        "boff": np.array([(p_ // 32) * (SEQ // 2) + ((p_ % 32) // 16) * SEQ for p_ in range(64)], np.float32).reshape(64, 1),
        "anti_ident": np.ascontiguousarray(np.eye(128, dtype=np.float32)[::-1]),
    }
    return c


def _perm_win(w_in):
    w = w_in
    aq = w[:, 0:512]
    ak = w[:, 512:640]
    av = w[:, 640:768]
    mq = w[:, 768:1280]
    mk = w[:, 1280:1792]
    mv = w[:, 1792:2304]
    mo = w[:, 2304:2816]
    gt = w[:, 2816:2832]
    heads = [aq[:, h * 64:(h + 1) * 64] for h in range(8)]
    aqp = np.concatenate([np.concatenate([heads[p], heads[4 + p]], 1) for p in range(4)], 1)
    return np.ascontiguousarray(np.concatenate([aqp, mo, ak, av, mv, gt, mq, mk], 1))


def kernel(x, c, ctx, c_ctx, w_mod, b_mod, norm1_w, norm2_w, w_in, b_gates, conv_qk, q_norm_w,
           k_norm_w, sink, mlstm_norm_w, w_out, w_router, w_gate, w_up, w_down):
    f = lambda a: np.ascontiguousarray(np.asarray(a, dtype=np.float32))
    x = f(x); c = f(c); ctx = f(ctx); c_ctx = f(c_ctx)
    if "nc" not in _CACHE:
        _CACHE["nc"] = build_program()
    nc = _CACHE["nc"]
    consts = _consts()
    shared = {
        "w_mod": f(w_mod)[0], "b_mod": f(b_mod), "norm1_w": f(norm1_w), "norm2_w": f(norm2_w),
        "w_in": _perm_win(f(w_in)[0]), "b_gates": f(b_gates), "conv_qk": f(conv_qk)[0],
        "q_norm_w": f(q_norm_w), "k_norm_w": f(k_norm_w), "sink": f(sink), "mlstm_norm_w": f(mlstm_norm_w),
        "w_out": f(w_out)[0], "w_router": f(w_router)[0], "w_gate": f(w_gate)[0], "w_up": f(w_up)[0],
        "w_down": f(w_down)[0],
    }
    shared.update(consts)
    if CFG["stop"] is not None:
        for k in ("w_gate", "w_up", "w_down"):
            shared.pop(k)
    in_maps = []
    for i in range(8):
        m = dict(shared)
        m["x"] = x[2 * i:2 * i + 2].reshape(NB * SEQ, D)
        m["ctx"] = ctx[2 * i:2 * i + 2].reshape(NB * CTX, D)
        m["cvec"] = np.ascontiguousarray(np.stack([c[2 * i], c[2 * i + 1], c_ctx], 0))
        in_maps.append(m)
    res = run_bass_kernel_spmd(nc, in_maps, core_ids=list(range(8)))
    _CACHE["last"] = res.results
    out = np.concatenate([np.asarray(r["out"]).reshape(NB, SEQ, D) for r in res.results], 0)
    return out.astype(np.float32)
```

```python
import numpy as np
from contextlib import ExitStack
import concourse.bass as bass
import concourse.mybir as mybir
from concourse.bass_utils import run_bass_kernel_spmd

F32 = mybir.dt.float32
BF16 = mybir.dt.bfloat16
I32 = mybir.dt.int32
U32 = mybir.dt.uint32
AF = mybir.ActivationFunctionType
ALU = mybir.AluOpType
AX = mybir.AxisListType

D = 1024
SEQ = 4096
NT = SEQ // 128
NG = SEQ // 512
CTX = 256
NB = 2
EPS = 1e-6
NEXP = 16
CAP = 512
QS = 128 ** -0.5
C_AQ, C_MO, C_AK, C_AV, C_MV, C_GT, C_MQ, C_MK = 0, 512, 1024, 1152, 1280, 1792, 1808, 2320
SEM_CHUNK = 16000
CFG = {"stop": None, "nb": NB, "ng1": None, "ng2": None}


class Sched:
    ENGS = ("tensor", "vector", "scalar", "gpsimd", "sync")

    def __init__(self, nc, tag):
        self.nc = nc
        self.tag = tag
        self.ops = []
        self.last_writer = {}
        self.readers = {}
        self.eng_count = {e: 0 for e in self.ENGS}
        self.dma_count = {}
        self.sem_names = set()
        self.group_keys = set()

    def _event_compute(self, eng):
        c = self.eng_count[eng]
        self.eng_count[eng] = c + 1
        name = "%sp_%s_%d" % (self.tag, eng, c // SEM_CHUNK)
        self.sem_names.add(name)
        return (name, (c % SEM_CHUNK) + 1, 1)

    def _event_dma(self, key):
        c = self.dma_count.get(key, 0)
        self.dma_count[key] = c + 1
        per = SEM_CHUNK // 16
        name = "%sd_%s_%d" % (self.tag, key, c // per)
        self.sem_names.add(name)
        return (name, ((c % per) + 1) * 16, 16)

    def op(self, eng, fn, reads=(), writes=(), dma=None):
        def expand(keys):
            out = []
            for k in keys:
                if isinstance(k, tuple) and len(k) > 0 and k[0] == "MULTI":
                    out.extend(k[1:])
                else:
                    out.append(k)
            return out
        reads = expand(reads)
        writes = expand(writes)
        waits = {}

        def need(ev):
            if ev is None:
                return
            n, v, _ = ev
            if waits.get(n, 0) < v:
                waits[n] = v

        for r in reads:
            need(self.last_writer.get(r))
        for w in writes:
            need(self.last_writer.get(w))
            for ev in self.readers.get(w, ()):
                need(ev)
        ev = self._event_dma(dma) if dma is not None else self._event_compute(eng)
        for r in reads:
            self.readers.setdefault(r, []).append(ev)
        for w in writes:
            self.last_writer[w] = ev
            self.readers[w] = []
        self.ops.append((eng, fn, waits, ev))

    def emit(self):
        nc = self.nc
        with ExitStack() as es:
            sems = {}
            for n in sorted(self.sem_names):
                sems[n] = es.enter_context(nc.semaphore(n))
            block = es.enter_context(nc.Block())
            final_waits = {}
            for (eng, fn, waits, ev) in self.ops:
                n, v, _ = ev
                if final_waits.get(n, 0) < v:
                    final_waits[n] = v
            gnames = set("%sd_%s_0" % (self.tag, k) for k in self.group_keys) if CFG.get("gk", 1) else set()

            def make(engname):
                def body(e):
                    seen = {}
                    for (eng, fn, waits, ev) in self.ops:
                        if eng != engname:
                            continue
                        for n, v in waits.items():
                            if engname == "tensor" and "p_tensor_" in n:
                                continue
                            if n in gnames:
                                v = final_waits[n]
                            if seen.get(n, 0) >= v:
                                continue
                            e.wait_ge(sems[n], v)
                            seen[n] = v
                        ins = fn(e)
                        ins.then_inc(sems[ev[0]], ev[2])
                    if engname == "sync":
                        for n, v in final_waits.items():
                            if seen.get(n, 0) >= v:
                                continue
                            e.wait_ge(sems[n], v)
                return body

            block.tensor(make("tensor"))
            block.vector(make("vector"))
            block.scalar(make("scalar"))
            block.gpsimd(make("gpsimd"))
            block.sync(make("sync"))


class Buf:
    def __init__(self, t, k):
        self.t = t
        self.k = k

    def __getitem__(self, idx):
        return self.t[idx]


def build_program():
    nc = bass.Bass("TRN2", target_bir_lowering=False)

    def din(name, shape, dt=F32):
        return nc.dram_tensor(name, list(shape), dt, kind="ExternalInput").ap()

    x_d = din("x", [NB * SEQ, D])
    ctx_d = din("ctx", [NB * CTX, D])
    cvec_d = din("cvec", [3, D])
    wmod_d = din("w_mod", [D, 6 * D])
    bmod_d = din("b_mod", [1, 6 * D])
    n1_d = din("norm1_w", [1, D])
    n2_d = din("norm2_w", [1, D])
    win_d = din("w_in", [D, 2832])
    bg_d = din("b_gates", [1, 16])
    conv_d = din("conv_qk", [3, D])
    qn_d = din("q_norm_w", [1, 64])
    kn_d = din("k_norm_w", [1, 64])
    sink_d = din("sink", [1, 8])
    mn_d = din("mlstm_norm_w", [1, 512])
    wout_d = din("w_out", [D, D])
    wr_d = din("w_router", [D, NEXP])
    if CFG["stop"] is None:
        wg_d = din("w_gate", [NEXP, D, 512])
        wu_d = din("w_up", [NEXP, D, 512])
        wd_d = din("w_down", [NEXP, 512, D])
    cos_d = din("rope_cos", [SEQ, 32])
    sin_d = din("rope_sin", [SEQ, 32])
    idb_d = din("ident_bf", [128, 128], BF16)
    idf_d = din("ident_f", [128, 128])
    trif_d = din("tri_f", [128, 128])
    trib_d = din("tri_b", [128, 128])
    nones_d = din("neg_ones", [128, 128])
    mskf_d = din("mask_f", [128, 128], BF16)
    mskb_d = din("mask_b", [128, 128], BF16)
    amlo_d = din("amask_lo", [128, 128], BF16)
    amhi_d = din("amask_hi", [128, 128], BF16)
    boff_d = din("boff", [64, 1])
    anti_d = din("anti_ident", [128, 128])
    out_d = nc.dram_tensor("out", [NB * SEQ, D], F32, kind="ExternalOutput").ap()
    hx2s_d = nc.dram_tensor("hx2s", [NB * SEQ, D], BF16).ap()
    modsc_d = nc.dram_tensor("modsc", [3, 6 * D], F32).ap()
    cdb_d = nc.dram_tensor("cdbs", [NT, 128, 4, 130], BF16).ap()

    with ExitStack() as es0:
        def sb0(name, shape, dt=F32):
            return Buf(es0.enter_context(nc.sbuf_tensor(name, list(shape), dt)), name)

        aff_all = sb0("aff_all", [128, NT // 2, 2, NB, NEXP])
        identb = sb0("identb", [128, 128], BF16)
        identf = sb0("identf", [128, 128])

        with ExitStack() as es:
            S = Sched(nc, "m")

            def sb(name, shape, dt=F32):
                return Buf(es.enter_context(nc.sbuf_tensor(name, list(shape), dt)), name)

            def ps(name, shape, dt=F32):
                return Buf(es.enter_context(nc.psum_tensor(name, list(shape), dt)), name)

            PB = [ps("mb%d" % i, [128, 512]) for i in range(4)]
            pbi = [0]

            def nextps():
                p = PB[pbi[0] % 4]
                pbi[0] += 1
                return p

            def dma(eng, out, in_, r, w, key, **kw):
                S.op(eng, lambda e: e.dma_start(out=out, in_=in_, **kw), reads=r, writes=w, dma=key)

            def mm(out, lhsT, rhs, start, stop, r, w):
                S.op("tensor", lambda e: e.matmul(out, lhsT, rhs, start=start, stop=stop), reads=r, writes=w)

            def tr(out, in_, ident, r, w):
                S.op("tensor", lambda e: e.transpose(out, in_, ident), reads=r, writes=w)

            def tt(eng, out, in0, in1, op, r, w):
                S.op(eng, lambda e: e.tensor_tensor(out=out, in0=in0, in1=in1, op=op), reads=r, writes=w)

            def cp(eng, out, in_, r, w):
                S.op(eng, lambda e: e.tensor_copy(out=out, in_=in_), reads=r, writes=w)

            PK = "prm0"
            S.group_keys.add(PK)

            def load_const(buf, src, eng="sync", **kw):
                dma(eng, buf[:], src, [], [buf.k], PK, **kw)

            load_const(identb, idb_d)
            load_const(identf, idf_d)
            c3 = sb("c3", [3, D]); load_const(c3, cvec_d)
            S.op("scalar", lambda e: e.activation(out=c3[:], in_=c3[:], func=AF.Silu), reads=[c3.k], writes=[c3.k])
            sT = sb("sT", [128, 8, 3])
            p = nextps()
            for kc in range(8):
                tr(p[:, kc * 4:kc * 4 + 3], c3[:, kc * 128:(kc + 1) * 128], identf[0:3, 0:3], [c3.k, identf.k], [p.k])
            cp("vector", sT[:], p[:, 0:32].rearrange("p (a b) -> p a b", a=8)[:, :, 0:3], [p.k], [sT.k])
            bm3 = sb("bm3", [3, 6 * D]); load_const(bm3, bmod_d[0].partition_broadcast(3))
            modrows = sb("modrows", [3, 6 * D])
            wmc = [sb("wmc%d" % i, [128, 8, 512]) for i in range(4)]
            for ci in range(12):
                wb = wmc[ci % 4]
                dma("sync", wb[:], wmod_d.rearrange("(c p) n -> p c n", p=128)[:, :, ci * 512:(ci + 1) * 512], [], [wb.k], wb.k)
                p = nextps()
                for kc in range(8):
                    mm(p[0:3, :], sT[:, kc, :], wb[:, kc, :], kc == 0, kc == 7, [sT.k, wb.k], [p.k])
                tt("vector", modrows[:, ci * 512:(ci + 1) * 512], p[0:3, :], bm3[:, ci * 512:(ci + 1) * 512], ALU.add,
                   [p.k, bm3.k], [modrows.k])
            dma("sync", modsc_d, modrows[:], [modrows.k], ["modsc"], modrows.k)

            S.emit()

        if CFG["stop"] == "mod0":
            return nc
        with ExitStack() as es:
            S = Sched(nc, "a")

            def sb(name, shape, dt=F32):
                return Buf(es.enter_context(nc.sbuf_tensor(name, list(shape), dt)), name)

            def ps(name, shape, dt=F32):
                return Buf(es.enter_context(nc.psum_tensor(name, list(shape), dt)), name)

            PB = [ps("pb%d" % i, [128, 512]) for i in range(7)]
            PT = ps("ptb", [128, 1024], BF16)
            class RR:
                def __init__(self, banks):
                    self.b = banks
                    self.i = 0

                def next(self):
                    p = self.b[self.i % len(self.b)]
                    self.i += 1
                    return p

            al_all = RR(PB)
            cur_al = [al_all]

            def nextps():
                return cur_al[0].next()

            def run(pairs, bg=None):
                live = list(pairs)
                while live:
                    for item in list(live):
                        cur_al[0] = item[1]
                        try:
                            next(item[0])
                        except StopIteration:
                            live.remove(item)
                    if bg:
                        cur_al[0] = al_all
                        try:
                            next(bg[0])
                        except StopIteration:
                            bg.pop(0)
                cur_al[0] = al_all

            def drain(bg):
                cur_al[0] = al_all
                while bg:
                    try:
                        next(bg[0])
                    except StopIteration:
                        bg.pop(0)

            def dma(eng, out, in_, r, w, key, **kw):
                S.op(eng, lambda e: e.dma_start(out=out, in_=in_, **kw), reads=r, writes=w, dma=key)

            def mm(out, lhsT, rhs, start, stop, r, w):
                S.op("tensor", lambda e: e.matmul(out, lhsT, rhs, start=start, stop=stop), reads=r, writes=w)

            def tr(out, in_, ident, r, w):
                S.op("tensor", lambda e: e.transpose(out, in_, ident), reads=r, writes=w)

            def act(out, in_, func, r, w, bias=None, scale=None, accum=None):
                kw = {}
                if bias is not None:
                    kw["bias"] = bias
                if scale is not None:
                    kw["scale"] = scale
                if accum is not None:
                    kw["accum_out"] = accum
                S.op("scalar", lambda e: e.activation(out=out, in_=in_, func=func, **kw), reads=r, writes=w)

            def tt(eng, out, in0, in1, op, r, w):
                S.op(eng, lambda e: e.tensor_tensor(out=out, in0=in0, in1=in1, op=op), reads=r, writes=w)

            def ts(eng, out, in0, s1, s2, op0, op1, r, w):
                if op1 is None:
                    S.op(eng, lambda e: e.tensor_scalar(out=out, in0=in0, scalar1=s1, scalar2=None, op0=op0), reads=r, writes=w)
                else:
                    S.op(eng, lambda e: e.tensor_scalar(out=out, in0=in0, scalar1=s1, scalar2=s2, op0=op0, op1=op1), reads=r, writes=w)

            def stt(out, in0, scalar, in1, op0, op1, r, w):
                S.op("vector", lambda e: e.scalar_tensor_tensor(out=out, in0=in0, scalar=scalar, in1=in1, op0=op0, op1=op1), reads=r, writes=w)

            def cp(eng, out, in_, r, w):
                if eng == "scalar":
                    S.op(eng, lambda e: e.copy(out=out, in_=in_), reads=r, writes=w)
                else:
                    S.op(eng, lambda e: e.tensor_copy(out=out, in_=in_), reads=r, writes=w)

            def red(out, in_, r, w, op=ALU.add):
                S.op("vector", lambda e: e.tensor_reduce(out=out, in_=in_, axis=AX.X, op=op), reads=r, writes=w)

            def recip(out, in_, r, w):
                S.op("vector", lambda e: e.reciprocal(out=out, in_=in_), reads=r, writes=w)

            def mset(eng, out, val, w):
                S.op(eng, lambda e: e.memset(out, val), writes=w)

            DBG = {}

            def dump(name, buf, ap, shape, dt=F32):
                if not CFG.get("dbg") or name in DBG:
                    return
                d = nc.dram_tensor("dbg_" + name, list(shape), dt, kind="ExternalOutput").ap()
                DBG[name] = d
                dma("sync", d, ap, [buf.k], ["dbg_" + name], "dbgk")

            PK = "prm"
            S.group_keys.update([PK])
            def load_const(buf, src, eng="sync", **kw):
                dma(eng, buf[:], src, [], [buf.k], PK, **kw)

            trif = sb("trif", [128, 128]); load_const(trif, trif_d)
            trib = sb("trib", [128, 128]); load_const(trib, trib_d)
            nones = sb("nones", [128, 128]); load_const(nones, nones_d)
            mskf = sb("mskf", [128, 128], BF16); load_const(mskf, mskf_d)
            mskb = sb("mskb", [128, 128], BF16); load_const(mskb, mskb_d)
            amlo = sb("amlo", [128, 128], BF16); load_const(amlo, amlo_d)
            amhi = sb("amhi", [128, 128], BF16); load_const(amhi, amhi_d)
            cosT = sb("cosT", [128, NT, 32]); load_const(cosT, cos_d.rearrange("(n p) f -> p n f", p=128))
            sinT = sb("sinT", [128, NT, 32]); load_const(sinT, sin_d.rearrange("(n p) f -> p n f", p=128))
            bgB = sb("bgB", [128, 16]); load_const(bgB, bg_d[0].partition_broadcast(128))
            qnB = sb("qnB", [128, 64]); load_const(qnB, qn_d[0].partition_broadcast(128))
            knB = sb("knB", [128, 64]); load_const(knB, kn_d[0].partition_broadcast(128))
            mnB = sb("mnB", [128, 512]); load_const(mnB, mn_d[0].partition_broadcast(128))
            esink = sb("esink", [128, 8]); load_const(esink, sink_d[0].partition_broadcast(128))
            act(esink[:], esink[:], AF.Exp, [esink.k], [esink.k])
            cwT = sb("cwT", [128, 8, 3])
            for jj in range(3):
                S.op("sync", lambda e, jj=jj: e.dma_start(out=cwT[:, :, jj], in_=conv_d[jj].rearrange("(c p) -> p c", p=128),
                                                         allow_slow_non_contiguous=True), writes=[cwT.k], dma="cwk")
            if CFG["stop"] == "s1":
                S.emit()
                return nc
            wr = sb("wr", [128, 8, NEXP]); load_const(wr, wr_d.rearrange("(c p) n -> p c n", p=128))
            win = sb("win", [128, 8, 2832], BF16)
            hTB = sb("hTB", [128, 8, 512], BF16)
            hTB.k = ("MULTI",) + tuple(("hTB", i_, j_) for i_ in range(4) for j_ in range(4))
            hTBf = hTB[:].rearrange("p a b -> p (a b)").bitcast(F32)
            stg_ap = [hTBf[:, i_ * 512:(i_ + 1) * 512] for i_ in range(4)]
            stg_k = [("MULTI",) + tuple(("hTB", i_, j_) for j_ in range(4)) for i_ in range(4)]
            stc1 = [0]
            for kc in range(8):
                for c0 in range(0, 2832, 512):
                    w_ = min(512, 2832 - c0)
                    si = stc1[0] % 4
                    eng = ("scalar", "gpsimd", "vector")[stc1[0] % 3]
                    stc1[0] += 1
                    dma("sync", stg_ap[si][:, 0:w_], win_d[kc * 128:(kc + 1) * 128, c0:c0 + w_], [], [stg_k[si]], "stg%d" % si)
                    cp(eng, win[:, kc, c0:c0 + w_], stg_ap[si][:, 0:w_], [stg_k[si]], [win.k])
            wout = sb("wout", [128, 8, D], BF16)

            def load_wout_scaled():
                for kc in range(8):
                    for c0 in range(0, D, 512):
                        si = stc1[0] % 4
                        stc1[0] += 1
                        dma("sync", stg_ap[si], wout_d[kc * 128:(kc + 1) * 128, c0:c0 + 512], [], [stg_k[si]], "stg%d" % si)
                        tt("vector", wout[:, kc, c0:c0 + 512], stg_ap[si], t1[:, c0:c0 + 512], ALU.mult, [stg_k[si], t1.k], [wout.k])

            if CFG["stop"] == "s2":
                S.emit()
                return nc
            A1 = sb("A1", [128, D]); B1 = sb("B1", [128, D])
            A2 = sb("A2", [128, D]); B2 = sb("B2", [128, D])
            t1 = sb("t1", [128, D])
            t1.k = ("MULTI", "t1a", "t1b")

            def load_mod(row, want):
                def bc(i):
                    return modsc_d[row, i * D:(i + 1) * D].partition_broadcast(128)
                if "A1" in want:
                    dma("sync", t1[:], bc(1), [], [t1.k], "t1d")
                    dma("sync", A1[:], n1_d[0].partition_broadcast(128), [], [A1.k], A1.k)
                    stt(A1[:], t1[:], 1.0, A1[:], ALU.add, ALU.mult, [t1.k, A1.k], [A1.k])
                if "B1" in want:
                    dma("sync", B1[:], bc(0), [], [B1.k], B1.k)
                if "G1" in want:
                    dma("sync", t1[:], bc(2), [], [t1.k], "t1d")
                if "A2" in want:
                    dma("sync", t1[:], bc(4), [], [t1.k], "t1d")
                    dma("sync", A2[:], n2_d[0].partition_broadcast(128), [], [A2.k], A2.k)
                    stt(A2[:], t1[:], 1.0, A2[:], ALU.add, ALU.mult, [t1.k, A2.k], [A2.k])
                if "B2" in want:
                    dma("sync", B2[:], bc(3), [], [B2.k], B2.k)

            xt = [sb("xt%d" % i, [128, D]) for i in range(2)]
            hx = [sb("hx%d" % i, [128, D], BF16) for i in range(2)]
            hT = sb("hT", [128, 8, 512], BF16)
            hT.k = ("MULTI",) + tuple(("hT", j_) for j_ in range(4))
            HB = [hT, hTB]

            def tile_keys(hb_, j_):
                if hb_ is hT:
                    return [("hT", j_)]
                return [("hTB", i_, j_) for i_ in range(4)]
            st4 = [sb("st4_%d" % i, [128, 4]) for i in range(2)]
            HS = sb("HS", [128, 8, 2 * NG])
            KT = sb("KT", [128, SEQ + CTX], BF16)
            V = sb("V", [128, NT + 2, 2, 65], BF16)
            KT.k = ("MULTI",) + tuple(("KT", i_) for i_ in range(NT + 2))
            V.k = ("MULTI",) + tuple(("V", i_) for i_ in range(NT + 2))
            mset("vector", V[:], 1.0, [V.k])
            CdS = [sb("CdS%d" % i, [128, 4, 130], BF16) for i in range(2)]
            CdL = [sb("CdL%d" % i, [128, 4, 130], BF16) for i in range(2)]
            Sst = {"f": sb("Sf", [128, 4, 130]), "b": sb("Sb", [128, 4, 130])}
            Sdec = sb("Sdec", [128, 4, 130])
            CdF = sb("CdF", [128, 4, 130], BF16)
            mqT = sb("mqT", [128, 4, 512], BF16)
            mkT = sb("mkT", [128, 4, 512], BF16)
            mqT.k = ("MULTI",) + tuple(("mqT", c_) for c_ in range(4))
            mkT.k = ("MULTI",) + tuple(("mkT", c_) for c_ in range(4))
            Kt = sb("Kt", [128, 4, 512], BF16)
            qk_sq = sb("qk_sq", [128, 512])
            qk_t = sb("qk_t", [128, 512])
            qk_a = sb("qk_a", [128, 256])
            qk_b = sb("qk_b", [128, 256])
            qk_o = sb("qk_o", [128, 512], BF16)
            qk_ss = sb("qk_ss", [128, 10])
            QT = sb("QT", [128, 4, 128], BF16)
            PTs = sb("PTs", [128, 5, 512], BF16)
            arec = sb("arec", [128, 4])
            hrs = sb("hrs", [128, 4])
            mixr = [sb("mix%d" % i, [128, D], BF16) for i in range(2)]
            cv = Buf(t1.t, t1.k)
            mixT = sb("mixT", [128, 8, 128], BF16)
            mvs = sb("mvs", [128, 512])
            sig = sb("sig", [128, 512])
            gt = sb("gt", [128, 16])
            l1 = sb("l1", [128, 8])
            g8 = sb("g8", [128, 8])
            ws8 = sb("ws8", [128, 8])
            E8 = sb("E8", [128, 8])
            dec4 = sb("dec4", [128, 4])
            Vw = {"f": sb("Vwf", [128, 4, 130], BF16), "b": sb("Vwb", [128, 4, 130], BF16)}
            Sm = {"f": sb("Smf", [128, 4, 128], BF16), "b": sb("Smb", [128, 4, 128], BF16)}
            sc8 = sb("sc8", [128, 8])
            sc8n = sb("sc8n", [128, 8])
            hs = sb("hs", [128, 512])
            xn = [sb("xn%d" % i, [128, D]) for i in range(1)]
            h2 = t1
            h2b = [sb("h2b%d" % i, [128, D], BF16) for i in range(1)]
            h2T = sb("h2T", [128, 8, 128])
            rt = sb("rt", [128, 16])
            rs1 = sb("rs1", [128, 1])
            rs2 = sb("rs2", [128, 1])
            if CFG["stop"] == "s3":
                S.emit()
                return nc
            ring = {"xt": 0, "hx": 0, "xn": 0, "h2b": 0, "st": 0, "cds": 0, "cdl": 0}

            def rr(name, lst):
                b = lst[ring[name] % len(lst)]
                ring[name] += 1
                return b

            def norm_mod(src_ap_rows, npart, A, Bm, out_buf, xbufs, xname):
                xb = rr(xname, xbufs)
                dma("sync", xb[0:npart, :], src_ap_rows, [], [xb.k], xb.k)
                s4 = rr("st", st4)
                act(out_buf[0:npart, :], xb[0:npart, :], AF.Square, [xb.k], [out_buf.k, s4.k], accum=s4[0:npart, 0:1])
                act(s4[0:npart, 1:2], s4[0:npart, 0:1], AF.Ln, [s4.k], [s4.k], bias=EPS, scale=1.0 / D)
                act(s4[0:npart, 2:3], s4[0:npart, 1:2], AF.Exp, [s4.k], [s4.k], scale=-0.5)
                stt(out_buf[0:npart, :], xb[0:npart, :], s4[0:npart, 2:3], A[0:npart, :], ALU.mult, ALU.mult,
                    [xb.k, s4.k, A.k], [out_buf.k])
                tt("vector", out_buf[0:npart, :], out_buf[0:npart, :], Bm[0:npart, :], ALU.add, [out_buf.k, Bm.k], [out_buf.k])
                return xb

            def gen_stage_tile(src_rows_fn, j, A, Bm, hb_=None):
                hb_ = hT if hb_ is None else hb_
                hb = rr("hx", hx)
                xb = rr("xt", xt)
                dma("sync", xb[:], src_rows_fn(j), [], [xb.k], xb.k)
                s4 = rr("st", st4)
                yield
                act(hb[:], xb[:], AF.Square, [xb.k], [hb.k, s4.k], accum=s4[:, 0:1])
                act(s4[:, 1:2], s4[:, 0:1], AF.Ln, [s4.k], [s4.k], bias=EPS, scale=1.0 / D)
                act(s4[:, 2:3], s4[:, 1:2], AF.Exp, [s4.k], [s4.k], scale=-0.5)
                yield
                stt(hb[:], xb[:], s4[:, 2:3], A[:], ALU.mult, ALU.mult, [xb.k, s4.k, A.k], [hb.k])
                yield
                tt("vector", hb[:], hb[:], Bm[:], ALU.add, [hb.k, Bm.k], [hb.k])
                yield
                for kc in range(8):
                    tr(PT[:, kc * 128:(kc + 1) * 128], hb[:, kc * 128:(kc + 1) * 128], identb[:], [hb.k, identb.k], [PT.k])
                cp("scalar", hb_[:, :, j * 128:(j + 1) * 128], PT[:].rearrange("p (a b) -> p a b", a=8), [PT.k], tile_keys(hb_, j))
                yield

            def stage_a(src_rows_fn, ntile, A, Bm, hb_=None):
                for j0 in range(0, ntile, 2):
                    run([(gen_stage_tile(src_rows_fn, j, A, Bm, hb_), al_all) for j in range(j0, min(j0 + 2, ntile))])

            def gen_feat_chunk(col0, cc, N, dst, cw0, hprev, hnext, hb_=None):
                hb_ = hT if hb_ is None else hb_
                HTK = [hb_.k]
                p = nextps()
                base = (cc % 2) * 512
                ck = "t1a" if cc % 2 == 0 else "t1b"
                cvv = lambda a_, b_: t1[:, base + a_:base + b_]
                for kc in range(8):
                    mm(p[:, 0:N], win[:, kc, col0 + cc * 128: col0 + (cc + 1) * 128], hb_[:, kc, 0:N], kc == 0, kc == 7,
                       [win.k] + HTK, [p.k])
                yield
                act(cvv(0, N), p[:, 0:N], AF.Identity, [p.k, cwT.k], [ck], scale=cwT[:, cw0 + cc, 1:2])
                yield
                stt(cvv(1, N), p[:, 0:N - 1], cwT[:, cw0 + cc, 0:1], cvv(1, N), ALU.mult, ALU.add, [p.k, cwT.k, ck], [ck])
                stt(cvv(0, N - 1), p[:, 1:N], cwT[:, cw0 + cc, 2:3], cvv(0, N - 1), ALU.mult, ALU.add, [p.k, cwT.k, ck], [ck])
                if hprev is not None:
                    stt(cvv(0, 1), HS[:, cw0 + cc, hprev:hprev + 1], cwT[:, cw0 + cc, 0:1], cvv(0, 1), ALU.mult, ALU.add,
                        [HS.k, cwT.k, ck], [ck])
                if hnext is not None:
                    stt(cvv(N - 1, N), HS[:, cw0 + cc, hnext:hnext + 1], cwT[:, cw0 + cc, 2:3], cvv(N - 1, N), ALU.mult, ALU.add,
                        [HS.k, cwT.k, ck], [ck])
                yield
                act(dst[:, cc, 0:N], cvv(0, N), AF.Silu, [ck], [dst.k[1 + cc]])
                yield

            def feat_proj(col0, nchunks, N, dst, cw0, hprev, hnext, hb_=None):
                for c0 in range(0, nchunks, 2):
                    run([(gen_feat_chunk(col0, cc, N, dst, cw0, hprev, hnext, hb_), al_all) for cc in range(c0, c0 + 2)])

            def k_tokmajor(ntile):
                for j in range(ntile):
                    for h in range(4):
                        tr(PT[:, h * 128:(h + 1) * 128], mkT[:, h, j * 128:(j + 1) * 128], identb[:], [mkT.k, identb.k], [PT.k])
                    cp("vector", Kt[:, j, :], PT[:, 0:512], [PT.k], [Kt.k])

            def qk_norm_rope(src, srck, H, wB, tile_n, out_ap):
                W = H * 64
                s3 = lambda a: a.rearrange("p (h d) -> p h d", h=H)
                act(qk_sq[:, 0:W], src, AF.Square, [srck], [qk_sq.k])
                red(qk_ss[:, 0:H], s3(qk_sq[:, 0:W]), [qk_sq.k], [qk_ss.k])
                act(qk_ss[:, 0:H], qk_ss[:, 0:H], AF.Ln, [qk_ss.k], [qk_ss.k], bias=EPS, scale=1.0 / 64)
                act(qk_ss[:, 0:H], qk_ss[:, 0:H], AF.Exp, [qk_ss.k], [qk_ss.k], scale=-0.5)
                tt("vector", s3(qk_t[:, 0:W]), s3(src), qk_ss[:, 0:H].unsqueeze(2).to_broadcast([128, H, 64]), ALU.mult,
                   [srck, qk_ss.k], [qk_t.k])
                wbc = wB[:, :].unsqueeze(1).to_broadcast([128, H, 64])
                if tile_n is None:
                    tt("gpsimd", s3(out_ap), s3(qk_t[:, 0:W]), wbc, ALU.mult, [qk_t.k, wB.k], [out_ap.tensor.name if False else "qk_o"])
                    return
                tt("gpsimd", s3(qk_t[:, 0:W]), s3(qk_t[:, 0:W]), wbc, ALU.mult, [qk_t.k, wB.k], [qk_t.k])
                x1 = s3(qk_t[:, 0:W])[:, :, 0:32]
                x2 = s3(qk_t[:, 0:W])[:, :, 32:64]
                cb = cosT[:, tile_n, :].unsqueeze(1).to_broadcast([128, H, 32])
                sbn = sinT[:, tile_n, :].unsqueeze(1).to_broadcast([128, H, 32])
                h3 = lambda a: a.rearrange("p (h d) -> p h d", h=H)
                a_ = h3(qk_a[:, 0:H * 32]); b_ = h3(qk_b[:, 0:H * 32])
                o3 = s3(out_ap)
                tt("vector", a_, x1, cb, ALU.mult, [qk_t.k, cosT.k], [qk_a.k])
                tt("gpsimd", b_, x2, sbn, ALU.mult, [qk_t.k, sinT.k], [qk_b.k])
                tt("vector", o3[:, :, 0:32], a_, b_, ALU.subtract, [qk_a.k, qk_b.k], ["qk_o"])
                tt("vector", a_, x2, cb, ALU.mult, [qk_t.k, cosT.k, "qk_o"], [qk_a.k])
                tt("gpsimd", b_, x1, sbn, ALU.mult, [qk_t.k, sinT.k, "qk_o"], [qk_b.k])
                tt("vector", o3[:, :, 32:64], a_, b_, ALU.add, [qk_a.k, qk_b.k], ["qk_o"])

            def gates_prep(psg_ap, psgk):
                tt("vector", gt[:], psg_ap, bgB[:], ALU.add, [psgk, bgB.k], [gt.k])
                g3 = gt[:].rearrange("p (a b) -> p a b", a=2)
                act(l1[:].rearrange("p (a b) -> p a b", a=2), g3[:, :, 4:8], AF.Exp, [gt.k], [l1.k], scale=-1.0)
                act(l1[:], l1[:], AF.Ln, [l1.k], [l1.k], bias=1.0)
                cp("gpsimd", g8[:].rearrange("p (a b) -> p a b", a=2), g3[:, :, 0:4], [gt.k], [g8.k])

            def chunk_weights(dirs, need_tot):
                p = nextps()
                if "f" in dirs:
                    mm(p[:, 0:4], trif[:], l1[:, 0:4], True, True, [trif.k, l1.k], [p.k])
                if "b" in dirs:
                    mm(p[:, 4:8], trib[:], l1[:, 4:8], True, True, [trib.k, l1.k], [p.k])
                c0 = 0 if need_tot == "f" else 4
                mm(p[:, 8:12], nones[:], l1[:, c0:c0 + 4], True, True, [nones.k, l1.k], [p.k])
                lo, hi = (0, 8) if len(dirs) == 2 else ((0, 4) if "f" in dirs else (4, 8))
                tt("vector", ws8[:, lo:hi], g8[:, lo:hi], p[:, lo:hi], ALU.subtract, [g8.k, p.k], [ws8.k])
                act(ws8[:, lo:hi], ws8[:, lo:hi], AF.Exp, [ws8.k], [ws8.k])
                act(E8[:, lo:hi], p[:, lo:hi], AF.Exp, [p.k], [E8.k])
                act(dec4[:], p[:, 8:12], AF.Exp, [p.k], [dec4.k])

            def make_vw(d):
                c0 = 0 if d == "f" else 4
                tt("vector", Vw[d][:, :, 0:128], mvs[:].rearrange("p (h d) -> p h d", h=4),
                   ws8[:, c0:c0 + 4].unsqueeze(2).to_broadcast([128, 4, 128]), ALU.mult, [mvs.k, ws8.k], [Vw[d].k])
                cp("gpsimd", Vw[d][:, :, 128:129], ws8[:, c0:c0 + 4].unsqueeze(2), [ws8.k], [Vw[d].k])

            def state_update(d, j, cd_out_ap, cd_key):
                St = Sst[d]
                decb = dec4[:].unsqueeze(2).to_broadcast([128, 4, 130])
                tt("vector", Sdec[:], St[:], decb, ALU.mult, [St.k, dec4.k], [Sdec.k])
                ts("vector", cd_out_ap, Sdec[:], QS, None, ALU.mult, None, [Sdec.k], [cd_key])
                pa = nextps(); pb = nextps()
                for h in range(4):
                    pp = pa if h < 2 else pb
                    o = pp[:, 0:260].rearrange("p (a b) -> p a b", a=2)[:, h % 2, 0:129]
                    mm(o, Kt[:, j, h * 128:(h + 1) * 128], Vw[d][:, h, 0:129], True, True, [Kt.k, Vw[d].k], [pp.k])
                tt("vector", St[:, 0:2, 0:129], Sdec[:, 0:2, 0:129], pa[:, 0:260].rearrange("p (a b) -> p a b", a=2)[:, :, 0:129],
                   ALU.add, [Sdec.k, pa.k], [St.k])
                tt("vector", St[:, 2:4, 0:129], Sdec[:, 2:4, 0:129], pb[:, 0:260].rearrange("p (a b) -> p a b", a=2)[:, :, 0:129],
                   ALU.add, [Sdec.k, pb.k], [St.k])

            def p1_tile(j, kv_col, v_idx, rope_n, dirs_states, cdb_idx):
                pX = nextps(); pY = nextps()
                for kc in range(8):
                    mm(pX[:], hT[:, kc, j * 128:(j + 1) * 128], win[:, kc, C_AK:C_AK + 512], kc == 0, kc == 7, [hT.k, win.k], [pX.k])
                for kc in range(8):
                    mm(pY[:, 0:272], hT[:, kc, j * 128:(j + 1) * 128], win[:, kc, C_AK + 512:C_AK + 784], kc == 0, kc == 7,
                       [hT.k, win.k], [pY.k])
                qk_norm_rope(pX[:, 0:128], pX.k, 2, knB, rope_n, qk_o[:, 0:128])
                tr(PT[:, 0:128], qk_o[:, 0:128], identb[:], ["qk_o", identb.k], [PT.k])
                cp("scalar", KT[:, kv_col:kv_col + 128], PT[:, 0:128], [PT.k], [KT.k])
                cp("scalar", V[:, v_idx, :, 0:64], pX[:, 128:256].rearrange("p (a b) -> p a b", a=2), [pX.k], [V.k])
                cp("scalar", mvs[:, 0:256], pX[:, 256:512], [pX.k], [mvs.k])
                cp("scalar", mvs[:, 256:512], pY[:, 0:256], [pY.k], [mvs.k])
                gates_prep(pY[:, 256:272], pY.k)
                for d in dirs_states:
                    chunk_weights([d], d)
                    make_vw(d)
                    if d == "b" and cdb_idx is not None:
                        cs = rr("cds", CdS)
                        state_update(d, j, cs[:], cs.k)
                        dma("sync", cdb_d[cdb_idx], cs[:], [cs.k], [("cdb", cdb_idx)], cs.k)
                    else:
                        state_update(d, j, CdF[:], CdF.k)

            def p1_group_ctx(b):
                stage_a(lambda j: ctx_d[b * CTX + j * 128: b * CTX + (j + 1) * 128, :], 2, A1, B1)
                feat_proj(C_MK, 4, 256, mkT, 4, None, None)
                k_tokmajor(2)
                for j in (0, 1):
                    p1_tile(j, SEQ + j * 128, NT + j, None, ["f"], None)
                for j in (1, 0):
                    p1_tile(j, SEQ + j * 128, NT + j, None, ["b"], None)

            def gen_p1_proj(j, pX, pY, hb_):
                for kc in range(8):
                    mm(pX[:], hb_[:, kc, j * 128:(j + 1) * 128], win[:, kc, C_AK:C_AK + 512], kc == 0, kc == 7, [hb_.k, win.k], [pX.k])
                yield
                for kc in range(8):
                    mm(pY[:, 0:272], hb_[:, kc, j * 128:(j + 1) * 128], win[:, kc, C_AK + 512:C_AK + 784], kc == 0, kc == 7,
                       [hb_.k, win.k], [pY.k])
                yield

            def gen_p1_k(j, pX, kv_col, v_idx, rope_n):
                cp("scalar", V[:, v_idx, :, 0:64], pX[:, 128:256].rearrange("p (a b) -> p a b", a=2), [pX.k], [V.k[1 + v_idx]])
                yield
                qk_norm_rope(pX[:, 0:128], pX.k, 2, knB, rope_n, qk_o[:, 0:128])
                yield
                tr(PT[:, 0:128], qk_o[:, 0:128], identb[:], ["qk_o", identb.k], [PT.k])
                cp("scalar", KT[:, kv_col:kv_col + 128], PT[:, 0:128], [PT.k], [KT.k[1 + v_idx]])
                yield

            def gen_p1_s(j, pX, pY, d, cdb_idx):
                cp("scalar", mvs[:, 0:256], pX[:, 256:512], [pX.k], [mvs.k])
                cp("scalar", mvs[:, 256:512], pY[:, 0:256], [pY.k], [mvs.k])
                gates_prep(pY[:, 256:272], pY.k)
                yield
                chunk_weights([d], d)
                yield
                make_vw(d)
                yield
                cs = rr("cds", CdS)
                state_update(d, j, cs[:], cs.k)
                dma("sync", cdb_d[cdb_idx], cs[:], [cs.k], [("cdb", cdb_idx)], cs.k)
                yield

            def xrows(b, g):
                return lambda j: x_d[b * SEQ + g * 512 + j * 128: b * SEQ + g * 512 + (j + 1) * 128, :]

            def p1_group(b, g, gnext):
                hb_ = HB[g % 2]
                feat_proj(C_MK, 4, 512, mkT, 4, (NG + g - 1) if g > 0 else None, (g + 1) if g < NG - 1 else None, hb_)
                k_tokmajor(4)
                al_S = RR(PB[4:7])
                pairs = [(PB[0], PB[1]), (PB[2], PB[3])]
                js = (3, 2, 1, 0)
                bg = []
                if gnext is not None:
                    bg = [gen_stage_tile(xrows(b, gnext), i_, A1, B1, HB[gnext % 2]) for i_ in range(4)]
                run([(gen_p1_proj(js[0], pairs[0][0], pairs[0][1], hb_), al_all)])
                for i, j in enumerate(js):
                    n = g * 4 + j
                    pX, pY = pairs[i % 2]
                    gens = [(gen_p1_k(j, pX, n * 128, n, n), al_S), (gen_p1_s(j, pX, pY, "b", n), al_S)]
                    if i + 1 < len(js):
                        gens.append((gen_p1_proj(js[i + 1], pairs[(i + 1) % 2][0], pairs[(i + 1) % 2][1], hb_), al_all))
                    run(gens, bg)
                drain(bg)

            def p0(b):
                hb = rr("hx", hx)
                xb = rr("xt", xt)
                xv = x_d[b * SEQ:(b + 1) * SEQ, :].rearrange("(g t) d -> t g d", t=512)
                dma("sync", xb[0:NG, :], xv[0], [], [xb.k], xb.k)
                dma("sync", xb[NG:2 * NG, :], xv[511], [], [xb.k], xb.k)
                s4 = rr("st", st4)
                act(hb[0:2 * NG, :], xb[0:2 * NG, :], AF.Square, [xb.k], [hb.k, s4.k], accum=s4[0:2 * NG, 0:1])
                act(s4[0:2 * NG, 1:2], s4[0:2 * NG, 0:1], AF.Ln, [s4.k], [s4.k], bias=EPS, scale=1.0 / D)
                act(s4[0:2 * NG, 2:3], s4[0:2 * NG, 1:2], AF.Exp, [s4.k], [s4.k], scale=-0.5)
                stt(t1[0:2 * NG, :], xb[0:2 * NG, :], s4[0:2 * NG, 2:3], A1[0:2 * NG, :], ALU.mult, ALU.mult, [xb.k, s4.k, A1.k], [t1.k])
                tt("gpsimd", hb[0:2 * NG, :], t1[0:2 * NG, :], B1[0:2 * NG, :], ALU.add, [t1.k, B1.k], [hb.k])
                for kc in range(8):
                    tr(PT[:, kc * 128:kc * 128 + 2 * NG], hb[0:2 * NG, kc * 128:(kc + 1) * 128], identb[0:2 * NG, 0:2 * NG], [hb.k, identb.k], [PT.k])
                cp("scalar", hT[:, :, 0:2 * NG], PT[:].rearrange("p (a b) -> p a b", a=8)[:, :, 0:2 * NG], [PT.k], [hT.k])
                p = nextps()
                for cc in range(8):
                    for kc in range(8):
                        mm(p[:, cc * 2 * NG:(cc + 1) * 2 * NG], win[:, kc, C_MQ + cc * 128:C_MQ + (cc + 1) * 128], hT[:, kc, 0:2 * NG],
                           kc == 0, kc == 7, [win.k, hT.k], [p.k])
                cp("vector", HS[:], p[:, 0:16 * NG].rearrange("p (a b) -> p a b", a=8), [p.k], [HS.k])

            def gen_attention(n):
                blocks = []
                if n > 0:
                    blocks.append((n - 1) * 128)
                blocks.append(n * 128)
                if n < NT - 1:
                    blocks.append((n + 1) * 128)
                blocks += [SEQ, SEQ + 128]
                for kv in range(2):
                    pr = slice(64 * kv, 64 * kv + 64)
                    for bi, col in enumerate(blocks):
                        p = nextps()
                        mm(p[:], KT[pr, col:col + 128], QT[pr, :, :], True, True, [KT.k, QT.k], [p.k])
                        act(PTs[:, bi, :], p[:], AF.Exp, [p.k], [PTs.k], scale=0.125)
                        if col == (n - 1) * 128:
                            m = amlo
                        elif col == (n + 1) * 128 and col < SEQ:
                            m = amhi
                        else:
                            m = None
                        if m is not None:
                            tt("gpsimd", PTs[:, bi, :].rearrange("p (g q) -> p g q", g=4),
                               PTs[:, bi, :].rearrange("p (g q) -> p g q", g=4),
                               m[:, :].unsqueeze(1).to_broadcast([128, 4, 128]), ALU.mult, [PTs.k, m.k], [PTs.k])
                        if bi == 2 or bi == len(blocks) - 1:
                            yield
                    po = nextps()
                    for gi in range(4):
                        for bi, col in enumerate(blocks):
                            vi = col // 128
                            mm(po[:, gi * 65:(gi + 1) * 65], PTs[:, bi, gi * 128:(gi + 1) * 128], V[:, vi, kv, :],
                               bi == 0, bi == len(blocks) - 1, [PTs.k, V.k], [po.k])
                        if gi == 3:
                            yield
                    po3 = po[:, 0:260].rearrange("p (g c) -> p g c", g=4)
                    tt("vector", arec[:], po3[:, :, 64], esink[:, kv * 4:kv * 4 + 4], ALU.add, [po.k, esink.k], [arec.k])
                    recip(arec[:], arec[:], [arec.k], [arec.k])
                    tt("vector", mixr[n % 2][:, kv * 256:(kv + 1) * 256].rearrange("p (g c) -> p g c", g=4), po3[:, :, 0:64],
                       arec[:].unsqueeze(2).to_broadcast([128, 4, 64]), ALU.mult, [po.k, arec.k], ["mixA%d" % (n % 2)])
                    yield

            def gen_head(b, g, j):
                hb_ = HB[g % 2]
                n = g * 4 + j
                tsl = slice(j * 128, (j + 1) * 128)
                pq = nextps(); po_ = nextps(); pv = nextps(); pg = nextps()
                for (pp, c0, w) in ((pq, C_AQ, 512), (pv, C_MV, 512), (pg, C_GT, 16), (po_, C_MO, 512)):
                    for kc in range(8):
                        mm(pp[:, 0:w], hb_[:, kc, tsl], win[:, kc, c0:c0 + w], kc == 0, kc == 7, tile_keys(hb_, j) + [win.k], [pp.k])
                    yield
                qk_norm_rope(pq[:], pq.k, 8, qnB, n, qk_o[:, 0:512])
                yield
                for pi in range(4):
                    tr(PT[:, pi * 128:(pi + 1) * 128], qk_o[:, pi * 128:(pi + 1) * 128], identb[:], ["qk_o", identb.k], [PT.k])
                cp("scalar", QT[:], PT[:, 0:512].rearrange("p (a b) -> p a b", a=4), [PT.k], [QT.k])
                yield
                cp("scalar", mvs[:], pv[:], [pv.k], [mvs.k])
                gates_prep(pg[:, 0:16], pg.k)
                yield
                act(sig[:], po_[:], AF.Exp, [po_.k], [sig.k], scale=-1.0)
                act(sig[:], sig[:], AF.Ln, [sig.k], [sig.k], bias=1.0)
                act(sig[:], sig[:], AF.Exp, [sig.k], [sig.k], scale=-1.0)
                tt("gpsimd", sig[:], sig[:], mnB[:], ALU.mult, [sig.k, mnB.k], [sig.k])
                yield

            def gen_mlstm(b, g, j):
                n = g * 4 + j
                tsl = slice(j * 128, (j + 1) * 128)
                chunk_weights(["f", "b"], "f")
                yield
                make_vw("f"); make_vw("b")
                pS = nextps()
                for h in range(4):
                    mm(pS[:, h * 128:(h + 1) * 128], mkT[:, h, tsl], mqT[:, h, tsl], True, True, [mkT.k, mqT.k], [pS.k])
                pS3 = pS[:].rearrange("p (h t) -> p h t", h=4)
                tt("vector", Sm["f"][:], pS3, mskf[:, :].unsqueeze(1).to_broadcast([128, 4, 128]), ALU.mult, [pS.k, mskf.k], [Sm["f"].k])
                tt("vector", Sm["b"][:], pS3, mskb[:, :].unsqueeze(1).to_broadcast([128, 4, 128]), ALU.mult, [pS.k, mskb.k], [Sm["b"].k])
                yield
                state_update("f", j, CdF[:], CdF.k)
                yield
                cl = rr("cdl", CdL)
                dma("sync", cl[:], cdb_d[n], [("cdb", n)], [cl.k], cl.k)
                for di, d in enumerate(("f", "b")):
                    pa = nextps(); pb = nextps()
                    for h in range(4):
                        pp = pa if h < 2 else pb
                        o = pp[:, 0:260].rearrange("p (a b) -> p a b", a=2)[:, h % 2, 0:129]
                        mm(o, Sm[d][:, h, :], Vw[d][:, h, 0:129], True, False, [Sm[d].k, Vw[d].k], [pp.k])
                        if d == "f":
                            mm(o, mqT[:, h, tsl], CdF[:, h, 0:129], False, True, [mqT.k, CdF.k], [pp.k])
                        else:
                            mm(o, mqT[:, h, tsl], cl[:, h, 0:129], False, True, [mqT.k, cl.k], [pp.k])
                    yield
                    c4 = di * 4
                    for half, pp in enumerate((pa, pb)):
                        c0 = c4 + half * 2
                        den = pp[:, 0:260].rearrange("p (a b) -> p a b", a=2)[:, :, 128]
                        tt("vector", sc8[:, c0:c0 + 2], den, E8[:, c0:c0 + 2], ALU.mult, [pp.k, E8.k], [sc8.k])
                    ts("vector", sc8n[:, c4:c4 + 4], sc8[:, c4:c4 + 4], -1.0, None, ALU.mult, None, [sc8.k], [sc8n.k])
                    tt("vector", sc8[:, c4:c4 + 4], sc8[:, c4:c4 + 4], sc8n[:, c4:c4 + 4], ALU.max, [sc8.k, sc8n.k], [sc8.k])
                    ts("vector", sc8[:, c4:c4 + 4], sc8[:, c4:c4 + 4], 1.0, None, ALU.max, None, [sc8.k], [sc8.k])
                    recip(sc8[:, c4:c4 + 4], sc8[:, c4:c4 + 4], [sc8.k], [sc8.k])
                    tt("vector", sc8[:, c4:c4 + 4], sc8[:, c4:c4 + 4], E8[:, c4:c4 + 4], ALU.mult, [sc8.k, E8.k], [sc8.k])
                    yield
                    for h in range(4):
                        pp = pa if h < 2 else pb
                        src = pp[:, 0:260].rearrange("p (a b) -> p a b", a=2)[:, h % 2, 0:128]
                        if d == "f":
                            ts("vector", hs[:, h * 128:(h + 1) * 128], src, sc8[:, h:h + 1], None, ALU.mult, None, [pp.k, sc8.k], [hs.k])
                        else:
                            stt(hs[:, h * 128:(h + 1) * 128], src, sc8[:, 4 + h:5 + h], hs[:, h * 128:(h + 1) * 128], ALU.mult, ALU.add,
                                [pp.k, sc8.k, hs.k], [hs.k])
                    yield
                for h in range(4):
                    act(Sm["f"][:, h, :], hs[:, h * 128:(h + 1) * 128], AF.Square, [hs.k], [Sm["f"].k, hrs.k], accum=hrs[:, h:h + 1])
                act(hrs[:], hrs[:], AF.Ln, [hrs.k], [hrs.k], bias=EPS, scale=1.0 / 128)
                act(hrs[:], hrs[:], AF.Exp, [hrs.k], [hrs.k], scale=-0.5)
                yield
                tt("vector", hs[:].rearrange("p (h d) -> p h d", h=4), hs[:].rearrange("p (h d) -> p h d", h=4),
                   hrs[:].unsqueeze(2).to_broadcast([128, 4, 128]), ALU.mult, [hs.k, hrs.k], [hs.k])
                tt("vector", mixr[n % 2][:, 512:1024], hs[:], sig[:], ALU.mult, [hs.k, sig.k], ["mixM%d" % (n % 2)])
                yield

            def gen_tail(b, g, j):
                n = g * 4 + j
                row0 = b * SEQ + n * 128
                for kc in range(8):
                    tr(PT[:, kc * 128:(kc + 1) * 128], mixr[n % 2][:, kc * 128:(kc + 1) * 128], identb[:],
                       ["mixA%d" % (n % 2), "mixM%d" % (n % 2), identb.k], [PT.k])
                cp("scalar", mixT[:], PT[:].rearrange("p (a b) -> p a b", a=8), [PT.k], [mixT.k])
                xb = rr("xt", xt)
                dma("sync", xb[:], x_d[row0:row0 + 128, :], [], [xb.k], xb.k)
                xo = rr("xn", xn)
                yield
                for half in range(2):
                    p = nextps()
                    for kc in range(8):
                        mm(p[:], mixT[:, kc, :], wout[:, kc, half * 512:(half + 1) * 512], kc == 0, kc == 7, [mixT.k, wout.k], [p.k])
                    hsl = slice(half * 512, (half + 1) * 512)
                    tt("vector", xo[:, hsl], p[:], xb[:, hsl], ALU.add, [p.k, xb.k], [xo.k])
                    yield
                dma("sync", out_d[row0:row0 + 128, :], xo[:], [xo.k], ["outrows"], xo.k)
                yield
                s4 = rr("st", st4)
                hb2 = rr("h2b", h2b)
                act(hb2[:], xo[:], AF.Square, [xo.k], [hb2.k, s4.k], accum=s4[:, 0:1])
                act(s4[:, 1:2], s4[:, 0:1], AF.Ln, [s4.k], [s4.k], bias=EPS, scale=1.0 / D)
                act(s4[:, 2:3], s4[:, 1:2], AF.Exp, [s4.k], [s4.k], scale=-0.5)
                yield
                stt(t1[:], xo[:], s4[:, 2:3], A2[:], ALU.mult, ALU.mult, [xo.k, s4.k, A2.k], [t1.k])
                tt("vector", t1[:], t1[:], B2[:], ALU.add, [t1.k, B2.k], [t1.k])
                yield
                cp("scalar", hb2[:], h2[:], [h2.k], [hb2.k])
                dma("sync", hx2s_d[row0:row0 + 128, :], hb2[:], [hb2.k], ["hx2s"], hb2.k)
                for half in range(2):
                    p = nextps()
                    for q4 in range(4):
                        kc = half * 4 + q4
                        tr(p[:, q4 * 128:(q4 + 1) * 128], h2[:, kc * 128:(kc + 1) * 128], identf[:], [h2.k, identf.k], [p.k])
                    cp("scalar", h2T[:, half * 4:half * 4 + 4, :], p[:].rearrange("p (a b) -> p a b", a=4), [p.k], [h2T.k])
                    yield
                p = nextps()
                for kc in range(8):
                    mm(p[:, 0:16], h2T[:, kc, :], wr[:, kc, :], kc == 0, kc == 7, [h2T.k, wr.k], [p.k])
                S.op("vector", lambda e, p=p: e.tensor_reduce(out=rs1[:], in_=p[:, 0:16], axis=AX.X, op=ALU.max, negate=True),
                     reads=[p.k], writes=[rs1.k])
                act(rt[:], p[:, 0:16], AF.Exp, [p.k, rs1.k], [rt.k, rs2.k], bias=rs1[:, 0:1], accum=rs2[:, 0:1])
                yield
                recip(rs2[:], rs2[:], [rs2.k], [rs2.k])
                ts("vector", aff_all[:, n % (NT // 2), n // (NT // 2), b, :], rt[:], rs2[:, 0:1], None, ALU.mult, None, [rt.k, rs2.k], [(aff_all.k, n, b)])
                yield

            def p2_prep2(b, g):
                hb_ = HB[g % 2]
                hp = (NG + g - 1) if g > 0 else None
                hn = (g + 1) if g < NG - 1 else None
                feat_proj(C_MQ, 4, 512, mqT, 0, hp, hn, hb_)
                feat_proj(C_MK, 4, 512, mkT, 4, hp, hn, hb_)
                k_tokmajor(4)

            def p2_batch(b, ngroups):
                ntile = ngroups * 4
                if CFG.get("psv", 1) == 1:
                    al_A = RR(PB[0:2]); al_M = RR(PB[3:6]); al_T = RR([PB[2], PB[6]]); al_H = RR(PB[0:4])
                else:
                    al_A = RR(PB[0:3]); al_M = RR(PB[3:6]); al_T = RR(PB[6:7]); al_H = RR(PB[0:4])
                stage_a(xrows(b, 0), 4, A1, B1, HB[0])
                p2_prep2(b, 0)
                run([(gen_head(b, 0, 0), al_H)])
                for n in range(ntile):
                    g, j = n // 4, n % 4
                    gens = [(gen_attention(n), al_A), (gen_mlstm(b, g, j), al_M)]
                    if n > 0:
                        gens.append((gen_tail(b, (n - 1) // 4, (n - 1) % 4), al_T))
                    if j == 0:
                        bg = []
                        if g + 1 < ngroups:
                            bg = [gen_stage_tile(xrows(b, g + 1), j_, A1, B1, HB[(g + 1) % 2]) for j_ in range(4)]
                    run(gens, bg)
                    if n + 1 < ntile:
                        if j == 3:
                            drain(bg)
                            p2_prep2(b, g + 1)
                        run([(gen_head(b, (n + 1) // 4, (n + 1) % 4), al_H)], bg)
                run([(gen_tail(b, (ntile - 1) // 4, (ntile - 1) % 4), al_T)])

            stop = CFG["stop"]
            for b in range(CFG["nb"]):
                if CFG.get("lm", 3) & 1:
                    load_mod(2, {"B1": B1} if CFG.get("lm", 3) & 4 else {"A1": A1, "B1": B1})
                if CFG.get("lm", 3) & 2:
                    mset("vector", Sst["f"][:], 0.0, [Sst["f"].k])
                    mset("vector", Sst["b"][:], 0.0, [Sst["b"].k])
                if stop == "mod":
                    continue
                p1_group_ctx(b)
                load_mod(b, {"A1": A1, "B1": B1, "A2": A2, "B2": B2})
                load_mod(b, {"G1": None})
                load_wout_scaled()
                if stop == "ctx":
                    continue
                p0(b)
                if stop == "p0":
                    continue
                g1s = list(range(NG - 1, -1, -1))
                if CFG["ng1"] is not None:
                    g1s = g1s[:CFG["ng1"]]
                stage_a(xrows(b, g1s[0]), 4, A1, B1, HB[g1s[0] % 2])
                for gi_, g in enumerate(g1s):
                    p1_group(b, g, g1s[gi_ + 1] if gi_ + 1 < len(g1s) else None)
                if stop == "p1":
                    continue
                p2_batch(b, NG if CFG["ng2"] is None else CFG["ng2"])
            S.emit()
        if CFG["stop"] in ("mod", "ctx", "p0", "p1", "p2"):
            return nc

        with ExitStack() as es:
            S = Sched(nc, "b")

            def sb(name, shape, dt=F32):
                return Buf(es.enter_context(nc.sbuf_tensor("z_" + name, list(shape), dt)), "z_" + name)

            def ps(name, shape, dt=F32):
                return Buf(es.enter_context(nc.psum_tensor(name, list(shape), dt)), name)

            PB = [ps("qb%d" % i, [128, 512]) for i in range(6)]
            PTS = [ps("qtb%d" % i, [128, 1024], BF16) for i in range(2)]
            pbi = [0]

            def nextps():
                p = PB[pbi[0] % 6]
                pbi[0] += 1
                return p

            G2 = [sb("G2_%d" % b, [128, D]) for b in range(NB)]
            for b in range(NB):
                S.op("sync", lambda e, b=b: e.dma_start(out=G2[b][:], in_=modsc_d[b, 5 * D:6 * D].partition_broadcast(128)),
                     writes=[G2[b].k], dma="prm2")
            S.group_keys.add("prm2")
            HSEQ = SEQ // 2
            affT = sb("affT", [64, HSEQ])
            mx = sb("mx", [64, CAP])
            ix = sb("ix", [64, CAP], U32)
            ixf = sb("ixf", [64, CAP])
            boff = sb("boff", [64, 1])
            antiI = sb("antiI", [128, 128])
            S.op("sync", lambda e: e.dma_start(out=boff[:], in_=boff_d), writes=[boff.k], dma="prm2")
            S.op("sync", lambda e: e.dma_start(out=antiI[:], in_=anti_d), writes=[antiI.k], dma="prm2")
            for m4 in range(4):
                p = nextps()
                for q in range(4):
                    m = m4 * 4 + q
                    S.op("tensor", lambda e, p=p, q=q, m=m: e.transpose(p[0:64, q * 128:(q + 1) * 128],
                                                                          aff_all[:, m, :, :, :].rearrange("p h b e -> p (h b e)"), identf[:]),
                         reads=[], writes=[p.k])
                S.op("vector", lambda e, p=p, m4=m4: e.tensor_copy(out=affT[:, m4 * 512:(m4 + 1) * 512], in_=p[0:64, :]),
                     reads=[p.k], writes=[affT.k])
            for k in range(CAP // 8):
                sl = slice(8 * k, 8 * k + 8)
                S.op("vector", lambda e, sl=sl: e.max(out=mx[:, sl], in_=affT[:]), reads=[affT.k], writes=[mx.k])
                S.op("vector", lambda e, sl=sl: e.max_index(out=ix[:, sl], in_max=mx[:, sl], in_values=affT[:]),
                     reads=[affT.k, mx.k], writes=[ix.k])
                if k < CAP // 8 - 1:
                    S.op("vector", lambda e, sl=sl: e.match_replace(out=affT[:], in_to_replace=mx[:, sl], in_values=affT[:], imm_value=-1.0),
                         reads=[affT.k, mx.k], writes=[affT.k])
            S.op("vector", lambda e: e.tensor_copy(out=ixf[:], in_=ix[:]), reads=[ix.k], writes=[ixf.k])
            S.op("vector", lambda e: e.tensor_scalar(out=ixf[:], in0=ixf[:], scalar1=boff[:, 0:1], scalar2=None, op0=ALU.add),
                 reads=[ixf.k, boff.k], writes=[ixf.k])
            idxT = sb("idxT", [128, 4, 32], I32)
            gateT = sb("gateT", [128, 4, 32])
            Tmx = sb("Tmx", [128, 4, 64])
            Tix = sb("Tix", [128, 4, 64])
            msk = sb("msk", [128, 4, 32])
            dif = sb("dif", [128, 4, 32])
            for (src, dst) in ((mx, Tmx), (ixf, Tix)):
                p = nextps()
                for q in range(4):
                    S.op("tensor", lambda e, p=p, q=q, src=src: e.transpose(p[:, q * 64:(q + 1) * 64], src[:, q * 128:(q + 1) * 128], identf[0:64, 0:64]),
                         reads=[src.k], writes=[p.k])
                S.op("vector", lambda e, p=p, dst=dst: e.tensor_copy(out=dst[:], in_=p[:, 0:256].rearrange("p (a b) -> p a b", a=4)),
                     reads=[p.k], writes=[dst.k])
            pB = nextps()
            for q in range(4):
                S.op("tensor", lambda e, q=q: e.matmul(pB[:, q * 64:q * 64 + 32], antiI[:], Tmx[:, 3 - q, 32:64], start=True, stop=True),
                     reads=[antiI.k, Tmx.k], writes=[pB.k])
                S.op("tensor", lambda e, q=q: e.matmul(pB[:, q * 64 + 32:q * 64 + 64], antiI[:], Tix[:, 3 - q, 32:64], start=True, stop=True),
                     reads=[antiI.k, Tix.k], writes=[pB.k])
            pB3 = pB[:, 0:256].rearrange("p (a b) -> p a b", a=4)
            S.op("vector", lambda e: e.tensor_tensor(out=msk[:], in0=Tmx[:, :, 0:32], in1=pB3[:, :, 0:32], op=ALU.is_ge),
                 reads=[Tmx.k, pB.k], writes=[msk.k])
            S.op("vector", lambda e: e.tensor_tensor(out=gateT[:], in0=Tmx[:, :, 0:32], in1=pB3[:, :, 0:32], op=ALU.max),
                 reads=[Tmx.k, pB.k], writes=[gateT.k])
            S.op("vector", lambda e: e.tensor_tensor(out=dif[:], in0=Tix[:, :, 0:32], in1=pB3[:, :, 32:64], op=ALU.subtract),
                 reads=[Tix.k, pB.k], writes=[dif.k])
            S.op("vector", lambda e: e.tensor_tensor(out=dif[:], in0=dif[:], in1=msk[:], op=ALU.mult),
                 reads=[dif.k, msk.k], writes=[dif.k])
            S.op("vector", lambda e: e.tensor_tensor(out=dif[:], in0=dif[:], in1=pB3[:, :, 32:64], op=ALU.add),
                 reads=[dif.k, pB.k], writes=[dif.k])
            S.op("vector", lambda e: e.tensor_copy(out=idxT[:], in_=dif[:]), reads=[dif.k], writes=[idxT.k])

            wgb = [sb("wg%d" % i, [128, 8, 512], BF16) for i in range(3)]
            wub = [sb("wu%d" % i, [128, 8, 512], BF16) for i in range(3)]
            wdb = [sb("wd%d" % i, [128, 4, D], BF16) for i in range(3)]
            xg = [sb("xg%d" % i, [128, D], BF16) for i in range(12)]
            xgTs = [sb("xgT%d" % i, [128, 8, 512], BF16) for i in range(2)]
            sgs = [sb("sg%d" % i, [128, 512]) for i in range(2)]
            hTe = sb("hTe", [128, 4, 512], BF16)
            ptc = [0]
            ys = [sb("ys%d" % i, [128, D]) for i in range(4)]

            stg = [sb("stg%d" % i, [128, 512]) for i in range(6)]
            stc = [0]

            def w_chunks(ex):
                wg_, wu_, wd_ = wgb[ex % 3], wub[ex % 3], wdb[ex % 3]
                out = []
                for kc in range(8):
                    out.append((wg_[:, kc, :], wg_.k, wg_d[ex, kc * 128:(kc + 1) * 128, :]))
                    out.append((wu_[:, kc, :], wu_.k, wu_d[ex, kc * 128:(kc + 1) * 128, :]))
                for fc in range(4):
                    for c0 in (0, 512):
                        out.append((wd_[:, fc, c0:c0 + 512], wd_.k, wd_d[ex, fc * 128:(fc + 1) * 128, c0:c0 + 512]))
                return out

            def load_chunk(ch):
                dst, dkey, src = ch
                st = stg[stc[0] % 6]
                eng = "scalar" if stc[0] % 2 == 0 else "gpsimd"
                stc[0] += 1
                S.op("sync", lambda e, st=st, src=src: e.dma_start(out=st[:], in_=src), writes=[st.k], dma=st.k)
                if eng == "scalar":
                    S.op("scalar", lambda e, st=st, dst=dst: e.copy(out=dst, in_=st[:]), reads=[st.k], writes=[dkey])
                else:
                    S.op("gpsimd", lambda e, st=st, dst=dst: e.tensor_copy(out=dst, in_=st[:]), reads=[st.k], writes=[dkey])

            def load_w(ex):
                for ch in w_chunks(ex):
                    load_chunk(ch)

            wstream = []
            for ex_ in range(2, NEXP):
                wstream.extend(w_chunks(ex_))

            def gen_wload(i):
                lo = 12 * (i - 1)
                for ch in wstream[lo:lo + 12] if i >= 1 else []:
                    load_chunk(ch)
                    yield

            items = [(ex, b) for ex in range(NEXP) for b in range(NB)]

            def gathers(i):
                ex, b = items[i]
                pe = b * 16 + ex
                for j in range(4):
                    xb = xg[(i % 3) * 4 + j]
                    S.op("gpsimd", lambda e, j=j, pe=pe, xb=xb: e.indirect_dma_start(
                        out=xb[:], out_offset=None, in_=hx2s_d,
                        in_offset=bass.IndirectOffsetOnAxis(ap=idxT[:, j, pe:pe + 1], axis=0)),
                        reads=[idxT.k], writes=[xb.k], dma=xb.k)

            def gen_trans(i):
                xgT = xgTs[i % 2]
                for j in range(4):
                    xb = xg[(i % 3) * 4 + j]
                    PT = PTS[ptc[0] % 2]
                    ptc[0] += 1
                    for kc in range(8):
                        S.op("tensor", lambda e, xb=xb, kc=kc, PT=PT: e.transpose(PT[:, kc * 128:(kc + 1) * 128], xb[:, kc * 128:(kc + 1) * 128], identb[:]),
                             reads=[xb.k], writes=[PT.k])
                    if j % 2 == 0:
                        S.op("scalar", lambda e, j=j, PT=PT, xgT=xgT: e.copy(out=xgT[:, :, j * 128:(j + 1) * 128], in_=PT[:].rearrange("p (a b) -> p a b", a=8)),
                             reads=[PT.k], writes=[xgT.k])
                    else:
                        S.op("vector", lambda e, j=j, PT=PT, xgT=xgT: e.tensor_copy(out=xgT[:, :, j * 128:(j + 1) * 128], in_=PT[:].rearrange("p (a b) -> p a b", a=8)),
                             reads=[PT.k], writes=[xgT.k])
                    yield

            def gen_ffn(i):
                ex, b = items[i]
                pe = b * 16 + ex
                xgT = xgTs[i % 2]
                wg_, wu_, wd_ = wgb[ex % 3], wub[ex % 3], wdb[ex % 3]
                for fc in range(4):
                    pg = nextps(); pu = nextps()
                    sg = sgs[fc % 2]
                    for kc in range(8):
                        S.op("tensor", lambda e, pg=pg, kc=kc, fc=fc: e.matmul(pg[:], wg_[:, kc, fc * 128:(fc + 1) * 128], xgT[:, kc, :],
                                                                             start=(kc == 0), stop=(kc == 7)),
                             reads=[wg_.k, xgT.k], writes=[pg.k])
                    for kc in range(8):
                        S.op("tensor", lambda e, pu=pu, kc=kc, fc=fc: e.matmul(pu[:], wu_[:, kc, fc * 128:(fc + 1) * 128], xgT[:, kc, :],
                                                                             start=(kc == 0), stop=(kc == 7)),
                             reads=[wu_.k, xgT.k], writes=[pu.k])
                    S.op("scalar", lambda e, pg=pg, sg=sg: e.activation(out=sg[:], in_=pg[:], func=AF.Silu), reads=[pg.k], writes=[sg.k])
                    S.op("vector", lambda e, pu=pu, fc=fc, sg=sg: e.tensor_tensor(out=hTe[:, fc, :], in0=pu[:], in1=sg[:], op=ALU.mult),
                         reads=[pu.k, sg.k], writes=[hTe.k])
                    yield
                for j in range(4):
                    for half in range(2):
                        p = nextps()
                        for fc in range(4):
                            S.op("tensor", lambda e, p=p, fc=fc, j=j, half=half: e.matmul(
                                p[:], hTe[:, fc, j * 128:(j + 1) * 128], wd_[:, fc, half * 512:(half + 1) * 512],
                                start=(fc == 0), stop=(fc == 3)), reads=[hTe.k, wd_.k], writes=[p.k])
                        S.op("vector", lambda e, p=p, j=j, half=half: e.scalar_tensor_tensor(
                            out=ys[j][:, half * 512:(half + 1) * 512], in0=p[:], scalar=gateT[:, j, pe:pe + 1],
                            in1=G2[b][:, half * 512:(half + 1) * 512], op0=ALU.mult, op1=ALU.mult),
                            reads=[p.k, gateT.k, G2[b].k], writes=[ys[j].k])
                    S.op("gpsimd", lambda e, j=j: e.indirect_dma_start(
                        out=out_d, out_offset=bass.IndirectOffsetOnAxis(ap=idxT[:, j, pe:pe + 1], axis=0),
                        in_=ys[j][:], in_offset=None, compute_op=ALU.add),
                        reads=[ys[j].k, idxT.k] + ["osc%d_%d_%d" % (b, (ex + 1) % 2, jj) for jj in range(4)],
                        writes=["osc%d_%d_%d" % (b, ex % 2, j)], dma=ys[j].k)
                    yield

            def run2(gens):
                live = list(gens)
                while live:
                    for g_ in list(live):
                        try:
                            next(g_)
                        except StopIteration:
                            live.remove(g_)

            load_w(0)
            load_w(1)
            gathers(0)
            gathers(1)
            run2([gen_trans(0)])
            for i in range(len(items)):
                ex, b = items[i]
                if i + 2 < len(items):
                    gathers(i + 2)
                if i + 1 < len(items):
                    run2([gen_ffn(i), gen_trans(i + 1), gen_wload(i)])
                else:
                    run2([gen_ffn(i), gen_wload(i)])
            S.emit()
    return nc


_CACHE = {}


def _consts():
    import ml_dtypes
    bf = ml_dtypes.bfloat16
    n = SEQ
    rows = n // 64
    row, col = np.meshgrid(np.arange(rows), np.arange(64), indexing="ij")
    n_freq = 16
    freqs = (10000.0 ** (-np.arange(n_freq, dtype=np.float32) / n_freq)).astype(np.float32)
    ang = np.concatenate([row.reshape(-1, 1).astype(np.float32) * freqs, col.reshape(-1, 1).astype(np.float32) * freqs], -1)
    s = np.arange(128)[:, None]
    t = np.arange(128)[None, :]
    c = {
        "rope_cos": np.cos(ang).astype(np.float32),
        "rope_sin": np.sin(ang).astype(np.float32),
        "ident_bf": np.eye(128, dtype=np.float32).astype(bf),
        "ident_f": np.eye(128, dtype=np.float32),
        "tri_f": (s > t).astype(np.float32),
        "tri_b": (s < t).astype(np.float32),
        "neg_ones": -np.ones((128, 128), np.float32),
        "mask_f": ((s <= t) * QS).astype(np.float32).astype(bf),
        "mask_b": ((s >= t) * QS).astype(np.float32).astype(bf),
        "amask_lo": (s >= t).astype(np.float32).astype(bf),
        "amask_hi": (s <= t).astype(np.float32).astype(bf),
        "boff": np.array([(p_ // 32) * (SEQ // 2) + ((p_ % 32) // 16) * SEQ for p_ in range(64)], np.float32).reshape(64, 1),
        "anti_ident": np.ascontiguousarray(np.eye(128, dtype=np.float32)[::-1]),
    }
    return c


def _perm_win(w_in):
    w = w_in
    aq = w[:, 0:512]
    ak = w[:, 512:640]
    av = w[:, 640:768]
    mq = w[:, 768:1280]
    mk = w[:, 1280:1792]
    mv = w[:, 1792:2304]
    mo = w[:, 2304:2816]
    gt = w[:, 2816:2832]
    heads = [aq[:, h * 64:(h + 1) * 64] for h in range(8)]
    aqp = np.concatenate([np.concatenate([heads[p], heads[4 + p]], 1) for p in range(4)], 1)
    return np.ascontiguousarray(np.concatenate([aqp, mo, ak, av, mv, gt, mq, mk], 1))


def kernel(x, c, ctx, c_ctx, w_mod, b_mod, norm1_w, norm2_w, w_in, b_gates, conv_qk, q_norm_w,
           k_norm_w, sink, mlstm_norm_w, w_out, w_router, w_gate, w_up, w_down):
    f = lambda a: np.ascontiguousarray(np.asarray(a, dtype=np.float32))
    x = f(x); c = f(c); ctx = f(ctx); c_ctx = f(c_ctx)
    if "nc" not in _CACHE:
        _CACHE["nc"] = build_program()
    nc = _CACHE["nc"]
    consts = _consts()
    shared = {
        "w_mod": f(w_mod)[0], "b_mod": f(b_mod), "norm1_w": f(norm1_w), "norm2_w": f(norm2_w),
        "w_in": _perm_win(f(w_in)[0]), "b_gates": f(b_gates), "conv_qk": f(conv_qk)[0],
        "q_norm_w": f(q_norm_w), "k_norm_w": f(k_norm_w), "sink": f(sink), "mlstm_norm_w": f(mlstm_norm_w),
        "w_out": f(w_out)[0], "w_router": f(w_router)[0], "w_gate": f(w_gate)[0], "w_up": f(w_up)[0],
        "w_down": f(w_down)[0],
    }
    shared.update(consts)
    if CFG["stop"] is not None:
        for k in ("w_gate", "w_up", "w_down"):
            shared.pop(k)
    in_maps = []
    for i in range(8):
        m = dict(shared)
        m["x"] = x[2 * i:2 * i + 2].reshape(NB * SEQ, D)
        m["ctx"] = ctx[2 * i:2 * i + 2].reshape(NB * CTX, D)
        m["cvec"] = np.ascontiguousarray(np.stack([c[2 * i], c[2 * i + 1], c_ctx], 0))
        in_maps.append(m)
    res = run_bass_kernel_spmd(nc, in_maps, core_ids=list(range(8)))
    _CACHE["last"] = res.results
    out = np.concatenate([np.asarray(r["out"]).reshape(NB, SEQ, D) for r in res.results], 0)
    return out.astype(np.float32)
```
